# Optimizing a Trainium2 kernel written in Bass

```python
import math
import jax
import jax.numpy as jnp
from jax import lax
import numpy as np

D_MODEL = 1024
BATCH = 8
SEQ = 8192
DEPTH = 2

CHUNK = 64
PLE_DIM = 256
N_BRANCH = 4
LN_EPS = 1e-5
DEEPNORM_ALPHA = (2 * DEPTH) ** 0.25
DEEPNORM_BETA = (8 * DEPTH) ** -0.25

SB_HEADS = 4
SB_HEAD_DIM = 64
SB_BLOCK = 128
SB_W = SB_HEADS * SB_HEAD_DIM
SG_GROUPS = 4
SG_GROUP_DIM = 64
SG_CHUNK = 128
SG_W = SG_GROUPS * SG_GROUP_DIM
CA_HEADS = 4
CA_HEAD_DIM = 64
CA_LEFT_CHUNKS = 8
CA_BAND = (CA_LEFT_CHUNKS + 1) * CHUNK
CA_REL_MAX = 256
CA_REL_SIZE = (CHUNK - 1) + CA_REL_MAX + 1
CA_W = CA_HEADS * CA_HEAD_DIM
SSD_HEADS = 8
SSD_HEAD_DIM = 64
SSD_GROUPS = 2
SSD_HPG = SSD_HEADS // SSD_GROUPS
SSD_STATE = 64
SSD_CONV = 4
SSD_CHUNK = CHUNK
SSD_INNER = SSD_HEADS * SSD_HEAD_DIM
SSD_CONV_DIM = SSD_INNER + 2 * SSD_GROUPS * SSD_STATE
D_FF = 2816
N_EXPERTS = 8
TOP_K = 2
D_FF_EXPERT = 3584
N_DENSE = (DEPTH + 1) // 2
N_MOE = DEPTH // 2

COLS_A = 3 * SB_W
COLS_B = 2 * SG_W
COLS_C = 3 * CA_W
COLS_D = SSD_INNER + SSD_CONV_DIM + SSD_HEADS
COLS_GATE = N_BRANCH * D_MODEL
D_IN = COLS_A + COLS_B + COLS_C + COLS_D + COLS_GATE

kernel_name = 'hybrid_sb_sgmlp_band_ssd_moe_trunk'


def _split_last(t, sizes):
    out, start = [], 0
    for n in sizes:
        out.append(t[..., start:start + n])
        start += n
    return out


def layer_norm(x, g, b):
    xf = x.astype(jnp.float32)
    mu = jnp.mean(xf, axis=-1, keepdims=True)
    var = jnp.mean(jnp.square(xf - mu), axis=-1, keepdims=True)
    return ((xf - mu) * lax.rsqrt(var + LN_EPS) * g + b).astype(x.dtype)


def stick_breaking_attention(q, k, v):
    bsz, seq = q.shape[0], q.shape[1]
    nb = seq // SB_BLOCK
    scale = SB_HEAD_DIM ** -0.5
    kf = k.astype(jnp.float32)
    vf = v.astype(jnp.float32)
    key_pos = jnp.arange(seq)
    q_blocks = jnp.moveaxis(q.reshape(bsz, nb, SB_BLOCK, SB_HEADS, SB_HEAD_DIM), 1, 0)

    def block(args):
        qb, bi = args
        z = jnp.einsum('bthd,bshd->bhts', qb.astype(jnp.float32), kf) * scale
        q_pos = bi * SB_BLOCK + jnp.arange(SB_BLOCK)
        mask = key_pos[None, :] < q_pos[:, None]
        log_beta = jnp.where(mask, jax.nn.log_sigmoid(z), -jnp.inf)
        log_keep = jnp.where(mask, jax.nn.log_sigmoid(-z), 0.0)
        later = lax.cumsum(log_keep, axis=3, reverse=True) - log_keep
        a = jnp.exp(log_beta + later)
        return jnp.einsum('bhts,bshd->bthd', a, vf)

    out = lax.map(block, (q_blocks, jnp.arange(nb)))
    return jnp.moveaxis(out, 0, 1).reshape(bsz, seq, SB_W).astype(q.dtype)


def spatial_gating(uv, ln_g, ln_b, w_s, b_s):
    bsz, seq = uv.shape[0], uv.shape[1]
    u, v = uv[..., :SG_W], uv[..., SG_W:]
    v = layer_norm(v, ln_g, ln_b)
    nc = seq // SG_CHUNK
    v = v.reshape(bsz, nc, SG_CHUNK, SG_GROUPS, SG_GROUP_DIM)
    causal = jnp.tril(jnp.ones((SG_CHUNK, SG_CHUNK), dtype=bool))
    w = jnp.where(causal[None], w_s, jnp.zeros_like(w_s))
    mixed = jnp.einsum('gts,bcsgd->bctgd', w, v) + jnp.transpose(b_s)[None, None, :, :, None]
    return u * mixed.reshape(bsz, seq, SG_W)


def chunk_band_attention(q, k, v, rel_bias):
    bsz, seq = q.shape[0], q.shape[1]
    nc = seq // CHUNK
    scale = CA_HEAD_DIM ** -0.5

    def chunks(t):
        return t.reshape(bsz, nc, CHUNK, CA_HEADS, CA_HEAD_DIM)

    pad = ((0, 0), (CA_LEFT_CHUNKS, 0), (0, 0), (0, 0), (0, 0))
    kp = jnp.pad(chunks(k), pad)
    vp = jnp.pad(chunks(v), pad)
    band_idx = jnp.arange(nc)[:, None] + jnp.arange(CA_LEFT_CHUNKS + 1)[None, :]
    kb = kp[:, band_idx].reshape(bsz, nc, CA_BAND, CA_HEADS, CA_HEAD_DIM)
    vb = vp[:, band_idx].reshape(bsz, nc, CA_BAND, CA_HEADS, CA_HEAD_DIM)
    scores = jnp.einsum('bcihd,bcjhd->bchij', chunks(q).astype(jnp.float32),
                        kb.astype(jnp.float32)) * scale
    i = jnp.arange(CHUNK)[:, None]
    j = jnp.arange(CA_BAND)[None, :]
    rel = CA_LEFT_CHUNKS * CHUNK + i - j
    rel_idx = jnp.clip(rel, -(CHUNK - 1), CA_REL_MAX) + (CHUNK - 1)
    bias = rel_bias.astype(jnp.float32)[:, rel_idx]
    key_pos = (jnp.arange(nc)[:, None] - CA_LEFT_CHUNKS) * CHUNK + jnp.arange(CA_BAND)[None, :]
    valid = key_pos >= 0
    scores = jnp.where(valid[None, :, None, None, :], scores + bias[None, None], -jnp.inf)
    probs = jax.nn.softmax(scores, axis=-1)
    out = jnp.einsum('bchij,bcjhd->bcihd', probs, vb.astype(jnp.float32))
    return out.reshape(bsz, seq, CA_W).astype(q.dtype)


def causal_depthwise_conv(x, w):
    return lax.conv_general_dilated(
        x, w[:, None, :], window_strides=(1,), padding=[(SSD_CONV - 1, 0)],
        dimension_numbers=('NWC', 'WIO', 'NWC'), feature_group_count=x.shape[-1])


def ssd_mixer(zxbcdt, conv_w, conv_b, dt_bias, a_log, d_skip, norm_g):
    bsz, seq = zxbcdt.shape[0], zxbcdt.shape[1]
    f32 = jnp.float32
    z, xbc, dt = _split_last(zxbcdt, [SSD_INNER, SSD_CONV_DIM, SSD_HEADS])
    xbc = jax.nn.silu(causal_depthwise_conv(xbc, conv_w) + conv_b)
    xs, bm, cm = _split_last(xbc, [SSD_INNER, SSD_GROUPS * SSD_STATE, SSD_GROUPS * SSD_STATE])
    dt = jax.nn.softplus(dt.astype(f32) + dt_bias)
    a = -jnp.exp(a_log.astype(f32))
    nc = seq // SSD_CHUNK
    x = xs.astype(f32).reshape(bsz, nc, SSD_CHUNK, SSD_GROUPS, SSD_HPG, SSD_HEAD_DIM)
    bm = bm.astype(f32).reshape(bsz, nc, SSD_CHUNK, SSD_GROUPS, SSD_STATE)
    cm = cm.astype(f32).reshape(bsz, nc, SSD_CHUNK, SSD_GROUPS, SSD_STATE)
    dt = dt.reshape(bsz, nc, SSD_CHUNK, SSD_GROUPS, SSD_HPG)
    a_cs = jnp.cumsum(dt * a.reshape(SSD_GROUPS, SSD_HPG), axis=2)
    xdt = x * dt[..., None]
    causal = jnp.tril(jnp.ones((SSD_CHUNK, SSD_CHUNK), dtype=bool))[None, None, :, :, None, None]
    seg = a_cs[:, :, :, None] - a_cs[:, :, None, :]
    decay = jnp.exp(jnp.where(causal, seg, -jnp.inf))
    cb = jnp.einsum('bclgn,bcsgn->bclsg', cm, bm)
    y_diag = jnp.einsum('bclsgh,bcsghp->bclghp', cb[..., None] * decay, xdt)
    decay_to_end = jnp.exp(a_cs[:, :, -1:] - a_cs)
    states = jnp.einsum('bcsgn,bcsgh,bcsghp->bcghpn', bm, decay_to_end, xdt)
    chunk_decay = jnp.exp(a_cs[:, :, -1])

    def step(h, inp):
        st, dec = inp
        return h * dec[..., None, None] + st, h

    h0 = jnp.zeros((bsz, SSD_GROUPS, SSD_HPG, SSD_HEAD_DIM, SSD_STATE), f32)
    _, h_prev = lax.scan(step, h0, (jnp.moveaxis(states, 1, 0), jnp.moveaxis(chunk_decay, 1, 0)))
    h_prev = jnp.moveaxis(h_prev, 0, 1)
    y_off = jnp.einsum('bclgn,bcghpn,bclgh->bclghp', cm, h_prev, jnp.exp(a_cs))
    y = y_diag + y_off + x * d_skip.astype(f32).reshape(SSD_GROUPS, SSD_HPG)[..., None]
    y = y.reshape(bsz, seq, SSD_INNER) * jax.nn.silu(z.astype(f32))
    yg = y.reshape(bsz, seq, SSD_GROUPS, SSD_INNER // SSD_GROUPS)
    yg = yg * lax.rsqrt(jnp.mean(jnp.square(yg), axis=-1, keepdims=True) + LN_EPS)
    return (yg.reshape(bsz, seq, SSD_INNER) * norm_g).astype(zxbcdt.dtype)


def swiglu(x, w_gate, w_up, w_down):
    return (jax.nn.silu(x @ w_gate) * (x @ w_up)) @ w_down


def moe_swiglu(x, w_router, w_gate, w_up, w_down):
    logits = jnp.einsum('bsd,de->bse', x, w_router).astype(jnp.float32)
    top_val, top_idx = lax.top_k(logits, TOP_K)
    gates = jax.nn.softmax(top_val, axis=-1)
    combine = jnp.sum(jax.nn.one_hot(top_idx, N_EXPERTS, dtype=jnp.float32) * gates[..., None],
                      axis=-2).astype(x.dtype)
    y = jnp.zeros_like(x)
    for e in range(N_EXPERTS):
        y = y + combine[..., e:e + 1] * swiglu(x, w_gate[e], w_up[e], w_down[e])
    return y


def setup_inputs(seed: int = 0) -> dict:
    key = jax.random.key(seed)
    k = jax.random.split(key, 32)
    f32 = jnp.float32

    def nrm(i, shape, scale):
        return jax.random.normal(k[i], shape, f32) * scale

    dt0 = jnp.exp(jax.random.uniform(k[15], (DEPTH, SSD_HEADS), f32,
                                     math.log(1e-3), math.log(1e-1)))
    return {
        'x': nrm(0, (BATCH, SEQ, D_MODEL), 1.0),
        'p': nrm(1, (DEPTH, BATCH, SEQ, PLE_DIM), 1.0),
        'w_in': nrm(2, (DEPTH, D_MODEL, D_IN), D_MODEL ** -0.5),
        'w_br_a': nrm(3, (DEPTH, SB_W, D_MODEL), SB_W ** -0.5),
        'w_br_b': nrm(4, (DEPTH, SG_W, D_MODEL), SG_W ** -0.5),
        'w_br_c': nrm(5, (DEPTH, CA_W, D_MODEL), CA_W ** -0.5),
        'w_br_d': nrm(6, (DEPTH, SSD_INNER, D_MODEL), SSD_INNER ** -0.5),
        'w_out': nrm(7, (DEPTH, D_MODEL, D_MODEL), DEEPNORM_BETA * D_MODEL ** -0.5),
        'sg_ln_g': 1.0 + nrm(8, (DEPTH, SG_W), 0.02),
        'sg_ln_b': nrm(9, (DEPTH, SG_W), 0.02),
        'sg_w': nrm(10, (DEPTH, SG_GROUPS, SG_CHUNK, SG_CHUNK), SG_CHUNK ** -0.5),
        'sg_b': 1.0 + nrm(11, (DEPTH, SG_GROUPS, SG_CHUNK), 0.1),
        'ca_rel_bias': nrm(12, (DEPTH, CA_HEADS, CA_REL_SIZE), 0.1),
        'ssd_conv_w': nrm(13, (DEPTH, SSD_CONV, SSD_CONV_DIM), SSD_CONV ** -0.5),
        'ssd_conv_b': nrm(14, (DEPTH, SSD_CONV_DIM), 0.02),
        'ssd_dt_bias': dt0 + jnp.log(-jnp.expm1(-dt0)),
        'ssd_a_log': jnp.log(jax.random.uniform(k[16], (DEPTH, SSD_HEADS), f32, 1.0, 16.0)),
        'ssd_d': 1.0 + nrm(17, (DEPTH, SSD_HEADS), 0.1),
        'ssd_norm_g': 1.0 + nrm(18, (DEPTH, SSD_INNER), 0.02),
        'ln1_g': 1.0 + nrm(19, (DEPTH, D_MODEL), 0.02),
        'ln1_b': nrm(20, (DEPTH, D_MODEL), 0.02),
        'ffn_w_gate': nrm(21, (N_DENSE, D_MODEL, D_FF), D_MODEL ** -0.5),
        'ffn_w_up': nrm(22, (N_DENSE, D_MODEL, D_FF), D_MODEL ** -0.5),
        'ffn_w_down': nrm(23, (N_DENSE, D_FF, D_MODEL), DEEPNORM_BETA * D_FF ** -0.5),
        'moe_router': nrm(24, (N_MOE, D_MODEL, N_EXPERTS), D_MODEL ** -0.5),
        'moe_w_gate': nrm(25, (N_MOE, N_EXPERTS, D_MODEL, D_FF_EXPERT), D_MODEL ** -0.5),
        'moe_w_up': nrm(26, (N_MOE, N_EXPERTS, D_MODEL, D_FF_EXPERT), D_MODEL ** -0.5),
        'moe_w_down': nrm(27, (N_MOE, N_EXPERTS, D_FF_EXPERT, D_MODEL),
                          DEEPNORM_BETA * D_FF_EXPERT ** -0.5),
        'ple_w_gate': nrm(28, (DEPTH, D_MODEL, D_MODEL), D_MODEL ** -0.5),
        'ple_w_proj': nrm(29, (DEPTH, PLE_DIM, D_MODEL), DEEPNORM_BETA * PLE_DIM ** -0.5),
        'ln2_g': 1.0 + nrm(30, (DEPTH, D_MODEL), 0.02),
        'ln2_b': nrm(31, (DEPTH, D_MODEL), 0.02),
    }


def reference(x, p, w_in, w_br_a, w_br_b, w_br_c, w_br_d, w_out, sg_ln_g, sg_ln_b, sg_w, sg_b,
              ca_rel_bias, ssd_conv_w, ssd_conv_b, ssd_dt_bias, ssd_a_log, ssd_d, ssd_norm_g,
              ln1_g, ln1_b, ffn_w_gate, ffn_w_up, ffn_w_down, moe_router, moe_w_gate, moe_w_up,
              moe_w_down, ple_w_gate, ple_w_proj, ln2_g, ln2_b):
    bsz, seq = x.shape[0], x.shape[1]
    for i in range(DEPTH):
        h = x @ w_in[i]
        qkv_a, uv_b, qkv_c, zxbcdt_d, gate_logits = _split_last(
            h, [COLS_A, COLS_B, COLS_C, COLS_D, COLS_GATE])
        qkv_a = qkv_a.reshape(bsz, seq, 3, SB_HEADS, SB_HEAD_DIM)
        y_a = stick_breaking_attention(qkv_a[:, :, 0], qkv_a[:, :, 1], qkv_a[:, :, 2])
        y_b = spatial_gating(jax.nn.gelu(uv_b), sg_ln_g[i], sg_ln_b[i], sg_w[i], sg_b[i])
        qkv_c = qkv_c.reshape(bsz, seq, 3, CA_HEADS, CA_HEAD_DIM)
        y_c = chunk_band_attention(qkv_c[:, :, 0], qkv_c[:, :, 1], qkv_c[:, :, 2], ca_rel_bias[i])
        y_d = ssd_mixer(zxbcdt_d, ssd_conv_w[i], ssd_conv_b[i], ssd_dt_bias[i], ssd_a_log[i],
                        ssd_d[i], ssd_norm_g[i])
        g = jax.nn.sigmoid(gate_logits.reshape(bsz, seq, N_BRANCH, D_MODEL))
        merged = (g[:, :, 0] * (y_a @ w_br_a[i]) + g[:, :, 1] * (y_b @ w_br_b[i])
                  + g[:, :, 2] * (y_c @ w_br_c[i]) + g[:, :, 3] * (y_d @ w_br_d[i]))
        x = layer_norm(DEEPNORM_ALPHA * x + merged @ w_out[i], ln1_g[i], ln1_b[i])
        if i % 2 == 0:
            f = swiglu(x, ffn_w_gate[i // 2], ffn_w_up[i // 2], ffn_w_down[i // 2])
        else:
            f = moe_swiglu(x, moe_router[i // 2], moe_w_gate[i // 2], moe_w_up[i // 2],
                           moe_w_down[i // 2])
        ple = jax.nn.sigmoid(x @ ple_w_gate[i]) * (p[i] @ ple_w_proj[i])
        x = layer_norm(DEEPNORM_ALPHA * x + f + ple, ln2_g[i], ln2_b[i])
    return x
```

```python
import contextlib
import numpy as np
import concourse.bass as bass
import concourse.mybir as mybir
from concourse.bass_utils import run_bass_kernel_spmd

F32 = mybir.dt.float32
BF16 = mybir.dt.bfloat16
AF = mybir.ActivationFunctionType
ALU = mybir.AluOpType

NDMA_SEM = 14
ALPHA = 4 ** 0.25
LN_EPS = 1e-5
D_FF = 2816
D_FFE = 3584
NEG = -30000.0


class Res:
    __slots__ = ("name", "w", "r", "excl")

    def __init__(self, name, excl=False):
        self.name = name
        self.w = None
        self.r = []
        self.excl = excl


class Op:
    __slots__ = ("eng", "fn", "deps", "signal", "val", "semkey", "isdma")

    def __init__(self, eng, fn):
        self.eng = eng
        self.fn = fn
        self.deps = []
        self.signal = False
        self.val = None
        self.semkey = None
        self.isdma = False


class Prog:
    def __init__(self, nc):
        self.nc = nc
        self.ops = {e: [] for e in ("pe", "act", "dve", "pool", "sp")}
        self.dma_rr = {"sp": 0, "pool": 0}
        self.dma_last = {}

    def _deps(self, op, reads, writes):
        ex = [r for r in reads if r.excl]
        if ex:
            reads = [r for r in reads if not r.excl]
            writes = list(writes) + ex
        deps = []
        for r in reads:
            if r.w is not None:
                deps.append(r.w)
        for r in writes:
            if r.w is not None:
                deps.append(r.w)
            deps.extend(r.r)
        for r in reads:
            r.r.append(op)
        for r in writes:
            r.w = op
            r.r = []
        seen = set()
        out = []
        for d in deps:
            if id(d) not in seen and d is not op:
                seen.add(id(d))
                out.append(d)
        return out

    def op(self, eng, fn, reads=(), writes=()):
        o = Op(eng, fn)
        o.semkey = eng
        o.deps = self._deps(o, reads, writes)
        if eng == "pe":
            o.deps = [d for d in o.deps if not (d.eng == "pe" and not d.isdma)]
        self.ops[eng].append(o)
        return o

    def dma(self, queue, fn, reads=(), writes=()):
        o = Op(queue, fn)
        o.isdma = True
        slot = self.dma_rr[queue]
        self.dma_rr[queue] = (slot + 1) % NDMA_SEM
        o.semkey = (queue, slot)
        o.deps = self._deps(o, reads, writes)
        prev = self.dma_last.get(o.semkey)
        if prev is not None:
            o.deps.append(prev)
        self.dma_last[o.semkey] = o
        o.signal = True
        self.ops[queue].append(o)
        return o

    def barrier(self):
        lasts = []
        for e, lst in self.ops.items():
            if lst:
                lasts.append(lst[-1])
        for k, o in self.dma_last.items():
            lasts.append(o)
        for e in ("pe", "act", "dve", "pool", "sp"):
            o = Op(e, lambda eng: eng.nop())
            o.semkey = e
            o.deps = [d for d in lasts if d.eng != e or d.isdma]
            self.ops[e].append(o)

    def emit(self, final_ops=()):
        nc = self.nc
        for e, lst in self.ops.items():
            for o in lst:
                for d in o.deps:
                    d.signal = True
        for o in final_ops:
            o.signal = True
        cnt = {}
        for e, lst in self.ops.items():
            for o in lst:
                if o.signal:
                    inc = 16 if o.isdma else 1
                    cnt[o.semkey] = cnt.get(o.semkey, 0) + inc
                    o.val = cnt[o.semkey]
        self.maxcnt = dict(cnt)
        with contextlib.ExitStack() as st:
            sems = {}
            for k in cnt:
                nm = k if isinstance(k, str) else f"{k[0]}{k[1]}"
                sems[k] = st.enter_context(nc.semaphore("s_" + nm))
            block = st.enter_context(nc.Block())
            engmap = {"pe": block.tensor, "act": block.scalar, "dve": block.vector,
                      "pool": block.gpsimd, "sp": block.sync}

            def make(e):
                lst = self.ops[e]

                def body(eng):
                    known = {}
                    for o in lst:
                        need = {}
                        for d in o.deps:
                            if d.val > known.get(d.semkey, 0):
                                need[d.semkey] = max(need.get(d.semkey, 0), d.val)
                        for k, v in need.items():
                            eng.wait_ge(sems[k], v)
                            known[k] = v
                        ins = o.fn(eng)
                        if o.signal:
                            ins.then_inc(sems[o.semkey], 16 if o.isdma else 1)
                    if e == "sp":
                        for o in final_ops:
                            if o.val > known.get(o.semkey, 0):
                                eng.wait_ge(sems[o.semkey], o.val)
                                known[o.semkey] = o.val
                return body

            for e in ("pe", "act", "dve", "pool", "sp"):
                engmap[e](make(e))


class Arena:
    def __init__(self, nc, st, nbytes):
        self.n4 = nbytes // 4
        self.t = st.enter_context(nc.sbuf_tensor("arena", [128, self.n4], F32))
        self.top = 0

    def alloc(self, shape, dt):
        esz = 2 if dt == BF16 else 4
        n = 1
        for s in shape:
            n *= s
        nb = (n * esz + 63) // 64 * 64
        off4 = self.top // 4
        self.top += nb
        assert self.top // 4 <= self.n4, ("arena overflow", self.top)
        v = self.t[:, off4:off4 + nb // 4]
        if dt != F32:
            v = v.bitcast(dt)
        v = v[:, 0:n]
        if len(shape) == 2:
            v = v.rearrange("p (a b) -> p a b", a=shape[0])
        elif len(shape) == 3:
            v = v.rearrange("p (a b c) -> p a b c", a=shape[0], b=shape[1])
        return v


class RR:
    def __init__(self, items):
        self.items = items
        self.i = 0

    def next(self):
        it = self.items[self.i % len(self.items)]
        self.i += 1
        return it


def build(S, dbg=(), phases=("p0", "l0", "l1"), subph=("1", "A", "C", "D", "34")):
    nc = bass.Bass("TRN2", target_bir_lowering=False)
    NT = S // 512
    NCH = S // 128
    I = {}

    def din(name, shape, dt=F32):
        I[name] = nc.dram_tensor(name, list(shape), dt, kind="ExternalInput").ap()
        return I[name]

    def dscr(name, shape, dt):
        kind = "ExternalOutput" if name in dbg else "Internal"
        return nc.dram_tensor(name, list(shape), dt, kind=kind).ap()

    x = din("x", [S, 1024])
    pin = din("p", [2, S, 256])
    w_in = din("w_in", [2, 1024, 7432])
    w_br = [din("w_br_a", [2, 256, 1024]), din("w_br_b", [2, 256, 1024]),
            din("w_br_c", [2, 256, 1024]), din("w_br_d", [2, 512, 1024])]
    w_out = din("w_out", [2, 1024, 1024])
    sg_ln_g = din("sg_ln_g", [2, 256]); sg_ln_b = din("sg_ln_b", [2, 256])
    sg_wT = din("sg_wT", [2, 128, 4, 128])
    sg_bT = din("sg_bT", [2, 128, 4])
    ca_biasT = din("ca_biasT", [2, 128, 4, 5, 128])
    conv_wT = din("conv_wT", [2, 128, 6, 4])
    conv_bT = din("conv_bT", [2, 128, 6])
    dt_bias = din("ssd_dt_bias", [2, 8]); a_log = din("ssd_a_log", [2, 8])
    ssd_d = din("ssd_d", [2, 8]); ssd_norm_g = din("ssd_norm_g", [2, 512])
    ln1_g = din("ln1_g", [2, 1024]); ln1_b = din("ln1_b", [2, 1024])
    if "34" in subph:
        ffn_wg = din("ffn_w_gate", [1, 1024, D_FF]); ffn_wu = din("ffn_w_up", [1, 1024, D_FF])
        ffn_wd = din("ffn_w_down", [1, D_FF, 1024])
        moe_r = din("moe_router", [1, 1024, 8])
        moe_wg = din("moe_w_gate", [1, 8, 1024, D_FFE]); moe_wu = din("moe_w_up", [1, 8, 1024, D_FFE])
        moe_wd = din("moe_w_down", [1, 8, D_FFE, 1024])
    ple_wg = din("ple_w_gate", [2, 1024, 1024]); ple_wp = din("ple_w_proj", [2, 256, 1024])
    ln2_g = din("ln2_g", [2, 1024]); ln2_b = din("ln2_b", [2, 1024])
    c_ident = din("c_ident", [128, 128])
    c_U = din("c_U", [128, 128])
    c_Lm8 = din("c_Lm8", [128, 128])
    c_maskd = din("c_maskd", [128, 4, 512])
    c_identf = c_ident

    out = nc.dram_tensor("out", [S, 1024], F32, kind="ExternalOutput").ap()

    xT = dscr("xT", [1024, S], BF16)
    xres = dscr("xres", [S, 1024], F32)
    qkTa = dscr("qkTa", [512, S], BF16)
    qkTc = dscr("qkTc", [512, S], BF16)
    xbcT = dscr("xbcT", [768, S], F32)
    v_a = dscr("v_a", [S, 256], BF16)
    v_c = dscr("v_c", [S, 256], BF16)
    zs = dscr("zs", [S, 512], F32)
    dtr = dscr("dtr", [128, NCH * 8], F32)
    ybT = dscr("ybT", [256, S], BF16)
    yaT = dscr("yaT", [256, S], BF16)
    ycT = dscr("ycT", [256, S], BF16)
    ydT = dscr("ydT", [512, S], BF16)
    x1T = dscr("x1T", [1024, S], BF16)
    x1res = dscr("x1res", [S, 1024], F32)
    pT = dscr("pT", [256, S], BF16)
    combs = dscr("combs", [128, NCH * 8], F32)

    st = contextlib.ExitStack()
    with st:
        P = Prog(nc)
        ar = Arena(nc, st, 200 * 1024)
        pb = []
        for i in range(7):
            pb.append((st.enter_context(nc.psum_tensor(f"pb{i}", [128, 512], F32))[:], Res(f"pb{i}", True)))
        ptb_t = st.enter_context(nc.psum_tensor("ptb", [128, 1024], BF16))
        R_ptb = Res("ptb", True)
        ptb = [(ptb_t[:, 0:512], R_ptb), (ptb_t[:, 512:1024], R_ptb)]

        def mm_group(out_ap, pairs, reads, writes):
            def fn(e):
                n = len(pairs)
                ins = None
                for i, (l, r) in enumerate(pairs):
                    ins = e.matmul(out_ap, lhsT=l, rhs=r, start=(i == 0), stop=(i == n - 1))
                return ins
            return P.op("pe", fn, reads, writes)

        ident = ar.alloc([128], BF16); R_const = Res("const")
        U_f = ar.alloc([128], F32)
        U_bf = ar.alloc([128], BF16)
        Lm8 = ar.alloc([128], BF16)
        onesm8 = ar.alloc([128], BF16)
        ones_f = ar.alloc([128], F32)
        epsb = ar.alloc([1], F32)
        P.dma("pool", lambda e: e.dma_start(out=ident, in_=c_ident), writes=[R_const])
        P.dma("sp", lambda e: e.dma_start(out=U_f, in_=c_U), writes=[R_const])
        P.dma("pool", lambda e: e.dma_start(out=U_bf, in_=c_U), writes=[R_const])
        P.dma("pool", lambda e: e.dma_start(out=Lm8, in_=c_Lm8), writes=[R_const])
        P.op("pool", lambda e: e.memset(onesm8, -8.0), writes=[R_const])
        P.op("pool", lambda e: e.memset(ones_f, 1.0), writes=[R_const])
        P.op("pool", lambda e: e.memset(epsb, LN_EPS), writes=[R_const])
        base_top = ar.top
        final_ops = []

        def phase0():
            xb = RR([(ar.alloc([1024], BF16), Res("xb")) for _ in range(2)])
            xs = RR([(ar.alloc([8, 128], BF16), Res("xs")) for _ in range(2)])
            Rx = Res("xT")
            for c in range(NCH):
                b, rb = xb.next()
                P.dma("pool", lambda e, b=b, c=c: e.dma_start(out=b, in_=x[c * 128:(c + 1) * 128, :]), writes=[rb])
                for half in range(2):
                    pt, rpt = ptb[half]
                    P.op("pe", lambda e, b=b, pt=pt, half=half: [e.transpose(out=pt[:, k * 128:(k + 1) * 128], in_=b[:, (half * 4 + k) * 128:(half * 4 + k + 1) * 128], identity=ident) for k in range(4)][-1],
                         reads=[rb, R_const], writes=[rpt])
                    s_, rs = xs.items[xs.i % 2]
                    eng = "dve" if half == 0 else "act"
                    if eng == "dve":
                        P.op("dve", lambda e, s_=s_, pt=pt, half=half: e.tensor_copy(out=s_[:, half * 4:half * 4 + 4, :], in_=pt.rearrange("p (k n) -> p k n", k=4)), reads=[rpt], writes=[rs])
                    else:
                        P.op("act", lambda e, s_=s_, pt=pt, half=half: e.copy(out=s_[:, half * 4:half * 4 + 4, :], in_=pt.rearrange("p (k n) -> p k n", k=4)), reads=[rpt], writes=[rs])
                s_, rs = xs.next()
                P.dma("sp", lambda e, s_=s_, c=c: e.dma_start(out=xT.rearrange("(k p) s -> p k s", p=128)[:, :, c * 128:(c + 1) * 128], in_=s_), reads=[rs], writes=[Rx])

        def phase1(li):
            w1 = ar.alloc([8, 3336], BF16); Rw1 = Res("w1")
            for k in range(8):
                P.dma("pool", lambda e, k=k: e.dma_start(out=w1[:, k, :], in_=w_in[li, k * 128:(k + 1) * 128, 0:3336]), writes=[Rw1])
            lng = ar.alloc([256], F32); lnb = ar.alloc([256], F32)
            sgw = ar.alloc([4, 128], BF16); sgw_f = ar.alloc([4, 128], F32); sgb = ar.alloc([4], F32)
            Rc = Res("p1const")
            P.dma("sp", lambda e: e.dma_start(out=lng, in_=sg_ln_g[li:li + 1, :].partition_broadcast(128)), writes=[Rc])
            P.dma("sp", lambda e: e.dma_start(out=lnb, in_=sg_ln_b[li:li + 1, :].partition_broadcast(128)), writes=[Rc])
            P.dma("sp", lambda e: e.dma_start(out=sgw_f, in_=sg_wT[li]), writes=[Rc])
            P.dma("sp", lambda e: e.dma_start(out=sgb, in_=sg_bT[li]), writes=[Rc])
            P.op("dve", lambda e: e.tensor_tensor(out=sgw, in0=sgw_f, in1=U_f.unsqueeze(1).to_broadcast([128, 4, 128]), op=ALU.mult), reads=[Rc, R_const], writes=[Rc])
            xTt = RR([(ar.alloc([8, 512], BF16), Res("xTt")) for _ in range(2)])
            stA = RR([(ar.alloc([4, 512], BF16), Res("stA")) for _ in range(2)])
            stC = RR([(ar.alloc([4, 512], BF16), Res("stC")) for _ in range(2)])
            stD = RR([(ar.alloc([6, 512], F32), Res("stD")) for _ in range(2)])
            stV = RR([(ar.alloc([512], BF16), Res("stV")) for _ in range(2)])
            stZ = RR([(ar.alloc([512], F32), Res("stZ")) for _ in range(2)])
            stDt = RR([(ar.alloc([8], F32), Res("stDt")) for _ in range(2)])
            uvg = RR([(ar.alloc([512], F32), Res("uvg")) for _ in range(2)])
            stat = RR([(ar.alloc([8], F32), Res("stat")) for _ in range(2)])
            rstd = RR([(ar.alloc([2], F32), Res("rstd")) for _ in range(2)])
            vn = RR([(ar.alloc([256], F32), Res("vn")) for _ in range(2)])
            vnb = RR([(ar.alloc([256], BF16), Res("vnb")) for _ in range(2)])
            ybt = RR([(ar.alloc([256], BF16), Res("ybt")) for _ in range(2)])
            stB = RR([(ar.alloc([2, 512], BF16), Res("stB")) for _ in range(2)])
            fmps = RR([pb[0], pb[1]])
            R_scr = {n: Res(n) for n in ["qkTa", "qkTc", "xbcT", "v_a", "v_c", "zs", "dtr", "ybT"]}
            RxT = Res("xT_r")
            evi = [0]

            def evac(out_ap, in_ap, reads, writes, func=None):
                if func is not None:
                    return P.op("act", lambda e: e.activation(out=out_ap, in_=in_ap, func=func), reads, writes)
                evi[0] += 1
                if evi[0] % 2:
                    return P.op("dve", lambda e: e.tensor_copy(out=out_ap, in_=in_ap), reads, writes)
                return P.op("act", lambda e: e.copy(out=out_ap, in_=in_ap), reads, writes)

            for t in range(NT):
                xt, rxt = xTt.next()
                tok = slice(t * 512, (t + 1) * 512)
                P.dma("sp", lambda e, xt=xt, tok=tok: e.dma_start(out=xt, in_=xT.rearrange("(k p) s -> p k s", p=128)[:, :, tok]), writes=[rxt])
                for (col0, nchk, stg, dst, rname) in ((0, 4, stA, qkTa, "qkTa"), (1280, 4, stC, qkTc, "qkTc"), (2560, 6, stD, xbcT, "xbcT")):
                    sg_, rsg = stg.next()
                    for j in range(nchk):
                        ps, rps = fmps.next()
                        c0 = col0 + j * 128
                        mm_group(ps, [(w1[:, k, c0:c0 + 128], xt[:, k, :]) for k in range(8)], [Rw1, rxt], [rps])
                        evac(sg_[:, j, :], ps, [rps], [rsg])
                    P.dma("sp", lambda e, sg_=sg_, dst=dst, tok=tok: e.dma_start(out=dst.rearrange("(k p) s -> p k s", p=128)[:, :, tok], in_=sg_), reads=[rsg], writes=[R_scr[rname]])
                sB, rsB = stB.next()
                for cc in range(4):
                    rows = slice(t * 512 + cc * 128, t * 512 + (cc + 1) * 128)
                    lh = [xt[:, k, cc * 128:(cc + 1) * 128] for k in range(8)]
                    psV, rV = pb[2]; psB, rB = pb[3]; psZ, rZ = pb[4]; psM, rM = pb[5]; psD, rD = pb[6]
                    mm_group(psV[:, 0:256], [(lh[k], w1[:, k, 512:768]) for k in range(8)], [Rw1, rxt], [rV])
                    mm_group(psV[:, 256:512], [(lh[k], w1[:, k, 1792:2048]) for k in range(8)], [Rw1, rxt], [rV])
                    mm_group(psB, [(lh[k], w1[:, k, 768:1280]) for k in range(8)], [Rw1, rxt], [rB])
                    mm_group(psZ, [(lh[k], w1[:, k, 2048:2560]) for k in range(8)], [Rw1, rxt], [rZ])
                    mm_group(psD[:, 0:8], [(lh[k], w1[:, k, 3328:3336]) for k in range(8)], [Rw1, rxt], [rD])
                    sv, rsv = stV.next()
                    evac(sv, psV, [rV], [rsv])
                    P.dma("sp", lambda e, sv=sv, rows=rows: e.dma_start(out=v_a[rows, :], in_=sv[:, 0:256]), reads=[rsv], writes=[R_scr["v_a"]])
                    P.dma("sp", lambda e, sv=sv, rows=rows: e.dma_start(out=v_c[rows, :], in_=sv[:, 256:512]), reads=[rsv], writes=[R_scr["v_c"]])
                    sz, rsz = stZ.next()
                    evac(sz, psZ, [rZ], [rsz], func=AF.Silu)
                    P.dma("sp", lambda e, sz=sz, rows=rows: e.dma_start(out=zs[rows, :], in_=sz), reads=[rsz], writes=[R_scr["zs"]])
                    sd, rsd = stDt.next()
                    P.op("dve", lambda e, sd=sd, psD=psD: e.tensor_copy(out=sd, in_=psD[:, 0:8]), [rD], [rsd])
                    P.dma("sp", lambda e, sd=sd, t=t, cc=cc: e.dma_start(out=dtr[:, (t * 4 + cc) * 8:(t * 4 + cc + 1) * 8], in_=sd), reads=[rsd], writes=[R_scr["dtr"]])
                    ug, rug = uvg.next()
                    P.op("act", lambda e, ug=ug, psB=psB: e.activation(out=ug, in_=psB, func=AF.Gelu_apprx_tanh), [rB], [rug])
                    sta, rsta = stat.next()
                    P.op("dve", lambda e, sta=sta, ug=ug: e.bn_stats(out=sta[:, 0:6], in_=ug[:, 256:512]), [rug], [rsta])
                    P.op("dve", lambda e, sta=sta: e.bn_aggr(out=sta[:, 6:8], in_=sta[:, 0:6]), [rsta], [rsta])
                    rs_, rrs = rstd.next()
                    P.op("act", lambda e, rs_=rs_, sta=sta: e.activation(out=rs_[:, 0:1], in_=sta[:, 7:8], func=AF.Sqrt, bias=epsb, scale=1.0), [rsta, R_const], [rrs])
                    P.op("dve", lambda e, rs_=rs_: e.reciprocal(out=rs_[:, 1:2], in_=rs_[:, 0:1]), [rrs], [rrs])
                    v_, rv_ = vn.next()
                    P.op("dve", lambda e, v_=v_, ug=ug, sta=sta, rs_=rs_: e.tensor_scalar(out=v_, in0=ug[:, 256:512], scalar1=sta[:, 6:7], scalar2=rs_[:, 1:2], op0=ALU.subtract, op1=ALU.mult), [rug, rsta, rrs], [rv_])
                    P.op("pool", lambda e, v_=v_: e.tensor_tensor(out=v_, in0=v_, in1=lng, op=ALU.mult), [rv_, Rc], [rv_])
                    vb, rvb = vnb.next()
                    P.op("pool", lambda e, v_=v_, vb=vb: e.tensor_tensor(out=vb, in0=v_, in1=lnb, op=ALU.add), [rv_, Rc], [rvb])

                    def mixfn(e, vb=vb, psM=psM):
                        ins = None
                        for g in range(4):
                            ins = e.matmul(psM[:, g * 64:(g + 1) * 64], lhsT=sgw[:, g, :], rhs=vb[:, g * 64:(g + 1) * 64], start=True, stop=True)
                        return ins
                    P.op("pe", mixfn, [rvb, Rc], [rM])
                    yb, ryb = ybt.next()

                    def gatefn(e, yb=yb, psM=psM, ug=ug):
                        ins = None
                        for g in range(4):
                            ins = e.scalar_tensor_tensor(out=yb[:, g * 64:(g + 1) * 64], in0=psM[:, g * 64:(g + 1) * 64], scalar=sgb[:, g:g + 1], in1=ug[:, g * 64:(g + 1) * 64], op0=ALU.add, op1=ALU.mult)
                        return ins
                    P.op("dve", gatefn, [rM, rug, Rc], [ryb])
                    pt, rpt = ptb[cc % 2]
                    P.op("pe", lambda e, pt=pt, yb=yb: [e.transpose(out=pt[:, k * 128:(k + 1) * 128], in_=yb[:, k * 128:(k + 1) * 128], identity=ident) for k in range(2)][-1], [ryb, R_const], [rpt])
                    P.op("act", lambda e, sB=sB, pt=pt, cc=cc: e.copy(out=sB[:, :, cc * 128:(cc + 1) * 128], in_=pt[:, 0:256].rearrange("p (k n) -> p k n", k=2)), [rpt], [rsB])
                P.dma("sp", lambda e, sB=sB, tok=tok: e.dma_start(out=ybT.rearrange("(k p) s -> p k s", p=128)[:, :, tok], in_=sB), reads=[rsB], writes=[R_scr["ybT"]])

        def phaseA():
            qk = ar.alloc([4, S], BF16); Rqk = Res("qkA")
            va = ar.alloc([NCH, 256], BF16); Rva = Res("vaA")
            maskd = ar.alloc([4, 512], BF16); Rm = Res("maskd")
            for k in range(4):
                P.dma("sp", lambda e, k=k: e.dma_start(out=qk[:, k, :], in_=qkTa[k * 128:(k + 1) * 128, :]), writes=[Rqk])
            P.dma("sp", lambda e: e.dma_start(out=va, in_=v_a.rearrange("(c p) f -> p c f", p=128)), writes=[Rva])
            P.dma("pool", lambda e: e.dma_start(out=maskd, in_=c_maskd), writes=[Rm])
            eb = RR([(ar.alloc([512], F32), Res("eb")) for _ in range(2)])
            spb = RR([(ar.alloc([512], BF16), Res("spb")) for _ in range(3)])
            ab = RR([(ar.alloc([512], BF16), Res("ab")) for _ in range(2)])
            racc = ar.alloc([512], F32); Rracc = Res("racc")
            raccb = RR([(ar.alloc([512], BF16), Res("raccb")) for _ in range(2)])
            ost = RR([(ar.alloc([512], BF16), Res("ostA")) for _ in range(2)])
            pz = RR([pb[0], pb[1]])
            pin_ = RR([pb[2], pb[3]])
            pout = RR([pb[4], pb[5]])
            RyaT = Res("yaT")
            for h in range(4):
                pr = slice((h % 2) * 64, (h % 2) * 64 + 64)
                qc_, kc_ = h // 2, 2 + h // 2
                for qt in range(NT):
                    q_ap = qk[pr, qc_, qt * 512:(qt + 1) * 512]
                    po, rpo = pout.next()
                    kbs = list(range(4 * qt + 3, -1, -1))
                    rb_prev = None
                    for n, kb in enumerate(kbs):
                        k_ap = kc = qk[pr, kc_, kb * 128:(kb + 1) * 128]
                        diag = kb - 4 * qt
                        z, rz = pz.next()
                        mm_group(z, [(k_ap, q_ap)], [Rqk], [rz])
                        e_, re_ = eb.next()
                        P.op("act", lambda e, e_=e_, z=z: e.activation(out=e_, in_=z, func=AF.Exp, scale=0.125), [rz], [re_])
                        sp_, rsp = spb.next()
                        P.op("act", lambda e, e_=e_, sp_=sp_: e.activation(out=sp_, in_=e_, func=AF.Ln, bias=1.0, scale=1.0), [re_], [rsp])
                        if diag >= 0:
                            P.op("pool", lambda e, sp_=sp_, diag=diag: e.tensor_tensor(out=sp_, in0=sp_, in1=maskd[:, diag, :], op=ALU.mult), [rsp, Rm], [rsp])
                        pi_, rpi = pin_.next()
                        pairs = [(k_ap, q_ap), (Lm8, sp_)]
                        rd = [Rqk, rsp, R_const]
                        if rb_prev is not None:
                            pairs.append((onesm8, rb_prev[0]))
                            rd.append(rb_prev[1])
                        mm_group(pi_, pairs, rd, [rpi])
                        a_, ra = ab.next()
                        P.op("act", lambda e, a_=a_, pi_=pi_: e.activation(out=a_, in_=pi_, func=AF.Exp, scale=0.125), [rpi], [ra])
                        if diag >= 0:
                            P.op("pool", lambda e, a_=a_, diag=diag: e.tensor_tensor(out=a_, in0=a_, in1=maskd[:, diag, :], op=ALU.mult), [ra, Rm], [ra])
                        v_ap = va[:, kb, h * 64:(h + 1) * 64]
                        P.op("pe", lambda e, po=po, v_ap=v_ap, a_=a_, n=n, last=(n == len(kbs) - 1): e.matmul(po[0:64, :], lhsT=v_ap, rhs=a_, start=(n == 0), stop=last), [Rva, ra], [rpo])
                        if n < len(kbs) - 1:
                            if n == 0:
                                P.op("pool", lambda e, sp_=sp_: e.tensor_copy(out=racc, in_=sp_), [rsp], [Rracc])
                            else:
                                P.op("pool", lambda e, sp_=sp_: e.tensor_tensor(out=racc, in0=racc, in1=sp_, op=ALU.add), [rsp, Rracc], [Rracc])
                            rb_, rrb = raccb.next()
                            P.op("dve", lambda e, rb_=rb_: e.tensor_copy(out=rb_, in_=racc), [Rracc], [rrb])
                            rb_prev = (rb_, rrb)
                    o_, ro = ost.next()
                    P.op("dve", lambda e, o_=o_, po=po: e.tensor_copy(out=o_[0:64, :], in_=po[0:64, :]), [rpo], [ro])
                    P.dma("sp", lambda e, o_=o_, h=h, qt=qt: e.dma_start(out=yaT[h * 64:(h + 1) * 64, qt * 512:(qt + 1) * 512], in_=o_[0:64, :]), reads=[ro], writes=[RyaT])

        def phaseC(li):
            qk = ar.alloc([4, S], BF16); Rqk = Res("qkC")
            v1 = ar.alloc([NCH, 4, 65], BF16); Rv1 = Res("v1C")
            bias = ar.alloc([4, 5, 128], F32); Rb = Res("biasC")
            for k in range(4):
                P.dma("sp", lambda e, k=k: e.dma_start(out=qk[:, k, :], in_=qkTc[k * 128:(k + 1) * 128, :]), writes=[Rqk])
            P.op("pool", lambda e: e.memset(v1, 1.0), writes=[Rv1])
            for hh in range(4):
                P.dma("sp", lambda e, hh=hh: e.dma_start(out=v1[:, :, hh, 0:64], in_=v_c.rearrange("(c p) f -> p c f", p=128)[:, :, hh * 64:(hh + 1) * 64]), writes=[Rv1])
            P.dma("sp", lambda e: e.dma_start(out=bias, in_=ca_biasT[li]), writes=[Rb])
            sbuf_s = RR([(ar.alloc([5, 128], F32), Res("sC")) for _ in range(2)])
            pT = RR([(ar.alloc([5, 128], BF16), Res("pT")) for _ in range(2)])
            rec = RR([(ar.alloc([4], F32), Res("recC")) for _ in range(2)])
            yc = RR([(ar.alloc([4, 64], BF16), Res("ycC")) for _ in range(2)])
            ost = RR([(ar.alloc([2, 128], BF16), Res("ostC")) for _ in range(2)])
            psA = RR([pb[0], pb[2]])
            psB = RR([pb[1], pb[3]])
            psO = RR([pb[4], pb[5]])
            RycT = Res("ycT")
            for qc in range(NCH):
                po, rpo = psO.next()
                os_ = [o for o in range(5) if qc - 4 + o >= 0]
                for h in range(4):
                    pr = slice((h % 2) * 64, (h % 2) * 64 + 64)
                    q_ap = qk[pr, h // 2, qc * 128:(qc + 1) * 128]
                    pa, rpa = psA.next()
                    pb_, rpb = psB.next()

                    def scfn(e, pa=pa, pb_=pb_, q_ap=q_ap, pr=pr, h=h, qc=qc, os_=os_):
                        ins = None
                        for o in os_:
                            kb = qc - 4 + o
                            dst = pa[:, o * 128:(o + 1) * 128] if o < 4 else pb_[:, 0:128]
                            ins = e.matmul(dst, lhsT=qk[pr, 2 + h // 2, kb * 128:(kb + 1) * 128], rhs=q_ap, start=True, stop=True)
                        return ins
                    P.op("pe", scfn, [Rqk], [rpa, rpb])
                    s_, rs = sbuf_s.next()
                    o_lo = [o for o in os_ if o < 4]
                    if o_lo:
                        a0, a1 = o_lo[0], o_lo[-1] + 1
                        P.op("dve", lambda e, s_=s_, pa=pa, h=h, a0=a0, a1=a1: e.scalar_tensor_tensor(out=s_[:, a0:a1, :], in0=pa[:, a0 * 128:a1 * 128].rearrange("p (o n) -> p o n", n=128), scalar=0.125, in1=bias[:, h, a0:a1, :], op0=ALU.mult, op1=ALU.add), [rpa, Rb], [rs])
                    P.op("dve", lambda e, s_=s_, pb_=pb_, h=h: e.scalar_tensor_tensor(out=s_[:, 4, :], in0=pb_[:, 0:128], scalar=0.125, in1=bias[:, h, 4, :], op0=ALU.mult, op1=ALU.add), [rpb, Rb], [rs])
                    p_, rp = pT.next()
                    o0 = os_[0]
                    P.op("act", lambda e, p_=p_, s_=s_, o0=o0: e.activation(out=p_[:, o0:5, :], in_=s_[:, o0:5, :], func=AF.Exp), [rs], [rp])

                    def avfn(e, po=po, p_=p_, h=h, qc=qc, os_=os_):
                        ins = None
                        for n, o in enumerate(os_):
                            kb = qc - 4 + o
                            ins = e.matmul(po[:, h * 65:(h + 1) * 65], lhsT=p_[:, o, :], rhs=v1[:, kb, h, :], start=(n == 0), stop=(n == len(os_) - 1))
                        return ins
                    P.op("pe", avfn, [rp, Rv1], [rpo])
                r_, rr = rec.next()
                pov = po[:, 0:260].rearrange("p (h d) -> p h d", h=4)
                P.op("dve", lambda e, r_=r_, pov=pov: e.reciprocal(out=r_, in_=pov[:, :, 64]), [rpo], [rr])
                y_, ry = yc.next()
                P.op("dve", lambda e, y_=y_, pov=pov, r_=r_: e.tensor_tensor(out=y_, in0=pov[:, :, 0:64], in1=r_.unsqueeze(2).to_broadcast([128, 4, 64]), op=ALU.mult), [rpo, rr], [ry])
                pt, rpt = ptb[qc % 2]
                P.op("pe", lambda e, pt=pt, y_=y_: [e.transpose(out=pt[:, k * 128:(k + 1) * 128], in_=y_[:, 2 * k:2 * k + 2, :].rearrange("p a b -> p (a b)"), identity=ident) for k in range(2)][-1], [ry, R_const], [rpt])
                o_, ro = ost.next()
                P.op("act", lambda e, o_=o_, pt=pt: e.copy(out=o_, in_=pt[:, 0:256].rearrange("p (k n) -> p k n", k=2)), [rpt], [ro])
                P.dma("sp", lambda e, o_=o_, qc=qc: e.dma_start(out=ycT.rearrange("(k p) s -> p k s", p=128)[:, :, qc * 128:(qc + 1) * 128], in_=o_), reads=[ro], writes=[RycT])

        def phaseD(li):
            cw = ar.alloc([6, 4], F32); cb = ar.alloc([6], F32); Rc = Res("dconst")
            dtb = ar.alloc([8], F32); alog = ar.alloc([8], F32); dsk = ar.alloc([8], F32)
            ng = ar.alloc([512], F32)
            P.dma("sp", lambda e: e.dma_start(out=cw, in_=conv_wT[li]), writes=[Rc])
            P.dma("sp", lambda e: e.dma_start(out=cb, in_=conv_bT[li]), writes=[Rc])
            P.dma("sp", lambda e: e.dma_start(out=dtb, in_=dt_bias[li:li + 1, :].partition_broadcast(128)), writes=[Rc])
            P.dma("sp", lambda e: e.dma_start(out=alog, in_=a_log[li:li + 1, :].partition_broadcast(128)), writes=[Rc])
            P.dma("sp", lambda e: e.dma_start(out=dsk, in_=ssd_d[li:li + 1, :].partition_broadcast(128)), writes=[Rc])
            P.dma("sp", lambda e: e.dma_start(out=ng, in_=ssd_norm_g[li:li + 1, :].partition_broadcast(128)), writes=[Rc])
            P.op("act", lambda e: e.activation(out=alog, in_=alog, func=AF.Exp), [Rc], [Rc])
            P.op("dve", lambda e: e.tensor_scalar(out=alog, in0=alog, scalar1=-1.0, scalar2=None, op0=ALU.mult), [Rc], [Rc])
            dta = ar.alloc([NCH, 8], F32); dtA = ar.alloc([NCH, 8], F32); Rdt = Res("dt")
            P.dma("sp", lambda e: e.dma_start(out=dta, in_=dtr.rearrange("p (c h) -> p c h", h=8)), writes=[Rdt])
            P.op("dve", lambda e: e.tensor_tensor(out=dta, in0=dta, in1=dtb.unsqueeze(1).to_broadcast([128, NCH, 8]), op=ALU.add), [Rdt, Rc], [Rdt])
            P.op("act", lambda e: e.activation(out=dta, in_=dta, func=AF.Exp), [Rdt], [Rdt])
            P.op("act", lambda e: e.activation(out=dta, in_=dta, func=AF.Ln, bias=1.0, scale=1.0), [Rdt], [Rdt])
            P.op("dve", lambda e: e.tensor_tensor(out=dtA, in0=dta, in1=alog.unsqueeze(1).to_broadcast([128, NCH, 8]), op=ALU.mult), [Rdt, Rc], [Rdt])
            cvo = ar.alloc([6, S], BF16); Rcv = Res("cvo")
            SEG = min(S, 2048)
            xin = RR([(ar.alloc([SEG + 3], F32), Res("xin")) for _ in range(2)])
            acc = RR([(ar.alloc([SEG], F32), Res("acc")) for _ in range(2)])
            for fc in range(6):
                for sg in range(S // SEG):
                    xi, rxi = xin.next()
                    if sg == 0:
                        P.op("pool", lambda e, xi=xi: e.memset(xi[:, 0:3], 0.0), writes=[rxi])
                        P.dma("sp", lambda e, xi=xi, fc=fc: e.dma_start(out=xi[:, 3:SEG + 3], in_=xbcT[fc * 128:(fc + 1) * 128, 0:SEG]), writes=[rxi])
                    else:
                        P.dma("sp", lambda e, xi=xi, fc=fc, sg=sg: e.dma_start(out=xi, in_=xbcT[fc * 128:(fc + 1) * 128, sg * SEG - 3:(sg + 1) * SEG]), writes=[rxi])
                    ac, rac = acc.next()
                    P.op("dve", lambda e, ac=ac, xi=xi, fc=fc: e.tensor_scalar(out=ac, in0=xi[:, 0:SEG], scalar1=cw[:, fc, 0:1], scalar2=cb[:, fc:fc + 1], op0=ALU.mult, op1=ALU.add), [rxi, Rc], [rac])
                    for k in (1, 2, 3):
                        eng = "dve"
                        P.op(eng, lambda e, ac=ac, xi=xi, fc=fc, k=k: e.scalar_tensor_tensor(out=ac, in0=xi[:, k:SEG + k], scalar=cw[:, fc, k:k + 1], in1=ac, op0=ALU.mult, op1=ALU.add), [rxi, Rc, rac], [rac])
                    P.op("act", lambda e, ac=ac, fc=fc, sg=sg: e.activation(out=cvo[:, fc, sg * SEG:(sg + 1) * SEG], in_=ac, func=AF.Silu), [rac], [Rcv])
            hst = ar.alloc([8, 64], F32); hsb = ar.alloc([8, 64], BF16); Rh = Res("hst"); Rhb = Res("hsb")
            P.op("pool", lambda e: e.memset(hst, 0.0), writes=[Rh])
            P.op("pool", lambda e: e.memset(hsb, 0.0), writes=[Rhb])
            xs_tm = ar.alloc([8, 64], F32); Rxs = Res("xs_tm")
            b_tm = ar.alloc([128], BF16); Rbt = Res("b_tm")
            rhsM = ar.alloc([8, 128], F32); RrM = Res("rhsM")
            acsc = ar.alloc([8], F32); Racs = Res("acsc")
            eacs = ar.alloc([8], F32); Reacs = Res("eacs")
            cd = ar.alloc([8], F32); Rcd = Res("cd")
            E = ar.alloc([8, 128], F32); RE = Res("E")
            cbm = ar.alloc([2, 128], F32); Rcbm = Res("cbm")
            MT = ar.alloc([8, 128], BF16); RMT = Res("MT")
            xdt = ar.alloc([8, 64], BF16); Rxdt = Res("xdt")
            xdtd = ar.alloc([8, 64], BF16); Rxdtd = Res("xdtd")
            t1 = ar.alloc([8, 64], F32); Rt1 = Res("t1")
            t2 = ar.alloc([8, 64], F32); Rt2 = Res("t2")
            htmp = ar.alloc([8, 64], F32); Rht = Res("htmp")
            zsb = RR([(ar.alloc([512], F32), Res("zsb")) for _ in range(2)])
            junk = ar.alloc([256], F32); Rj = Res("junkD")
            ssq = ar.alloc([4], F32); Rssq = Res("ssq")
            yd = ar.alloc([512], BF16); Ryd = Res("yd")
            ost = RR([(ar.alloc([4, 128], BF16), Res("ostD")) for _ in range(2)])
            RydT = Res("ydT")
            p_acs, r_acs = pb[0]; p_r0, r_r0 = pb[1]; p_r1, r_r1 = pb[2]; p_cb, r_cb = pb[3]
            p_yd, r_yd = pb[4]; p_yo, r_yo = pb[5]; p_st, r_st = pb[6]
            ptw = ptb_t[:]
            import os as _os
            DST = int(_os.environ.get("DSTOP", "9"))
            for c in range(NCH):
                if DST < 2:
                    break
                cs = slice(c * 128, (c + 1) * 128)
                zb, rzb = zsb.next()
                P.dma("sp", lambda e, zb=zb, cs=cs: e.dma_start(out=zb, in_=zs[cs, :]), writes=[rzb])
                P.op("pe", lambda e, cs=cs: [e.transpose(out=ptw[:, k * 128:(k + 1) * 128], in_=cvo[:, k, cs], identity=ident) for k in range(5)][-1], [Rcv, R_const], [R_ptb])
                P.op("act", lambda e: e.copy(out=xs_tm.rearrange("p h d -> p (h d)"), in_=ptw[:, 0:512]), [R_ptb], [Rxs])
                P.op("dve", lambda e: e.tensor_copy(out=b_tm, in_=ptw[:, 512:640]), [R_ptb], [Rbt])
                P.op("pe", lambda e, c=c: e.matmul(p_acs[:, 0:8], lhsT=U_f, rhs=dtA[:, c, :], start=True, stop=True), [Rdt, R_const], [r_acs])
                P.op("pool", lambda e, c=c: e.tensor_tensor(out=rhsM, in0=U_f.unsqueeze(1).to_broadcast([128, 8, 128]), in1=dtA[:, c, :].unsqueeze(2).to_broadcast([128, 8, 128]), op=ALU.mult), [Rdt, R_const], [RrM])
                P.op("pe", lambda e: e.matmul(p_r0, lhsT=ones_f, rhs=rhsM[:, 0:4, :].rearrange("p a b -> p (a b)"), start=True, stop=True), [RrM, R_const], [r_r0])
                P.op("pe", lambda e: e.matmul(p_r1, lhsT=ones_f, rhs=rhsM[:, 4:8, :].rearrange("p a b -> p (a b)"), start=True, stop=True), [RrM, R_const], [r_r1])
                P.op("dve", lambda e: e.tensor_copy(out=acsc, in_=p_acs[:, 0:8]), [r_acs], [Racs])
                P.op("act", lambda e: e.activation(out=eacs, in_=p_acs[:, 0:8], func=AF.Exp), [r_acs], [Reacs])
                for j, (pr_, rr_) in enumerate(((p_r0, r_r0), (p_r1, r_r1))):
                    P.op("dve", lambda e, pr_=pr_, j=j: e.tensor_tensor(out=E[:, 4 * j:4 * j + 4, :], in0=pr_.rearrange("p (a b) -> p a b", a=4), in1=acsc[:, 4 * j:4 * j + 4].unsqueeze(2).to_broadcast([128, 4, 128]), op=ALU.subtract), [rr_, Racs], [RE])
                    P.op("act", lambda e, pr_=pr_, j=j: e.activation(out=cd[:, 4 * j:4 * j + 4], in_=pr_.rearrange("p (a b) -> p a b", a=4)[:, :, 127], func=AF.Exp), [rr_], [Rcd])
                P.op("act", lambda e: e.activation(out=E, in_=E, func=AF.Exp), [RE], [RE])
                if DST < 3:
                    continue
                cb_dst = ((p_cb, r_cb), (p_r1, r_r1))
                for g in range(2):
                    pc_, rc_ = cb_dst[g]
                    P.op("pe", lambda e, g=g, pc_=pc_, cs=cs: e.matmul(pc_[:, 0:128], lhsT=cvo[g * 64:(g + 1) * 64, 4, cs], rhs=cvo[g * 64:(g + 1) * 64, 5, cs], start=True, stop=True), [Rcv], [rc_])
                    P.op("dve", lambda e, g=g, pc_=pc_: e.tensor_tensor(out=cbm[:, g, :], in0=pc_[:, 0:128], in1=U_f, op=ALU.mult), [rc_, R_const], [Rcbm])
                for g in range(2):
                    eng = "dve"
                    P.op(eng, lambda e, g=g: e.scalar_tensor_tensor(out=MT[:, 4 * g:4 * g + 4, :], in0=E[:, 4 * g:4 * g + 4, :], scalar=1.0, in1=cbm[:, g, :].unsqueeze(1).to_broadcast([128, 4, 128]), op0=ALU.min, op1=ALU.mult), [RE, Rcbm], [RMT])
                P.op("pool", lambda e, c=c: e.tensor_tensor(out=xdt, in0=xs_tm, in1=dta[:, c, :].unsqueeze(2).to_broadcast([128, 8, 64]), op=ALU.mult), [Rxs, Rdt], [Rxdt])
                P.op("dve", lambda e: e.tensor_tensor(out=xdtd, in0=xdt, in1=E[:, :, 127].unsqueeze(2).to_broadcast([128, 8, 64]), op=ALU.mult), [Rxdt, RE], [Rxdtd])
                def ydfn(e):
                    ins = None
                    for h in range(8):
                        ins = e.matmul(p_yd[:, h * 64:(h + 1) * 64], lhsT=MT[:, h, :], rhs=xdt[:, h, :], start=True, stop=True)
                    return ins
                P.op("pe", ydfn, [RMT, Rxdt], [r_yd])
                yo_dst = ((p_yo, r_yo), (p_acs, r_acs))
                for g in range(2):
                    py_, ry_ = yo_dst[g]
                    P.op("pe", lambda e, g=g, py_=py_, cs=cs: e.matmul(py_[:, 0:256], lhsT=cvo[g * 64:(g + 1) * 64, 5, cs], rhs=hsb[g * 64:(g + 1) * 64, 4 * g:4 * g + 4, :].rearrange("p a b -> p (a b)"), start=True, stop=True), [Rcv, Rhb], [ry_])
                P.op("pe", lambda e: e.matmul(p_st, lhsT=b_tm, rhs=xdtd.rearrange("p a b -> p (a b)"), start=True, stop=True), [Rbt, Rxdtd], [r_st])
                P.op("pool", lambda e: e.tensor_tensor(out=htmp, in0=hst, in1=cd.unsqueeze(2).to_broadcast([128, 8, 64]), op=ALU.mult), [Rh, Rcd], [Rht])
                P.op("dve", lambda e: e.tensor_tensor(out=hst, in0=htmp, in1=p_st.rearrange("p (a b) -> p a b", a=8), op=ALU.add), [Rht, r_st], [Rh])
                P.op("act", lambda e: e.copy(out=hsb, in_=hst), [Rh], [Rhb])
                if DST < 4:
                    continue
                for g in range(2):
                    py_, ry_ = yo_dst[g]
                    P.op("dve", lambda e, g=g, py_=py_: e.tensor_tensor(out=t1[:, 4 * g:4 * g + 4, :], in0=py_[:, 0:256].rearrange("p (a b) -> p a b", a=4), in1=eacs[:, 4 * g:4 * g + 4].unsqueeze(2).to_broadcast([128, 4, 64]), op=ALU.mult), [ry_, Reacs], [Rt1])
                P.op("dve", lambda e: e.tensor_tensor(out=t1, in0=t1, in1=p_yd.rearrange("p (a b) -> p a b", a=8), op=ALU.add), [Rt1, r_yd], [Rt1])
                P.op("pool", lambda e: e.tensor_tensor(out=t2, in0=xs_tm, in1=dsk.unsqueeze(2).to_broadcast([128, 8, 64]), op=ALU.mult), [Rxs, Rc], [Rt2])
                P.op("pool", lambda e: e.tensor_tensor(out=t1, in0=t1, in1=t2, op=ALU.add), [Rt1, Rt2], [Rt1])
                P.op("pool", lambda e, zb=zb: e.tensor_tensor(out=t1.rearrange("p a b -> p (a b)"), in0=t1.rearrange("p a b -> p (a b)"), in1=zb, op=ALU.mult), [Rt1, rzb], [Rt1])
                t1f = t1.rearrange("p a b -> p (a b)")
                for g in range(2):
                    P.op("act", lambda e, g=g: e.activation(out=junk, in_=t1f[:, g * 256:(g + 1) * 256], func=AF.Square, accum_out=ssq[:, g:g + 1]), [Rt1], [Rj, Rssq])
                P.op("act", lambda e: e.activation(out=ssq[:, 2:4], in_=ssq[:, 0:2], func=AF.Sqrt, bias=epsb, scale=1.0 / 256), [Rssq, R_const], [Rssq])
                P.op("dve", lambda e: e.reciprocal(out=ssq[:, 2:4], in_=ssq[:, 2:4]), [Rssq], [Rssq])
                P.op("dve", lambda e: e.tensor_tensor(out=t2.rearrange("p (g a) b -> p g (a b)", g=2), in0=t1.rearrange("p (g a) b -> p g (a b)", g=2), in1=ssq[:, 2:4].unsqueeze(2).to_broadcast([128, 2, 256]), op=ALU.mult), [Rt1, Rssq], [Rt2])
                P.op("pool", lambda e: e.tensor_tensor(out=yd, in0=t2.rearrange("p a b -> p (a b)"), in1=ng, op=ALU.mult), [Rt2, Rc], [Ryd])
                P.op("pe", lambda e: [e.transpose(out=ptw[:, k * 128:(k + 1) * 128], in_=yd[:, k * 128:(k + 1) * 128], identity=ident) for k in range(4)][-1], [Ryd, R_const], [R_ptb])
                o_, ro = ost.next()
                P.op("act", lambda e, o_=o_: e.copy(out=o_, in_=ptw[:, 0:512].rearrange("p (k n) -> p k n", k=4)), [R_ptb], [ro])
                P.dma("sp", lambda e, o_=o_, cs=cs: e.dma_start(out=ydT.rearrange("(k p) s -> p k s", p=128)[:, :, cs], in_=o_), reads=[ro], writes=[RydT])

        def layernorm(t, rt, g_bc, b_bc, Rgb, out_ap, rout, tmp):
            st_, rst = tmp
            P.op("dve", lambda e: e.bn_stats(out=st_[:, 0:6], in_=t[:, 0:512]), [rt], [rst])
            P.op("dve", lambda e: e.bn_stats(out=st_[:, 6:12], in_=t[:, 512:1024]), [rt], [rst])
            P.op("dve", lambda e: e.bn_aggr(out=st_[:, 12:14], in_=st_[:, 0:12]), [rst], [rst])
            P.op("act", lambda e: e.activation(out=st_[:, 14:15], in_=st_[:, 13:14], func=AF.Sqrt, bias=epsb, scale=1.0), [rst, R_const], [rst])
            P.op("dve", lambda e: e.reciprocal(out=st_[:, 15:16], in_=st_[:, 14:15]), [rst], [rst])
            P.op("dve", lambda e: e.tensor_scalar(out=t, in0=t, scalar1=st_[:, 12:13], scalar2=st_[:, 15:16], op0=ALU.subtract, op1=ALU.mult), [rt, rst], [rt])
            P.op("pool", lambda e: e.tensor_tensor(out=t, in0=t, in1=g_bc, op=ALU.mult), [rt, Rgb], [rt])
            P.op("pool", lambda e: e.tensor_tensor(out=out_ap, in0=t, in1=b_bc, op=ALU.add), [rt, Rgb], [rout])

        def to_xT(src, rsrc, dstT, cols, Rdst, xbst, ostt):
            xb_, rxb = xbst.next()
            P.op("act", lambda e: e.copy(out=xb_, in_=src), [rsrc], [rxb])
            o_, ro = ostt.next()
            for half in range(2):
                pt, rpt = ptb[half]
                P.op("pe", lambda e, pt=pt, half=half: [e.transpose(out=pt[:, k * 128:(k + 1) * 128], in_=xb_[:, (half * 4 + k) * 128:(half * 4 + k + 1) * 128], identity=ident) for k in range(4)][-1], [rxb, R_const], [rpt])
                P.op("dve", lambda e, pt=pt, half=half: e.tensor_copy(out=o_[:, half * 4:half * 4 + 4, :], in_=pt.rearrange("p (k n) -> p k n", k=4)), [rpt], [ro])
            P.dma("sp", lambda e: e.dma_start(out=dstT.rearrange("(k p) s -> p k s", p=128)[:, :, cols], in_=o_), reads=[ro], writes=[Rdst])

        def phaseP(li):
            pbuf = RR([(ar.alloc([256], BF16), Res("pbuf")) for _ in range(2)])
            ost = RR([(ar.alloc([2, 128], BF16), Res("ostP")) for _ in range(2)])
            RpT = Res("pT")
            for c in range(NCH):
                b, rb = pbuf.next()
                P.dma("pool", lambda e, b=b, c=c: e.dma_start(out=b, in_=pin[li, c * 128:(c + 1) * 128, :]), writes=[rb])
                pt, rpt = ptb[c % 2]
                P.op("pe", lambda e, pt=pt, b=b: [e.transpose(out=pt[:, k * 128:(k + 1) * 128], in_=b[:, k * 128:(k + 1) * 128], identity=ident) for k in range(2)][-1], [rb, R_const], [rpt])
                o_, ro = ost.next()
                P.op("dve", lambda e, o_=o_, pt=pt: e.tensor_copy(out=o_, in_=pt[:, 0:256].rearrange("p (k n) -> p k n", k=2)), [rpt], [ro])
                P.dma("sp", lambda e, o_=o_, c=c: e.dma_start(out=pT.rearrange("(k p) s -> p k s", p=128)[:, :, c * 128:(c + 1) * 128], in_=o_), reads=[ro], writes=[RpT])

        def phase3(li):
            moe = (li % 2 == 1)
            xsrc = x if li == 0 else xres
            Rw = Res("w3")
            wg = ar.alloc([8, 4096], BF16)
            for k in range(8):
                P.dma("pool", lambda e, k=k: e.dma_start(out=wg[:, k, :], in_=w_in[li, k * 128:(k + 1) * 128, 3336:7432]), writes=[Rw])
            wb = ar.alloc([10, 1024], BF16)
            kofs = [0, 2, 4, 6]
            for b in range(4):
                nk = 4 if b == 3 else 2
                for k in range(nk):
                    P.dma("pool", lambda e, b=b, k=k: e.dma_start(out=wb[:, kofs[b] + k, :], in_=w_br[b][li, k * 128:(k + 1) * 128, :]), writes=[Rw])
            wo = ar.alloc([8, 1024], BF16)
            for k in range(8):
                P.dma("pool", lambda e, k=k: e.dma_start(out=wo[:, k, :], in_=w_out[li, k * 128:(k + 1) * 128, :]), writes=[Rw])
            g_bc = ar.alloc([1024], F32); b_bc = ar.alloc([1024], F32); Rgb = Res("ln1gb")
            P.dma("sp", lambda e: e.dma_start(out=g_bc, in_=ln1_g[li:li + 1, :].partition_broadcast(128)), writes=[Rgb])
            P.dma("sp", lambda e: e.dma_start(out=b_bc, in_=ln1_b[li:li + 1, :].partition_broadcast(128)), writes=[Rgb])
            if moe:
                wr = ar.alloc([8, 8], F32); identf = ar.alloc([128], F32)
                P.dma("sp", lambda e: e.dma_start(out=wr, in_=moe_r[0].rearrange("(k p) e -> p k e", p=128)), writes=[Rw])
                P.dma("sp", lambda e: e.dma_start(out=identf, in_=c_identf), writes=[Rw])
                x1Tf = ar.alloc([8, 128], F32); Rx1Tf = Res("x1Tf")
                rt_ = ar.alloc([64], F32); Rrt = Res("router_tmp")
            xTt = RR([(ar.alloc([8, 512], BF16), Res("xTt3")) for _ in range(2)])
            yT = RR([(ar.alloc([10, 512], BF16), Res("yT3")) for _ in range(2)])
            gsb = RR([(ar.alloc([512], F32), Res("gsb")) for _ in range(2)])
            mtmp = RR([(ar.alloc([512], F32), Res("mtmp")) for _ in range(2)])
            macc = ar.alloc([512], F32); Rmacc = Res("macc")
            mT = ar.alloc([8, 512], BF16); RmT = Res("mT")
            xr = RR([(ar.alloc([1024], F32), Res("xr")) for _ in range(1)])
            tln = RR([(ar.alloc([1024], F32), Res("tln")) for _ in range(1)])
            x1b = RR([(ar.alloc([1024], F32), Res("x1b")) for _ in range(2)])
            lntmp = RR([(ar.alloc([16], F32), Res("lntmp")) for _ in range(2)])
            xbst = RR([(ar.alloc([1024], BF16), Res("xbst")) for _ in range(2)])
            ostt = RR([(ar.alloc([8, 128], BF16), Res("ostt")) for _ in range(2)])
            pg = RR([pb[0], pb[1]]); pp = RR([pb[2], pb[3]]); po_ = RR([pb[4], pb[5]])
            Rx1T = Res("x1T"); Rx1r = Res("x1res"); Rcomb = Res("combs")
            ysrc = [(yaT, 2), (ybT, 2), (ycT, 2), (ydT, 4)]
            for t in range(NT):
                tok = slice(t * 512, (t + 1) * 512)
                xt, rxt = xTt.next()
                P.dma("sp", lambda e, xt=xt, tok=tok: e.dma_start(out=xt, in_=xT.rearrange("(k p) s -> p k s", p=128)[:, :, tok]), writes=[rxt])
                yt, ryt = yT.next()
                for b in range(4):
                    src, nk = ysrc[b]
                    P.dma("sp", lambda e, yt=yt, src=src, nk=nk, b=b, tok=tok: e.dma_start(out=yt[:, kofs[b]:kofs[b] + nk, :], in_=src.rearrange("(k p) s -> p k s", p=128)[:, :, tok]), writes=[ryt])
                for fc in range(8):
                    for b in range(4):
                        nk = 4 if b == 3 else 2
                        pg_, rpg = pg.next()
                        c0 = b * 1024 + fc * 128
                        mm_group(pg_, [(wg[:, k, c0:c0 + 128], xt[:, k, :]) for k in range(8)], [Rw, rxt], [rpg])
                        g_, rg = gsb.next()
                        P.op("act", lambda e, g_=g_, pg_=pg_: e.activation(out=g_, in_=pg_, func=AF.Sigmoid), [rpg], [rg])
                        pp_, rpp = pp.next()
                        mm_group(pp_, [(wb[:, kofs[b] + k, fc * 128:(fc + 1) * 128], yt[:, kofs[b] + k, :]) for k in range(nk)], [Rw, ryt], [rpp])
                        if b == 0:
                            P.op("dve", lambda e, g_=g_, pp_=pp_: e.tensor_tensor(out=macc, in0=g_, in1=pp_, op=ALU.mult), [rg, rpp], [Rmacc])
                        else:
                            m_, rm = mtmp.next()
                            P.op("dve", lambda e, g_=g_, pp_=pp_, m_=m_: e.tensor_tensor(out=m_, in0=g_, in1=pp_, op=ALU.mult), [rg, rpp], [rm])
                            if b < 3:
                                P.op("pool", lambda e, m_=m_: e.tensor_tensor(out=macc, in0=macc, in1=m_, op=ALU.add), [Rmacc, rm], [Rmacc])
                            else:
                                P.op("pool", lambda e, m_=m_, fc=fc: e.tensor_tensor(out=mT[:, fc, :], in0=macc, in1=m_, op=ALU.add), [Rmacc, rm], [RmT])
                for cc in range(4):
                    c = t * 4 + cc
                    rows = slice(c * 128, (c + 1) * 128)
                    xr_, rxr = xr.next()
                    P.dma("sp", lambda e, xr_=xr_, rows=rows: e.dma_start(out=xr_, in_=xsrc[rows, :]), writes=[rxr])
                    tl, rtl = tln.next()
                    for half in range(2):
                        po, rpo = po_.next()
                        hs = slice(half * 512, (half + 1) * 512)
                        mm_group(po, [(mT[:, k, cc * 128:(cc + 1) * 128], wo[:, k, hs]) for k in range(8)], [RmT, Rw], [rpo])
                        P.op("dve", lambda e, tl=tl, xr_=xr_, po=po, hs=hs: e.scalar_tensor_tensor(out=tl[:, hs], in0=xr_[:, hs], scalar=ALPHA, in1=po, op0=ALU.mult, op1=ALU.add), [rxr, rpo], [rtl])
                    x1_, rx1 = x1b.next()
                    layernorm(tl, rtl, g_bc, b_bc, Rgb, x1_, rx1, lntmp.next())
                    P.dma("sp", lambda e, x1_=x1_, rows=rows: e.dma_start(out=x1res[rows, :], in_=x1_), reads=[rx1], writes=[Rx1r])
                    to_xT(x1_, rx1, x1T, rows, Rx1T, xbst, ostt)
                    if moe:
                        for half in range(2):
                            pr_, rpr = pb[half]
                            P.op("pe", lambda e, pr_=pr_, half=half, x1_=x1_: [e.transpose(out=pr_[:, k * 128:(k + 1) * 128], in_=x1_[:, (half * 4 + k) * 128:(half * 4 + k + 1) * 128], identity=identf) for k in range(4)][-1], [rx1, Rw], [rpr])
                            P.op("act", lambda e, pr_=pr_, half=half: e.copy(out=x1Tf[:, half * 4:half * 4 + 4, :], in_=pr_.rearrange("p (k n) -> p k n", k=4)), [rpr], [Rx1Tf])
                        pl, rpl = pb[2]
                        mm_group(pl[:, 0:8], [(x1Tf[:, k, :], wr[:, k, :]) for k in range(8)], [Rx1Tf, Rw], [rpl])
                        lg = rt_[:, 0:8]; eq = rt_[:, 8:16]; lg2 = rt_[:, 16:24]; sel = rt_[:, 24:32]; ex = rt_[:, 32:40]
                        m1 = rt_[:, 40:41]; m2 = rt_[:, 41:42]; nm1 = rt_[:, 42:43]; den = rt_[:, 43:44]; cmb = rt_[:, 48:56]
                        P.op("dve", lambda e: e.tensor_copy(out=lg, in_=pl[:, 0:8]), [rpl], [Rrt])
                        P.op("dve", lambda e: e.reduce_max(out=m1, in_=lg, axis=mybir.AxisListType.X), [Rrt], [Rrt])
                        P.op("dve", lambda e: e.tensor_scalar(out=eq, in0=lg, scalar1=m1, scalar2=None, op0=ALU.is_equal), [Rrt], [Rrt])
                        P.op("dve", lambda e: e.scalar_tensor_tensor(out=lg2, in0=eq, scalar=-1e30, in1=lg, op0=ALU.mult, op1=ALU.add), [Rrt], [Rrt])
                        P.op("dve", lambda e: e.reduce_max(out=m2, in_=lg2, axis=mybir.AxisListType.X), [Rrt], [Rrt])
                        P.op("dve", lambda e: e.tensor_scalar(out=sel, in0=lg, scalar1=m2, scalar2=None, op0=ALU.is_ge), [Rrt], [Rrt])
                        P.op("dve", lambda e: e.tensor_scalar(out=nm1, in0=m1, scalar1=-1.0, scalar2=None, op0=ALU.mult), [Rrt], [Rrt])
                        P.op("act", lambda e: e.activation(out=ex, in_=lg, func=AF.Exp, bias=nm1, scale=1.0), [Rrt], [Rrt])
                        P.op("dve", lambda e: e.tensor_tensor(out=ex, in0=ex, in1=sel, op=ALU.mult), [Rrt], [Rrt])
                        P.op("dve", lambda e: e.reduce_sum(out=den, in_=ex, axis=mybir.AxisListType.X), [Rrt], [Rrt])
                        P.op("dve", lambda e: e.reciprocal(out=den, in_=den), [Rrt], [Rrt])
                        P.op("dve", lambda e: e.tensor_scalar(out=cmb, in0=ex, scalar1=den, scalar2=None, op0=ALU.mult), [Rrt], [Rrt])
                        P.dma("sp", lambda e, c=c: e.dma_start(out=combs[:, c * 8:(c + 1) * 8], in_=cmb), reads=[Rrt], writes=[Rcomb])

        def phase4(li):
            moe = (li % 2 == 1)
            last = (li == 1)
            TT = min(S, 1024)
            NCT = TT // 128
            Rw = Res("w4")
            wpg = ar.alloc([8, 1024], BF16); wpp = ar.alloc([2, 1024], BF16)
            for k in range(8):
                P.dma("pool", lambda e, k=k: e.dma_start(out=wpg[:, k, :], in_=ple_wg[li, k * 128:(k + 1) * 128, :]), writes=[Rw])
            for k in range(2):
                P.dma("pool", lambda e, k=k: e.dma_start(out=wpp[:, k, :], in_=ple_wp[li, k * 128:(k + 1) * 128, :]), writes=[Rw])
            g_bc = ar.alloc([1024], F32); b_bc = ar.alloc([1024], F32); Rgb = Res("ln2gb")
            P.dma("sp", lambda e: e.dma_start(out=g_bc, in_=ln2_g[li:li + 1, :].partition_broadcast(128)), writes=[Rgb])
            P.dma("sp", lambda e: e.dma_start(out=b_bc, in_=ln2_b[li:li + 1, :].partition_broadcast(128)), writes=[Rgb])
            acc = ar.alloc([NCT, 1024], F32); Racc = [Res(f"acc{i}") for i in range(NCT)]
            x1t = ar.alloc([8, TT], BF16); Rx1t = Res("x1t")
            pt_ = ar.alloc([2, TT], BF16); Rpt = Res("ptile")
            cmbt = ar.alloc([NCT, 8], F32); Rcm = Res("cmbt")
            wgs = RR([(ar.alloc([8, 512], BF16), Res("wgs")) for _ in range(2)])
            wus = RR([(ar.alloc([8, 512], BF16), Res("wus")) for _ in range(2)])
            wds = RR([(ar.alloc([4, 1024], BF16), Res("wds")) for _ in range(2)])
            actT = RR([(ar.alloc([4, TT], BF16), Res("actT")) for _ in range(2)])
            sgl = RR([(ar.alloc([512], F32), Res("sgl")) for _ in range(2)])
            xr = RR([(ar.alloc([1024], F32), Res("xr4")) for _ in range(2)])
            x2b = RR([(ar.alloc([1024], F32), Res("x2b")) for _ in range(2)])
            lntmp = RR([(ar.alloc([16], F32), Res("lntmp4")) for _ in range(2)])
            xbst = RR([(ar.alloc([1024], BF16), Res("xbst4")) for _ in range(2)])
            ostt = RR([(ar.alloc([8, 128], BF16), Res("ostt4")) for _ in range(2)])
            pgu = RR([(pb[0], pb[1]), (pb[2], pb[3])])
            pdn = RR([pb[4], pb[5]])
            Rout = Res("outdst"); RxTn = Res("xTnext")
            if moe:
                experts = [(moe_wg[0, e_], moe_wu[0, e_], moe_wd[0, e_], D_FFE, e_) for e_ in range(8)]
            else:
                experts = [(ffn_wg[0], ffn_wu[0], ffn_wd[0], D_FF, None)]
            for tt in range(S // TT):
                tok = slice(tt * TT, (tt + 1) * TT)
                P.dma("sp", lambda e, tok=tok: e.dma_start(out=x1t, in_=x1T.rearrange("(k p) s -> p k s", p=128)[:, :, tok]), writes=[Rx1t])
                P.dma("sp", lambda e, tok=tok: e.dma_start(out=pt_, in_=pT.rearrange("(k p) s -> p k s", p=128)[:, :, tok]), writes=[Rpt])
                if moe:
                    P.dma("sp", lambda e, tt=tt: e.dma_start(out=cmbt, in_=combs[:, tt * NCT * 8:(tt + 1) * NCT * 8].rearrange("p (c e) -> p c e", e=8)), writes=[Rcm])
                for cc in range(NCT):
                    cs = slice(cc * 128, (cc + 1) * 128)
                    for half in range(2):
                        hs = slice(half * 512, (half + 1) * 512)
                        (pa, rpa), (pb2, rpb2) = pgu.next()
                        mm_group(pa, [(x1t[:, k, cs], wpg[:, k, hs]) for k in range(8)], [Rx1t, Rw], [rpa])
                        mm_group(pb2, [(pt_[:, k, cs], wpp[:, k, hs]) for k in range(2)], [Rpt, Rw], [rpb2])
                        sg_, rsg = sgl.next()
                        P.op("act", lambda e, sg_=sg_, pa=pa: e.activation(out=sg_, in_=pa, func=AF.Sigmoid), [rpa], [rsg])
                        P.op("dve", lambda e, sg_=sg_, pb2=pb2, cc=cc, hs=hs: e.tensor_tensor(out=acc[:, cc, hs], in0=sg_, in1=pb2, op=ALU.mult), [rsg, rpb2], [Racc[cc]])
                for (wg_d, wu_d, wd_d, dff, eidx) in experts:
                    f0 = 0
                    while f0 < dff:
                        fw = min(512, dff - f0)
                        nfc = fw // 128
                        wg_, rwg = wgs.next(); wu_, rwu = wus.next(); wd_, rwd = wds.next()
                        for k in range(8):
                            P.dma("pool", lambda e, wg_=wg_, wg_d=wg_d, k=k, f0=f0, fw=fw: e.dma_start(out=wg_[:, k, 0:fw], in_=wg_d[k * 128:(k + 1) * 128, f0:f0 + fw]), writes=[rwg])
                            P.dma("pool", lambda e, wu_=wu_, wu_d=wu_d, k=k, f0=f0, fw=fw: e.dma_start(out=wu_[:, k, 0:fw], in_=wu_d[k * 128:(k + 1) * 128, f0:f0 + fw]), writes=[rwu])
                        for fc in range(nfc):
                            P.dma("pool", lambda e, wd_=wd_, wd_d=wd_d, fc=fc, f0=f0: e.dma_start(out=wd_[:, fc, :], in_=wd_d[f0 + fc * 128:f0 + (fc + 1) * 128, :]), writes=[rwd])
                        at, rat = actT.next()
                        for ts in range(TT // 512):
                            tsl = slice(ts * 512, (ts + 1) * 512)
                            for fc in range(nfc):
                                (pgt, rpgt), (put, rput) = pgu.next()
                                fs = slice(fc * 128, (fc + 1) * 128)
                                mm_group(pgt, [(wg_[:, k, fs], x1t[:, k, tsl]) for k in range(8)], [rwg, Rx1t], [rpgt])
                                mm_group(put, [(wu_[:, k, fs], x1t[:, k, tsl]) for k in range(8)], [rwu, Rx1t], [rput])
                                sg_, rsg = sgl.next()
                                P.op("act", lambda e, sg_=sg_, pgt=pgt: e.activation(out=sg_, in_=pgt, func=AF.Silu), [rpgt], [rsg])
                                P.op("dve", lambda e, sg_=sg_, put=put, at=at, fc=fc, tsl=tsl: e.tensor_tensor(out=at[:, fc, tsl], in0=sg_, in1=put, op=ALU.mult), [rsg, rput], [rat])
                        for cc in range(NCT):
                            cs = slice(cc * 128, (cc + 1) * 128)
                            for half in range(2):
                                hs = slice(half * 512, (half + 1) * 512)
                                pd, rpd = pdn.next()
                                mm_group(pd, [(at[:, fc, cs], wd_[:, fc, hs]) for fc in range(nfc)], [rat, rwd], [rpd])
                                if eidx is None:
                                    P.op("dve", lambda e, pd=pd, cc=cc, hs=hs: e.tensor_tensor(out=acc[:, cc, hs], in0=acc[:, cc, hs], in1=pd, op=ALU.add), [rpd, Racc[cc]], [Racc[cc]])
                                else:
                                    P.op("dve", lambda e, pd=pd, cc=cc, hs=hs, eidx=eidx: e.scalar_tensor_tensor(out=acc[:, cc, hs], in0=pd, scalar=cmbt[:, cc, eidx:eidx + 1], in1=acc[:, cc, hs], op0=ALU.mult, op1=ALU.add), [rpd, Racc[cc], Rcm], [Racc[cc]])
                        f0 += fw
                for cc in range(NCT):
                    c = tt * NCT + cc
                    rows = slice(c * 128, (c + 1) * 128)
                    xr_, rxr = xr.next()
                    P.dma("sp", lambda e, xr_=xr_, rows=rows: e.dma_start(out=xr_, in_=x1res[rows, :]), writes=[rxr])
                    P.op("dve", lambda e, xr_=xr_, cc=cc: e.scalar_tensor_tensor(out=acc[:, cc, :], in0=xr_, scalar=ALPHA, in1=acc[:, cc, :], op0=ALU.mult, op1=ALU.add), [rxr, Racc[cc]], [Racc[cc]])
                    x2_, rx2 = x2b.next()
                    layernorm(acc[:, cc, :], Racc[cc], g_bc, b_bc, Rgb, x2_, rx2, lntmp.next())
                    dst = out if last else xres
                    o = P.dma("sp", lambda e, x2_=x2_, rows=rows, dst=dst: e.dma_start(out=dst[rows, :], in_=x2_), reads=[rx2], writes=[Rout])
                    if not last:
                        to_xT(x2_, rx2, xT, rows, RxTn, xbst, ostt)

        if "p0" in phases:
            phase0()
            P.barrier()
            ar.top = base_top
        for li in (0, 1):
            if f"l{li}" not in phases:
                continue
            if "1" in subph:
                phase1(li)
                P.barrier()
                ar.top = base_top
            if "A" in subph:
                phaseA()
                P.barrier()
                ar.top = base_top
            if "C" in subph:
                phaseC(li)
                P.barrier()
                ar.top = base_top
            if "D" in subph:
                phaseD(li)
                P.barrier()
                ar.top = base_top
            if "34" in subph:
                phaseP(li)
                P.barrier()
                ar.top = base_top
                phase3(li)
                P.barrier()
                ar.top = base_top
                phase4(li)
                P.barrier()
                ar.top = base_top

        P.barrier()
        lasts = list(P.dma_last.values())
        P.emit(final_ops=lasts)
    P.inputs = I
    return nc, P


def host_consts():
    r = np.arange(128)
    U = (r[:, None] <= r[None, :]).astype(np.float32)
    Lm8 = -8.0 * (r[:, None] >= r[None, :]).astype(np.float32)
    t = np.arange(512)
    maskd = np.zeros((128, 4, 512), np.float32)
    for i in range(4):
        maskd[:, i, :] = (r[:, None] + 128 * i < t[None, :])
    return {"c_ident": np.eye(128, dtype=np.float32), "c_U": U, "c_Lm8": Lm8, "c_maskd": maskd}


def host_layout(inputs, b, S):
    f = lambda a: np.ascontiguousarray(np.asarray(a, dtype=np.float32))
    m = {}
    m["x"] = f(inputs["x"][b, :S])
    m["p"] = f(inputs["p"][:, b, :S])
    for k in ["w_in", "w_br_a", "w_br_b", "w_br_c", "w_br_d", "w_out", "sg_ln_g", "sg_ln_b",
              "ssd_dt_bias", "ssd_a_log", "ssd_d", "ssd_norm_g", "ln1_g", "ln1_b",
              "ffn_w_gate", "ffn_w_up", "ffn_w_down", "moe_router", "moe_w_gate", "moe_w_up",
              "moe_w_down", "ple_w_gate", "ple_w_proj", "ln2_g", "ln2_b"]:
        m[k] = f(inputs[k])
    m["sg_wT"] = f(np.transpose(np.asarray(inputs["sg_w"]), (0, 3, 1, 2)))
    m["sg_bT"] = f(np.transpose(np.asarray(inputs["sg_b"]), (0, 2, 1)))
    rb = np.asarray(inputs["ca_rel_bias"], dtype=np.float32)
    j = np.arange(128)[:, None, None]
    o = np.arange(5)[None, :, None]
    i = np.arange(128)[None, None, :]
    tq = 512 + i
    sk = o * 128 + j
    rel = tq - sk
    idx = np.clip(rel, -63, 256) + 63
    diff = tq // 64 - sk // 64
    ok = (diff >= 0) & (diff <= 8)
    g = rb[:, :, idx]
    g = np.where(ok[None, None], g, np.float32(NEG))
    m["ca_biasT"] = f(np.transpose(g, (0, 2, 1, 3, 4)))
    cw = np.asarray(inputs["ssd_conv_w"], dtype=np.float32)
    m["conv_wT"] = f(np.transpose(cw.reshape(2, 4, 6, 128), (0, 3, 2, 1)))
    cb = np.asarray(inputs["ssd_conv_b"], dtype=np.float32)
    m["conv_bT"] = f(np.transpose(cb.reshape(2, 6, 128), (0, 2, 1)))
    m.update(host_consts())
    return m


def kernel(**inputs):
    S = 8192
    nc, _ = build(S)
    in_maps = [host_layout(inputs, b, S) for b in range(8)]
    res = run_bass_kernel_spmd(nc, in_maps, core_ids=list(range(8)))
    return np.stack([np.asarray(r["out"], dtype=np.float32) for r in res.results], axis=0)
```

```python
import contextlib
import numpy as np
import concourse.bass as bass
import concourse.mybir as mybir
from concourse.bass_utils import run_bass_kernel_spmd

F32 = mybir.dt.float32
BF16 = mybir.dt.bfloat16
AF = mybir.ActivationFunctionType
ALU = mybir.AluOpType

NDMA_SEM = 14
ALPHA = 4 ** 0.25
LN_EPS = 1e-5
D_FF = 2816
D_FFE = 3584
NEG = -30000.0


class Res:
    __slots__ = ("name", "w", "r", "excl")

    def __init__(self, name, excl=False):
        self.name = name
        self.w = None
        self.r = []
        self.excl = excl


class Op:
    __slots__ = ("eng", "fn", "deps", "signal", "val", "semkey", "isdma")

    def __init__(self, eng, fn):
        self.eng = eng
        self.fn = fn
        self.deps = []
        self.signal = False
        self.val = None
        self.semkey = None
        self.isdma = False


class Prog:
    def __init__(self, nc):
        self.nc = nc
        self.ops = {e: [] for e in ("pe", "act", "dve", "pool", "sp")}
        self.dma_rr = {"sp": 0, "pool": 0}
        self.dma_last = {}

    def _deps(self, op, reads, writes):
        ex = [r for r in reads if r.excl]
        if ex:
            reads = [r for r in reads if not r.excl]
            writes = list(writes) + ex
        deps = []
        for r in reads:
            if r.w is not None:
                deps.append(r.w)
        for r in writes:
            if r.w is not None:
                deps.append(r.w)
            deps.extend(r.r)
        for r in reads:
            r.r.append(op)
        for r in writes:
            r.w = op
            r.r = []
        seen = set()
        out = []
        for d in deps:
            if id(d) not in seen and d is not op:
                seen.add(id(d))
                out.append(d)
        return out

    def op(self, eng, fn, reads=(), writes=()):
        o = Op(eng, fn)
        o.semkey = eng
        o.deps = self._deps(o, reads, writes)
        if eng == "pe":
            o.deps = [d for d in o.deps if not (d.eng == "pe" and not d.isdma)]
        self.ops[eng].append(o)
        return o

    def dma(self, queue, fn, reads=(), writes=()):
        o = Op(queue, fn)
        o.isdma = True
        slot = self.dma_rr[queue]
        self.dma_rr[queue] = (slot + 1) % NDMA_SEM
        o.semkey = (queue, slot)
        o.deps = self._deps(o, reads, writes)
        prev = self.dma_last.get(o.semkey)
        if prev is not None:
            o.deps.append(prev)
        self.dma_last[o.semkey] = o
        o.signal = True
        self.ops[queue].append(o)
        return o

    def barrier(self):
        lasts = []
        for e, lst in self.ops.items():
            if lst:
                lasts.append(lst[-1])
        for k, o in self.dma_last.items():
            lasts.append(o)
        for e in ("pe", "act", "dve", "pool", "sp"):
            o = Op(e, lambda eng: eng.nop())
            o.semkey = e
            o.deps = [d for d in lasts if d.eng != e or d.isdma]
            self.ops[e].append(o)

    def emit(self, final_ops=()):
        nc = self.nc
        for e, lst in self.ops.items():
            for o in lst:
                for d in o.deps:
                    d.signal = True
        for o in final_ops:
            o.signal = True
        cnt = {}
        for e, lst in self.ops.items():
            for o in lst:
                if o.signal:
                    inc = 16 if o.isdma else 1
                    cnt[o.semkey] = cnt.get(o.semkey, 0) + inc
                    o.val = cnt[o.semkey]
        self.maxcnt = dict(cnt)
        with contextlib.ExitStack() as st:
            sems = {}
            for k in cnt:
                nm = k if isinstance(k, str) else f"{k[0]}{k[1]}"
                sems[k] = st.enter_context(nc.semaphore("s_" + nm))
            block = st.enter_context(nc.Block())
            engmap = {"pe": block.tensor, "act": block.scalar, "dve": block.vector,
                      "pool": block.gpsimd, "sp": block.sync}

            def make(e):
                lst = self.ops[e]

                def body(eng):
                    known = {}
                    for o in lst:
                        need = {}
                        for d in o.deps:
                            if d.val > known.get(d.semkey, 0):
                                need[d.semkey] = max(need.get(d.semkey, 0), d.val)
                        for k, v in need.items():
                            eng.wait_ge(sems[k], v)
                            known[k] = v
                        ins = o.fn(eng)
                        if o.signal:
                            ins.then_inc(sems[o.semkey], 16 if o.isdma else 1)
                    if e == "sp":
                        for o in final_ops:
                            if o.val > known.get(o.semkey, 0):
                                eng.wait_ge(sems[o.semkey], o.val)
                                known[o.semkey] = o.val
                return body

            for e in ("pe", "act", "dve", "pool", "sp"):
                engmap[e](make(e))


class Arena:
    def __init__(self, nc, st, nbytes):
        self.n4 = nbytes // 4
        self.t = st.enter_context(nc.sbuf_tensor("arena", [128, self.n4], F32))
        self.top = 0

    def alloc(self, shape, dt):
        esz = 2 if dt == BF16 else 4
        n = 1
        for s in shape:
            n *= s
        nb = (n * esz + 63) // 64 * 64
        off4 = self.top // 4
        self.top += nb
        assert self.top // 4 <= self.n4, ("arena overflow", self.top)
        v = self.t[:, off4:off4 + nb // 4]
        if dt != F32:
            v = v.bitcast(dt)
        v = v[:, 0:n]
        if len(shape) == 2:
            v = v.rearrange("p (a b) -> p a b", a=shape[0])
        elif len(shape) == 3:
            v = v.rearrange("p (a b c) -> p a b c", a=shape[0], b=shape[1])
        return v


class RR:
    def __init__(self, items):
        self.items = items
        self.i = 0

    def next(self):
        it = self.items[self.i % len(self.items)]
        self.i += 1
        return it


def build(S, dbg=(), phases=("p0", "l0", "l1"), subph=("1", "A", "C", "D", "34")):
    nc = bass.Bass("TRN2", target_bir_lowering=False)
    NT = S // 512
    NCH = S // 128
    I = {}

    def din(name, shape, dt=F32):
        I[name] = nc.dram_tensor(name, list(shape), dt, kind="ExternalInput").ap()
        return I[name]

    def dscr(name, shape, dt):
        kind = "ExternalOutput" if name in dbg else "Internal"
        return nc.dram_tensor(name, list(shape), dt, kind=kind).ap()

    x = din("x", [S, 1024])
    pin = din("p", [2, S, 256])
    w_in = din("w_in", [2, 1024, 7432])
    w_br = [din("w_br_a", [2, 256, 1024]), din("w_br_b", [2, 256, 1024]),
            din("w_br_c", [2, 256, 1024]), din("w_br_d", [2, 512, 1024])]
    w_out = din("w_out", [2, 1024, 1024])
    sg_ln_g = din("sg_ln_g", [2, 256]); sg_ln_b = din("sg_ln_b", [2, 256])
    sg_wT = din("sg_wT", [2, 128, 4, 128])
    sg_bT = din("sg_bT", [2, 128, 4])
    ca_biasT = din("ca_biasT", [2, 128, 4, 5, 128])
    conv_wT = din("conv_wT", [2, 128, 6, 4])
    conv_bT = din("conv_bT", [2, 128, 6])
    dt_bias = din("ssd_dt_bias", [2, 8]); a_log = din("ssd_a_log", [2, 8])
    ssd_d = din("ssd_d", [2, 8]); ssd_norm_g = din("ssd_norm_g", [2, 512])
    ln1_g = din("ln1_g", [2, 1024]); ln1_b = din("ln1_b", [2, 1024])
    if "34" in subph:
        ffn_wg = din("ffn_w_gate", [1, 1024, D_FF]); ffn_wu = din("ffn_w_up", [1, 1024, D_FF])
        ffn_wd = din("ffn_w_down", [1, D_FF, 1024])
        moe_r = din("moe_router", [1, 1024, 8])
        moe_wg = din("moe_w_gate", [1, 8, 1024, D_FFE]); moe_wu = din("moe_w_up", [1, 8, 1024, D_FFE])
        moe_wd = din("moe_w_down", [1, 8, D_FFE, 1024])
    ple_wg = din("ple_w_gate", [2, 1024, 1024]); ple_wp = din("ple_w_proj", [2, 256, 1024])
    ln2_g = din("ln2_g", [2, 1024]); ln2_b = din("ln2_b", [2, 1024])
    c_ident = din("c_ident", [128, 128])
    c_U = din("c_U", [128, 128])
    c_Lm8 = din("c_Lm8", [128, 128])
    c_maskd = din("c_maskd", [128, 4, 512])
    c_identf = c_ident

    out = nc.dram_tensor("out", [S, 1024], F32, kind="ExternalOutput").ap()

    xT = dscr("xT", [1024, S], BF16)
    xres = dscr("xres", [S, 1024], F32)
    qkTa = dscr("qkTa", [512, S], BF16)
    qkTc = dscr("qkTc", [512, S], BF16)
    xbcT = dscr("xbcT", [768, S], F32)
    v_a = dscr("v_a", [S, 256], BF16)
    v_c = dscr("v_c", [S, 256], BF16)
    zs = dscr("zs", [S, 512], F32)
    dtr = dscr("dtr", [128, NCH * 8], F32)
    ybT = dscr("ybT", [256, S], BF16)
    yaT = dscr("yaT", [256, S], BF16)
    ycT = dscr("ycT", [256, S], BF16)
    ydT = dscr("ydT", [512, S], BF16)
    x1T = dscr("x1T", [1024, S], BF16)
    x1res = dscr("x1res", [S, 1024], F32)
    pT = dscr("pT", [256, S], BF16)
    combs = dscr("combs", [128, NCH * 8], F32)

    st = contextlib.ExitStack()
    with st:
        P = Prog(nc)
        ar = Arena(nc, st, 200 * 1024)
        pbw = []
        for i in range(3):
            pbw.append((st.enter_context(nc.psum_tensor(f"pbw{i}", [128, 1024], F32))[:], Res(f"pbw{i}", True)))
        pb = []
        for i in range(3):
            for hlf in range(2):
                pb.append((pbw[i][0][:, hlf * 512:(hlf + 1) * 512], Res(f"pb{2 * i + hlf}", True)))
        pb.append((st.enter_context(nc.psum_tensor("pb6", [128, 512], F32))[:], Res("pb6", True)))
        ptb_t = st.enter_context(nc.psum_tensor("ptb", [128, 1024], BF16))
        R_ptb = Res("ptb", True)
        ptb = [(ptb_t[:, 0:512], R_ptb), (ptb_t[:, 512:1024], R_ptb)]

        def mm_group(out_ap, pairs, reads, writes):
            def fn(e):
                n = len(pairs)
                ins = None
                for i, (l, r) in enumerate(pairs):
                    ins = e.matmul(out_ap, lhsT=l, rhs=r, start=(i == 0), stop=(i == n - 1))
                return ins
            return P.op("pe", fn, reads, writes)

        ident = ar.alloc([128], BF16); R_const = Res("const")
        U_f = ar.alloc([128], F32)
        U_bf = ar.alloc([128], BF16)
        Lm8 = ar.alloc([128], BF16)
        onesm8 = ar.alloc([128], BF16)
        ones_f = ar.alloc([128], F32)
        epsb = ar.alloc([1], F32)
        P.dma("pool", lambda e: e.dma_start(out=ident, in_=c_ident), writes=[R_const])
        P.dma("sp", lambda e: e.dma_start(out=U_f, in_=c_U), writes=[R_const])
        P.dma("pool", lambda e: e.dma_start(out=U_bf, in_=c_U), writes=[R_const])
        P.dma("pool", lambda e: e.dma_start(out=Lm8, in_=c_Lm8), writes=[R_const])
        P.op("pool", lambda e: e.memset(onesm8, -8.0), writes=[R_const])
        P.op("pool", lambda e: e.memset(ones_f, 1.0), writes=[R_const])
        P.op("pool", lambda e: e.memset(epsb, LN_EPS), writes=[R_const])
        base_top = ar.top
        final_ops = []

        def phase0():
            xb = RR([(ar.alloc([1024], BF16), Res("xb")) for _ in range(2)])
            xs = RR([(ar.alloc([8, 128], BF16), Res("xs")) for _ in range(2)])
            Rx = Res("xT")
            for c in range(NCH):
                b, rb = xb.next()
                P.dma("pool", lambda e, b=b, c=c: e.dma_start(out=b, in_=x[c * 128:(c + 1) * 128, :]), writes=[rb])
                for half in range(2):
                    pt, rpt = ptb[half]
                    P.op("pe", lambda e, b=b, pt=pt, half=half: [e.transpose(out=pt[:, k * 128:(k + 1) * 128], in_=b[:, (half * 4 + k) * 128:(half * 4 + k + 1) * 128], identity=ident) for k in range(4)][-1],
                         reads=[rb, R_const], writes=[rpt])
                    s_, rs = xs.items[xs.i % 2]
                    eng = "dve" if half == 0 else "act"
                    if eng == "dve":
                        P.op("dve", lambda e, s_=s_, pt=pt, half=half: e.tensor_copy(out=s_[:, half * 4:half * 4 + 4, :], in_=pt.rearrange("p (k n) -> p k n", k=4)), reads=[rpt], writes=[rs])
                    else:
                        P.op("act", lambda e, s_=s_, pt=pt, half=half: e.copy(out=s_[:, half * 4:half * 4 + 4, :], in_=pt.rearrange("p (k n) -> p k n", k=4)), reads=[rpt], writes=[rs])
                s_, rs = xs.next()
                P.dma("sp", lambda e, s_=s_, c=c: e.dma_start(out=xT.rearrange("(k p) s -> p k s", p=128)[:, :, c * 128:(c + 1) * 128], in_=s_), reads=[rs], writes=[Rx])

        def phase1(li):
            w1 = ar.alloc([8, 3336], BF16); Rw1 = Res("w1")
            for k in range(8):
                P.dma("pool", lambda e, k=k: e.dma_start(out=w1[:, k, :], in_=w_in[li, k * 128:(k + 1) * 128, 0:3336]), writes=[Rw1])
            lng = ar.alloc([256], F32); lnb = ar.alloc([256], F32)
            sgw = ar.alloc([4, 128], BF16); sgw_f = ar.alloc([4, 128], F32); sgb = ar.alloc([4], F32)
            Rc = Res("p1const")
            P.dma("sp", lambda e: e.dma_start(out=lng, in_=sg_ln_g[li:li + 1, :].partition_broadcast(128)), writes=[Rc])
            P.dma("sp", lambda e: e.dma_start(out=lnb, in_=sg_ln_b[li:li + 1, :].partition_broadcast(128)), writes=[Rc])
            P.dma("sp", lambda e: e.dma_start(out=sgw_f, in_=sg_wT[li]), writes=[Rc])
            P.dma("sp", lambda e: e.dma_start(out=sgb, in_=sg_bT[li]), writes=[Rc])
            P.op("dve", lambda e: e.tensor_tensor(out=sgw, in0=sgw_f, in1=U_f.unsqueeze(1).to_broadcast([128, 4, 128]), op=ALU.mult), reads=[Rc, R_const], writes=[Rc])
            xTt = RR([(ar.alloc([8, 512], BF16), Res("xTt")) for _ in range(2)])
            stA = RR([(ar.alloc([4, 512], BF16), Res("stA")) for _ in range(2)])
            stC = RR([(ar.alloc([4, 512], BF16), Res("stC")) for _ in range(2)])
            stD = RR([(ar.alloc([6, 512], F32), Res("stD")) for _ in range(2)])
            stV = RR([(ar.alloc([512], BF16), Res("stV")) for _ in range(2)])
            stZ = RR([(ar.alloc([512], F32), Res("stZ")) for _ in range(2)])
            stDt = RR([(ar.alloc([8], F32), Res("stDt")) for _ in range(2)])
            uvg = RR([(ar.alloc([512], F32), Res("uvg")) for _ in range(2)])
            stat = RR([(ar.alloc([8], F32), Res("stat")) for _ in range(2)])
            rstd = RR([(ar.alloc([2], F32), Res("rstd")) for _ in range(2)])
            vn = RR([(ar.alloc([256], F32), Res("vn")) for _ in range(2)])
            vnb = RR([(ar.alloc([256], BF16), Res("vnb")) for _ in range(2)])
            ybt = RR([(ar.alloc([256], BF16), Res("ybt")) for _ in range(2)])
            stB = RR([(ar.alloc([2, 512], BF16), Res("stB")) for _ in range(2)])
            fmps = RR([pb[0], pb[1]])
            R_scr = {n: Res(n) for n in ["qkTa", "qkTc", "xbcT", "v_a", "v_c", "zs", "dtr", "ybT"]}
            RxT = Res("xT_r")
            evi = [0]

            def evac(out_ap, in_ap, reads, writes, func=None):
                if func is not None:
                    return P.op("act", lambda e: e.activation(out=out_ap, in_=in_ap, func=func), reads, writes)
                evi[0] += 1
                if evi[0] % 2:
                    return P.op("dve", lambda e: e.tensor_copy(out=out_ap, in_=in_ap), reads, writes)
                return P.op("act", lambda e: e.copy(out=out_ap, in_=in_ap), reads, writes)

            for t in range(NT):
                xt, rxt = xTt.next()
                tok = slice(t * 512, (t + 1) * 512)
                P.dma("sp", lambda e, xt=xt, tok=tok: e.dma_start(out=xt, in_=xT.rearrange("(k p) s -> p k s", p=128)[:, :, tok]), writes=[rxt])
                for (col0, nchk, stg, dst, rname) in ((0, 4, stA, qkTa, "qkTa"), (1280, 4, stC, qkTc, "qkTc"), (2560, 6, stD, xbcT, "xbcT")):
                    sg_, rsg = stg.next()
                    for j in range(nchk):
                        ps, rps = fmps.next()
                        c0 = col0 + j * 128
                        mm_group(ps, [(w1[:, k, c0:c0 + 128], xt[:, k, :]) for k in range(8)], [Rw1, rxt], [rps])
                        evac(sg_[:, j, :], ps, [rps], [rsg])
                    P.dma("sp", lambda e, sg_=sg_, dst=dst, tok=tok: e.dma_start(out=dst.rearrange("(k p) s -> p k s", p=128)[:, :, tok], in_=sg_), reads=[rsg], writes=[R_scr[rname]])
                sB, rsB = stB.next()
                for cc in range(4):
                    rows = slice(t * 512 + cc * 128, t * 512 + (cc + 1) * 128)
                    lh = [xt[:, k, cc * 128:(cc + 1) * 128] for k in range(8)]
                    psV, rV = pb[2]; psB, rB = pb[3]; psZ, rZ = pb[4]; psM, rM = pb[5]; psD, rD = pb[6]
                    mm_group(psV[:, 0:256], [(lh[k], w1[:, k, 512:768]) for k in range(8)], [Rw1, rxt], [rV])
                    mm_group(psV[:, 256:512], [(lh[k], w1[:, k, 1792:2048]) for k in range(8)], [Rw1, rxt], [rV])
                    mm_group(psB, [(lh[k], w1[:, k, 768:1280]) for k in range(8)], [Rw1, rxt], [rB])
                    mm_group(psZ, [(lh[k], w1[:, k, 2048:2560]) for k in range(8)], [Rw1, rxt], [rZ])
                    mm_group(psD[:, 0:8], [(lh[k], w1[:, k, 3328:3336]) for k in range(8)], [Rw1, rxt], [rD])
                    sv, rsv = stV.next()
                    evac(sv, psV, [rV], [rsv])
                    P.dma("sp", lambda e, sv=sv, rows=rows: e.dma_start(out=v_a[rows, :], in_=sv[:, 0:256]), reads=[rsv], writes=[R_scr["v_a"]])
                    P.dma("sp", lambda e, sv=sv, rows=rows: e.dma_start(out=v_c[rows, :], in_=sv[:, 256:512]), reads=[rsv], writes=[R_scr["v_c"]])
                    sz, rsz = stZ.next()
                    evac(sz, psZ, [rZ], [rsz], func=AF.Silu)
                    P.dma("sp", lambda e, sz=sz, rows=rows: e.dma_start(out=zs[rows, :], in_=sz), reads=[rsz], writes=[R_scr["zs"]])
                    sd, rsd = stDt.next()
                    P.op("dve", lambda e, sd=sd, psD=psD: e.tensor_copy(out=sd, in_=psD[:, 0:8]), [rD], [rsd])
                    P.dma("sp", lambda e, sd=sd, t=t, cc=cc: e.dma_start(out=dtr[:, (t * 4 + cc) * 8:(t * 4 + cc + 1) * 8], in_=sd), reads=[rsd], writes=[R_scr["dtr"]])
                    ug, rug = uvg.next()
                    P.op("act", lambda e, ug=ug, psB=psB: e.activation(out=ug, in_=psB, func=AF.Gelu_apprx_tanh), [rB], [rug])
                    sta, rsta = stat.next()
                    P.op("dve", lambda e, sta=sta, ug=ug: e.bn_stats(out=sta[:, 0:6], in_=ug[:, 256:512]), [rug], [rsta])
                    P.op("dve", lambda e, sta=sta: e.bn_aggr(out=sta[:, 6:8], in_=sta[:, 0:6]), [rsta], [rsta])
                    rs_, rrs = rstd.next()
                    P.op("act", lambda e, rs_=rs_, sta=sta: e.activation(out=rs_[:, 0:1], in_=sta[:, 7:8], func=AF.Sqrt, bias=epsb, scale=1.0), [rsta, R_const], [rrs])
                    P.op("dve", lambda e, rs_=rs_: e.reciprocal(out=rs_[:, 1:2], in_=rs_[:, 0:1]), [rrs], [rrs])
                    v_, rv_ = vn.next()
                    P.op("dve", lambda e, v_=v_, ug=ug, sta=sta, rs_=rs_: e.tensor_scalar(out=v_, in0=ug[:, 256:512], scalar1=sta[:, 6:7], scalar2=rs_[:, 1:2], op0=ALU.subtract, op1=ALU.mult), [rug, rsta, rrs], [rv_])
                    P.op("pool", lambda e, v_=v_: e.tensor_tensor(out=v_, in0=v_, in1=lng, op=ALU.mult), [rv_, Rc], [rv_])
                    vb, rvb = vnb.next()
                    P.op("pool", lambda e, v_=v_, vb=vb: e.tensor_tensor(out=vb, in0=v_, in1=lnb, op=ALU.add), [rv_, Rc], [rvb])

                    def mixfn(e, vb=vb, psM=psM):
                        ins = None
                        for g in range(4):
                            ins = e.matmul(psM[:, g * 64:(g + 1) * 64], lhsT=sgw[:, g, :], rhs=vb[:, g * 64:(g + 1) * 64], start=True, stop=True)
                        return ins
                    P.op("pe", mixfn, [rvb, Rc], [rM])
                    yb, ryb = ybt.next()

                    def gatefn(e, yb=yb, psM=psM, ug=ug):
                        ins = None
                        for g in range(4):
                            ins = e.scalar_tensor_tensor(out=yb[:, g * 64:(g + 1) * 64], in0=psM[:, g * 64:(g + 1) * 64], scalar=sgb[:, g:g + 1], in1=ug[:, g * 64:(g + 1) * 64], op0=ALU.add, op1=ALU.mult)
                        return ins
                    P.op("dve", gatefn, [rM, rug, Rc], [ryb])
                    pt, rpt = ptb[cc % 2]
                    P.op("pe", lambda e, pt=pt, yb=yb: [e.transpose(out=pt[:, k * 128:(k + 1) * 128], in_=yb[:, k * 128:(k + 1) * 128], identity=ident) for k in range(2)][-1], [ryb, R_const], [rpt])
                    P.op("act", lambda e, sB=sB, pt=pt, cc=cc: e.copy(out=sB[:, :, cc * 128:(cc + 1) * 128], in_=pt[:, 0:256].rearrange("p (k n) -> p k n", k=2)), [rpt], [rsB])
                P.dma("sp", lambda e, sB=sB, tok=tok: e.dma_start(out=ybT.rearrange("(k p) s -> p k s", p=128)[:, :, tok], in_=sB), reads=[rsB], writes=[R_scr["ybT"]])

        def phaseA():
            qk = ar.alloc([4, S], BF16); Rqk = Res("qkA")
            va = ar.alloc([NCH, 256], BF16); Rva = Res("vaA")
            maskd = ar.alloc([4, 512], BF16); Rm = Res("maskd")
            for k in range(4):
                P.dma("sp", lambda e, k=k: e.dma_start(out=qk[:, k, :], in_=qkTa[k * 128:(k + 1) * 128, :]), writes=[Rqk])
            P.dma("sp", lambda e: e.dma_start(out=va, in_=v_a.rearrange("(c p) f -> p c f", p=128)), writes=[Rva])
            P.dma("pool", lambda e: e.dma_start(out=maskd, in_=c_maskd), writes=[Rm])
            eb = RR([(ar.alloc([1024], F32), Res("eb")) for _ in range(2)])
            spb = RR([(ar.alloc([1024], BF16), Res("spb")) for _ in range(4)])
            ab = RR([(ar.alloc([1024], BF16), Res("ab")) for _ in range(3)])
            racc = ar.alloc([1024], F32); Rracc = Res("racc")
            raccb = RR([(ar.alloc([1024], BF16), Res("raccb")) for _ in range(3)])
            ost = RR([(ar.alloc([512], BF16), Res("ostA")) for _ in range(4)])
            zsets = RR(pbw)
            pos = [pb[6], (ptb_t[:].bitcast(F32), R_ptb)]
            RyaT = Res("yaT")
            steps = []
            for hp in range(2):
                for qt in range(NT):
                    kbs = list(range(4 * qt + 3, -1, -1))
                    for n, kb in enumerate(kbs):
                        steps.append(dict(hp=hp, qt=qt, n=n, kb=kb, diag=kb - 4 * qt, last=(n == len(kbs) - 1)))
            for i, sd in enumerate(steps):
                sd["rbp"] = None

            def v2(ap):
                return ap.rearrange("p (a b) -> p a b", a=2)

            def S1(i):
                sd = steps[i]
                hp, qt, kb, n, diag = sd["hp"], sd["qt"], sd["kb"], sd["n"], sd["diag"]
                zw, rzw = zsets.next(); sd["z"] = (zw, rzw)

                def zfn(e):
                    ins = None
                    for j in range(2):
                        pr = slice(j * 64, j * 64 + 64)
                        ins = e.matmul(zw[:, j * 512:(j + 1) * 512], lhsT=qk[pr, 2 + hp, kb * 128:(kb + 1) * 128], rhs=qk[pr, hp, qt * 512:(qt + 1) * 512], start=True, stop=True)
                    return ins
                P.op("pe", zfn, [Rqk], [rzw])
                e_, re_ = eb.next()
                P.op("act", lambda e: e.activation(out=e_, in_=zw, func=AF.Exp, scale=0.125), [rzw], [re_])
                sp_, rsp = spb.next(); sd["sp"] = (sp_, rsp)
                P.op("act", lambda e: e.activation(out=sp_, in_=e_, func=AF.Ln, bias=1.0, scale=1.0), [re_], [rsp])
                if diag >= 0:
                    P.op("pool", lambda e: e.tensor_tensor(out=v2(sp_), in0=v2(sp_), in1=maskd[:, diag, :].unsqueeze(1).to_broadcast([128, 2, 512]), op=ALU.mult), [rsp, Rm], [rsp])
                if not sd["last"]:
                    if n == 0:
                        P.op("dve", lambda e: e.tensor_copy(out=racc, in_=sp_), [rsp], [Rracc])
                    else:
                        P.op("dve", lambda e: e.tensor_tensor(out=racc, in0=racc, in1=sp_, op=ALU.add), [rsp, Rracc], [Rracc])
                    rb_, rrb = raccb.next()
                    P.op("dve", lambda e: e.tensor_copy(out=rb_, in_=racc), [Rracc], [rrb])
                    steps[i + 1]["rbp"] = (rb_, rrb)

            def S2(i):
                sd = steps[i]
                diag = sd["diag"]
                zw, rzw = sd["z"]; sp_, rsp = sd["sp"]; rbp = sd["rbp"]

                def accfn(e):
                    ins = None
                    for j in range(2):
                        o = zw[:, j * 512:(j + 1) * 512]
                        ins = e.matmul(o, lhsT=Lm8, rhs=sp_[:, j * 512:(j + 1) * 512], start=False, stop=(rbp is None))
                        if rbp is not None:
                            ins = e.matmul(o, lhsT=onesm8, rhs=rbp[0][:, j * 512:(j + 1) * 512], start=False, stop=True)
                    return ins
                rd = [rsp, R_const] + ([rbp[1]] if rbp is not None else [])
                P.op("pe", accfn, rd, [rzw])
                a_, ra = ab.next(); sd["a"] = (a_, ra)
                P.op("act", lambda e: e.activation(out=a_, in_=zw, func=AF.Exp, scale=0.125), [rzw], [ra])
                if diag >= 0:
                    P.op("pool", lambda e: e.tensor_tensor(out=v2(a_), in0=v2(a_), in1=maskd[:, diag, :].unsqueeze(1).to_broadcast([128, 2, 512]), op=ALU.mult), [ra, Rm], [ra])

            def S3(i):
                sd = steps[i]
                hp, qt, kb, n, last = sd["hp"], sd["qt"], sd["kb"], sd["n"], sd["last"]
                a_, ra = sd["a"]

                def avfn(e):
                    ins = None
                    for j in range(2):
                        h = 2 * hp + j
                        ins = e.matmul(pos[j][0][0:64, :], lhsT=va[:, kb, h * 64:(h + 1) * 64], rhs=a_[:, j * 512:(j + 1) * 512], start=(n == 0), stop=last)
                    return ins
                P.op("pe", avfn, [Rva, ra], [pos[0][1], pos[1][1]])
                if last:
                    for j in range(2):
                        h = 2 * hp + j
                        o_, ro = ost.next()
                        P.op("dve", lambda e, o_=o_, j=j: e.tensor_copy(out=o_[0:64, :], in_=pos[j][0][0:64, :]), [pos[j][1]], [ro])
                        P.dma("sp", lambda e, o_=o_, h=h: e.dma_start(out=yaT[h * 64:(h + 1) * 64, qt * 512:(qt + 1) * 512], in_=o_[0:64, :]), reads=[ro], writes=[RyaT])
                sd.clear()

            NS = len(steps)
            for s in range(NS + 2):
                if s < NS:
                    S1(s)
                if 1 <= s <= NS:
                    S2(s - 1)
                if s >= 2:
                    S3(s - 2)

        def phaseC(li):
            qk = ar.alloc([4, S], BF16); Rqk = Res("qkC")
            v1 = ar.alloc([NCH, 4, 65], BF16); Rv1 = Res("v1C")
            bias = ar.alloc([4, 5, 128], F32); Rb = Res("biasC")
            for k in range(4):
                P.dma("sp", lambda e, k=k: e.dma_start(out=qk[:, k, :], in_=qkTc[k * 128:(k + 1) * 128, :]), writes=[Rqk])
            P.op("pool", lambda e: e.memset(v1, 1.0), writes=[Rv1])
            for hh in range(4):
                P.dma("sp", lambda e, hh=hh: e.dma_start(out=v1[:, :, hh, 0:64], in_=v_c.rearrange("(c p) f -> p c f", p=128)[:, :, hh * 64:(hh + 1) * 64]), writes=[Rv1])
            P.dma("sp", lambda e: e.dma_start(out=bias, in_=ca_biasT[li]), writes=[Rb])
            sbuf_s = RR([(ar.alloc([5, 128], F32), Res("sC")) for _ in range(3)])
            pT = RR([(ar.alloc([5, 128], BF16), Res("pT")) for _ in range(4)])
            rec = RR([(ar.alloc([4], F32), Res("recC")) for _ in range(2)])
            yc = RR([(ar.alloc([4, 64], BF16), Res("ycC")) for _ in range(2)])
            ost = RR([(ar.alloc([2, 128], BF16), Res("ostC")) for _ in range(2)])
            psA = RR([pb[0], pb[2]])
            psB = RR([pb[1], pb[3]])
            psO = RR([pb[4], pb[5]])
            RycT = Res("ycT")
            cur = {}

            def C1(qc, h):
                os_ = [o for o in range(5) if qc - 4 + o >= 0]
                pr = slice((h % 2) * 64, (h % 2) * 64 + 64)
                q_ap = qk[pr, h // 2, qc * 128:(qc + 1) * 128]
                pa, rpa = psA.next()
                pb_, rpb = psB.next()

                def scfn(e):
                    ins = None
                    for o in os_:
                        kb = qc - 4 + o
                        dst = pa[:, o * 128:(o + 1) * 128] if o < 4 else pb_[:, 0:128]
                        ins = e.matmul(dst, lhsT=qk[pr, 2 + h // 2, kb * 128:(kb + 1) * 128], rhs=q_ap, start=True, stop=True)
                    return ins
                P.op("pe", scfn, [Rqk], [rpa, rpb])
                s_, rs = sbuf_s.next()
                o_lo = [o for o in os_ if o < 4]
                if o_lo:
                    a0, a1 = o_lo[0], o_lo[-1] + 1
                    P.op("dve", lambda e: e.scalar_tensor_tensor(out=s_[:, a0:a1, :], in0=pa[:, a0 * 128:a1 * 128].rearrange("p (o n) -> p o n", n=128), scalar=0.125, in1=bias[:, h, a0:a1, :], op0=ALU.mult, op1=ALU.add), [rpa, Rb], [rs])
                P.op("dve", lambda e: e.scalar_tensor_tensor(out=s_[:, 4, :], in0=pb_[:, 0:128], scalar=0.125, in1=bias[:, h, 4, :], op0=ALU.mult, op1=ALU.add), [rpb, Rb], [rs])
                p_, rp = pT.next()
                o0 = os_[0]
                P.op("act", lambda e: e.activation(out=p_[:, o0:5, :], in_=s_[:, o0:5, :], func=AF.Exp), [rs], [rp])
                cur[(qc, h)] = (p_, rp, os_)

            def C2(qc, h):
                p_, rp, os_ = cur.pop((qc, h))
                if h == 0:
                    cur["po"] = psO.next()
                po, rpo = cur["po"]

                def avfn(e):
                    ins = None
                    for n, o in enumerate(os_):
                        kb = qc - 4 + o
                        ins = e.matmul(po[:, h * 65:(h + 1) * 65], lhsT=p_[:, o, :], rhs=v1[:, kb, h, :], start=(n == 0), stop=(n == len(os_) - 1))
                    return ins
                P.op("pe", avfn, [rp, Rv1], [rpo])
                if h < 3:
                    return
                r_, rr = rec.next()
                pov = po[:, 0:260].rearrange("p (h d) -> p h d", h=4)
                P.op("dve", lambda e: e.reciprocal(out=r_, in_=pov[:, :, 64]), [rpo], [rr])
                y_, ry = yc.next()
                P.op("dve", lambda e: e.tensor_tensor(out=y_, in0=pov[:, :, 0:64], in1=r_.unsqueeze(2).to_broadcast([128, 4, 64]), op=ALU.mult), [rpo, rr], [ry])
                pt, rpt = ptb[qc % 2]
                P.op("pe", lambda e: [e.transpose(out=pt[:, k * 128:(k + 1) * 128], in_=y_[:, 2 * k:2 * k + 2, :].rearrange("p a b -> p (a b)"), identity=ident) for k in range(2)][-1], [ry, R_const], [rpt])
                o_, ro = ost.next()
                P.op("act", lambda e: e.copy(out=o_, in_=pt[:, 0:256].rearrange("p (k n) -> p k n", k=2)), [rpt], [ro])
                P.dma("sp", lambda e: e.dma_start(out=ycT.rearrange("(k p) s -> p k s", p=128)[:, :, qc * 128:(qc + 1) * 128], in_=o_), reads=[ro], writes=[RycT])

            units = [(qc, h) for qc in range(NCH) for h in range(4)]
            for u in range(len(units) + 2):
                if u < len(units):
                    C1(*units[u])
                if u >= 2:
                    C2(*units[u - 2])

        def phaseD(li):
            cw = ar.alloc([6, 4], F32); cb = ar.alloc([6], F32); Rc = Res("dconst")
            dtb = ar.alloc([8], F32); alog = ar.alloc([8], F32); dsk = ar.alloc([8], F32)
            ng = ar.alloc([512], F32)
            P.dma("sp", lambda e: e.dma_start(out=cw, in_=conv_wT[li]), writes=[Rc])
            P.dma("sp", lambda e: e.dma_start(out=cb, in_=conv_bT[li]), writes=[Rc])
            P.dma("sp", lambda e: e.dma_start(out=dtb, in_=dt_bias[li:li + 1, :].partition_broadcast(128)), writes=[Rc])
            P.dma("sp", lambda e: e.dma_start(out=alog, in_=a_log[li:li + 1, :].partition_broadcast(128)), writes=[Rc])
            P.dma("sp", lambda e: e.dma_start(out=dsk, in_=ssd_d[li:li + 1, :].partition_broadcast(128)), writes=[Rc])
            P.dma("sp", lambda e: e.dma_start(out=ng, in_=ssd_norm_g[li:li + 1, :].partition_broadcast(128)), writes=[Rc])
            P.op("act", lambda e: e.activation(out=alog, in_=alog, func=AF.Exp), [Rc], [Rc])
            P.op("dve", lambda e: e.tensor_scalar(out=alog, in0=alog, scalar1=-1.0, scalar2=None, op0=ALU.mult), [Rc], [Rc])
            dta = ar.alloc([NCH, 8], F32); dtA = ar.alloc([NCH, 8], F32); Rdt = Res("dt")
            P.dma("sp", lambda e: e.dma_start(out=dta, in_=dtr.rearrange("p (c h) -> p c h", h=8)), writes=[Rdt])
            P.op("dve", lambda e: e.tensor_tensor(out=dta, in0=dta, in1=dtb.unsqueeze(1).to_broadcast([128, NCH, 8]), op=ALU.add), [Rdt, Rc], [Rdt])
            P.op("act", lambda e: e.activation(out=dta, in_=dta, func=AF.Exp), [Rdt], [Rdt])
            P.op("act", lambda e: e.activation(out=dta, in_=dta, func=AF.Ln, bias=1.0, scale=1.0), [Rdt], [Rdt])
            P.op("dve", lambda e: e.tensor_tensor(out=dtA, in0=dta, in1=alog.unsqueeze(1).to_broadcast([128, NCH, 8]), op=ALU.mult), [Rdt, Rc], [Rdt])
            cvo = ar.alloc([6, S], BF16); Rcv = Res("cvo")
            SEG = min(S, 2048)
            xin = RR([(ar.alloc([SEG + 3], F32), Res("xin")) for _ in range(2)])
            acc = RR([(ar.alloc([SEG], F32), Res("acc")) for _ in range(2)])
            for fc in range(6):
                for sg in range(S // SEG):
                    xi, rxi = xin.next()
                    if sg == 0:
                        P.op("pool", lambda e, xi=xi: e.memset(xi[:, 0:3], 0.0), writes=[rxi])
                        P.dma("sp", lambda e, xi=xi, fc=fc: e.dma_start(out=xi[:, 3:SEG + 3], in_=xbcT[fc * 128:(fc + 1) * 128, 0:SEG]), writes=[rxi])
                    else:
                        P.dma("sp", lambda e, xi=xi, fc=fc, sg=sg: e.dma_start(out=xi, in_=xbcT[fc * 128:(fc + 1) * 128, sg * SEG - 3:(sg + 1) * SEG]), writes=[rxi])
                    ac, rac = acc.next()
                    P.op("dve", lambda e, ac=ac, xi=xi, fc=fc: e.tensor_scalar(out=ac, in0=xi[:, 0:SEG], scalar1=cw[:, fc, 0:1], scalar2=cb[:, fc:fc + 1], op0=ALU.mult, op1=ALU.add), [rxi, Rc], [rac])
                    for k in (1, 2, 3):
                        eng = "dve"
                        P.op(eng, lambda e, ac=ac, xi=xi, fc=fc, k=k: e.scalar_tensor_tensor(out=ac, in0=xi[:, k:SEG + k], scalar=cw[:, fc, k:k + 1], in1=ac, op0=ALU.mult, op1=ALU.add), [rxi, Rc, rac], [rac])
                    P.op("act", lambda e, ac=ac, fc=fc, sg=sg: e.activation(out=cvo[:, fc, sg * SEG:(sg + 1) * SEG], in_=ac, func=AF.Silu), [rac], [Rcv])
            hst = ar.alloc([8, 64], F32); hsb = ar.alloc([8, 64], BF16); Rh = Res("hst"); Rhb = Res("hsb")
            P.op("pool", lambda e: e.memset(hst, 0.0), writes=[Rh])
            P.op("pool", lambda e: e.memset(hsb, 0.0), writes=[Rhb])
            xs_tm = ar.alloc([8, 64], F32); Rxs = Res("xs_tm")
            b_tm = ar.alloc([128], BF16); Rbt = Res("b_tm")
            rhsM = ar.alloc([8, 128], F32); RrM = Res("rhsM")
            acsc = ar.alloc([8], F32); Racs = Res("acsc")
            eacs = ar.alloc([8], F32); Reacs = Res("eacs")
            cd = ar.alloc([8], F32); Rcd = Res("cd")
            E = ar.alloc([8, 128], F32); RE = Res("E")
            cbm = ar.alloc([2, 128], F32); Rcbm = Res("cbm")
            MT = ar.alloc([8, 128], BF16); RMT = Res("MT")
            xdt = ar.alloc([8, 64], BF16); Rxdt = Res("xdt")
            xdtd = ar.alloc([8, 64], BF16); Rxdtd = Res("xdtd")
            t1 = ar.alloc([8, 64], F32); Rt1 = Res("t1")
            t2 = ar.alloc([8, 64], F32); Rt2 = Res("t2")
            htmp = ar.alloc([8, 64], F32); Rht = Res("htmp")
            zsb = RR([(ar.alloc([512], F32), Res("zsb")) for _ in range(2)])
            junk = ar.alloc([256], F32); Rj = Res("junkD")
            ssq = ar.alloc([4], F32); Rssq = Res("ssq")
            yd = ar.alloc([512], BF16); Ryd = Res("yd")
            ost = RR([(ar.alloc([4, 128], BF16), Res("ostD")) for _ in range(2)])
            RydT = Res("ydT")
            p_acs, r_acs = pb[0]; p_r0, r_r0 = pb[1]; p_r1, r_r1 = pb[2]; p_cb, r_cb = pb[3]
            p_yd, r_yd = pb[4]; p_yo, r_yo = pb[5]; p_st, r_st = pb[6]
            ptw = ptb_t[:]
            import os as _os
            DST = int(_os.environ.get("DSTOP", "9"))
            for c in range(NCH):
                if DST < 2:
                    break
                cs = slice(c * 128, (c + 1) * 128)
                zb, rzb = zsb.next()
                P.dma("sp", lambda e, zb=zb, cs=cs: e.dma_start(out=zb, in_=zs[cs, :]), writes=[rzb])
                P.op("pe", lambda e, cs=cs: [e.transpose(out=ptw[:, k * 128:(k + 1) * 128], in_=cvo[:, k, cs], identity=ident) for k in range(5)][-1], [Rcv, R_const], [R_ptb])
                P.op("act", lambda e: e.copy(out=xs_tm.rearrange("p h d -> p (h d)"), in_=ptw[:, 0:512]), [R_ptb], [Rxs])
                P.op("dve", lambda e: e.tensor_copy(out=b_tm, in_=ptw[:, 512:640]), [R_ptb], [Rbt])
                P.op("pe", lambda e, c=c: e.matmul(p_acs[:, 0:8], lhsT=U_f, rhs=dtA[:, c, :], start=True, stop=True), [Rdt, R_const], [r_acs])
                P.op("pool", lambda e, c=c: e.tensor_tensor(out=rhsM, in0=U_f.unsqueeze(1).to_broadcast([128, 8, 128]), in1=dtA[:, c, :].unsqueeze(2).to_broadcast([128, 8, 128]), op=ALU.mult), [Rdt, R_const], [RrM])
                P.op("pe", lambda e: e.matmul(p_r0, lhsT=ones_f, rhs=rhsM[:, 0:4, :].rearrange("p a b -> p (a b)"), start=True, stop=True), [RrM, R_const], [r_r0])
                P.op("pe", lambda e: e.matmul(p_r1, lhsT=ones_f, rhs=rhsM[:, 4:8, :].rearrange("p a b -> p (a b)"), start=True, stop=True), [RrM, R_const], [r_r1])
                P.op("dve", lambda e: e.tensor_copy(out=acsc, in_=p_acs[:, 0:8]), [r_acs], [Racs])
                P.op("act", lambda e: e.activation(out=eacs, in_=p_acs[:, 0:8], func=AF.Exp), [r_acs], [Reacs])
                for j, (pr_, rr_) in enumerate(((p_r0, r_r0), (p_r1, r_r1))):
                    P.op("dve", lambda e, pr_=pr_, j=j: e.tensor_tensor(out=E[:, 4 * j:4 * j + 4, :], in0=pr_.rearrange("p (a b) -> p a b", a=4), in1=acsc[:, 4 * j:4 * j + 4].unsqueeze(2).to_broadcast([128, 4, 128]), op=ALU.subtract), [rr_, Racs], [RE])
                    P.op("act", lambda e, pr_=pr_, j=j: e.activation(out=cd[:, 4 * j:4 * j + 4], in_=pr_.rearrange("p (a b) -> p a b", a=4)[:, :, 127], func=AF.Exp), [rr_], [Rcd])
                P.op("act", lambda e: e.activation(out=E, in_=E, func=AF.Exp), [RE], [RE])
                if DST < 3:
                    continue
                cb_dst = ((p_cb, r_cb), (p_r1, r_r1))
                for g in range(2):
                    pc_, rc_ = cb_dst[g]
                    P.op("pe", lambda e, g=g, pc_=pc_, cs=cs: e.matmul(pc_[:, 0:128], lhsT=cvo[g * 64:(g + 1) * 64, 4, cs], rhs=cvo[g * 64:(g + 1) * 64, 5, cs], start=True, stop=True), [Rcv], [rc_])
                    P.op("dve", lambda e, g=g, pc_=pc_: e.tensor_tensor(out=cbm[:, g, :], in0=pc_[:, 0:128], in1=U_f, op=ALU.mult), [rc_, R_const], [Rcbm])
                for g in range(2):
                    eng = "dve"
                    P.op(eng, lambda e, g=g: e.scalar_tensor_tensor(out=MT[:, 4 * g:4 * g + 4, :], in0=E[:, 4 * g:4 * g + 4, :], scalar=1.0, in1=cbm[:, g, :].unsqueeze(1).to_broadcast([128, 4, 128]), op0=ALU.min, op1=ALU.mult), [RE, Rcbm], [RMT])
                P.op("pool", lambda e, c=c: e.tensor_tensor(out=xdt, in0=xs_tm, in1=dta[:, c, :].unsqueeze(2).to_broadcast([128, 8, 64]), op=ALU.mult), [Rxs, Rdt], [Rxdt])
                P.op("dve", lambda e: e.tensor_tensor(out=xdtd, in0=xdt, in1=E[:, :, 127].unsqueeze(2).to_broadcast([128, 8, 64]), op=ALU.mult), [Rxdt, RE], [Rxdtd])
                def ydfn(e):
                    ins = None
                    for h in range(8):
                        ins = e.matmul(p_yd[:, h * 64:(h + 1) * 64], lhsT=MT[:, h, :], rhs=xdt[:, h, :], start=True, stop=True)
                    return ins
                P.op("pe", ydfn, [RMT, Rxdt], [r_yd])
                yo_dst = ((p_yo, r_yo), (p_acs, r_acs))
                for g in range(2):
                    py_, ry_ = yo_dst[g]
                    P.op("pe", lambda e, g=g, py_=py_, cs=cs: e.matmul(py_[:, 0:256], lhsT=cvo[g * 64:(g + 1) * 64, 5, cs], rhs=hsb[g * 64:(g + 1) * 64, 4 * g:4 * g + 4, :].rearrange("p a b -> p (a b)"), start=True, stop=True), [Rcv, Rhb], [ry_])
                P.op("pe", lambda e: e.matmul(p_st, lhsT=b_tm, rhs=xdtd.rearrange("p a b -> p (a b)"), start=True, stop=True), [Rbt, Rxdtd], [r_st])
                P.op("pool", lambda e: e.tensor_tensor(out=htmp, in0=hst, in1=cd.unsqueeze(2).to_broadcast([128, 8, 64]), op=ALU.mult), [Rh, Rcd], [Rht])
                P.op("dve", lambda e: e.tensor_tensor(out=hst, in0=htmp, in1=p_st.rearrange("p (a b) -> p a b", a=8), op=ALU.add), [Rht, r_st], [Rh])
                P.op("act", lambda e: e.copy(out=hsb, in_=hst), [Rh], [Rhb])
                if DST < 4:
                    continue
                for g in range(2):
                    py_, ry_ = yo_dst[g]
                    P.op("dve", lambda e, g=g, py_=py_: e.tensor_tensor(out=t1[:, 4 * g:4 * g + 4, :], in0=py_[:, 0:256].rearrange("p (a b) -> p a b", a=4), in1=eacs[:, 4 * g:4 * g + 4].unsqueeze(2).to_broadcast([128, 4, 64]), op=ALU.mult), [ry_, Reacs], [Rt1])
                P.op("dve", lambda e: e.tensor_tensor(out=t1, in0=t1, in1=p_yd.rearrange("p (a b) -> p a b", a=8), op=ALU.add), [Rt1, r_yd], [Rt1])
                P.op("pool", lambda e: e.tensor_tensor(out=t2, in0=xs_tm, in1=dsk.unsqueeze(2).to_broadcast([128, 8, 64]), op=ALU.mult), [Rxs, Rc], [Rt2])
                P.op("pool", lambda e: e.tensor_tensor(out=t1, in0=t1, in1=t2, op=ALU.add), [Rt1, Rt2], [Rt1])
                P.op("pool", lambda e, zb=zb: e.tensor_tensor(out=t1.rearrange("p a b -> p (a b)"), in0=t1.rearrange("p a b -> p (a b)"), in1=zb, op=ALU.mult), [Rt1, rzb], [Rt1])
                t1f = t1.rearrange("p a b -> p (a b)")
                for g in range(2):
                    P.op("act", lambda e, g=g: e.activation(out=junk, in_=t1f[:, g * 256:(g + 1) * 256], func=AF.Square, accum_out=ssq[:, g:g + 1]), [Rt1], [Rj, Rssq])
                P.op("act", lambda e: e.activation(out=ssq[:, 2:4], in_=ssq[:, 0:2], func=AF.Sqrt, bias=epsb, scale=1.0 / 256), [Rssq, R_const], [Rssq])
                P.op("dve", lambda e: e.reciprocal(out=ssq[:, 2:4], in_=ssq[:, 2:4]), [Rssq], [Rssq])
                P.op("dve", lambda e: e.tensor_tensor(out=t2.rearrange("p (g a) b -> p g (a b)", g=2), in0=t1.rearrange("p (g a) b -> p g (a b)", g=2), in1=ssq[:, 2:4].unsqueeze(2).to_broadcast([128, 2, 256]), op=ALU.mult), [Rt1, Rssq], [Rt2])
                P.op("pool", lambda e: e.tensor_tensor(out=yd, in0=t2.rearrange("p a b -> p (a b)"), in1=ng, op=ALU.mult), [Rt2, Rc], [Ryd])
                P.op("pe", lambda e: [e.transpose(out=ptw[:, k * 128:(k + 1) * 128], in_=yd[:, k * 128:(k + 1) * 128], identity=ident) for k in range(4)][-1], [Ryd, R_const], [R_ptb])
                o_, ro = ost.next()
                P.op("act", lambda e, o_=o_: e.copy(out=o_, in_=ptw[:, 0:512].rearrange("p (k n) -> p k n", k=4)), [R_ptb], [ro])
                P.dma("sp", lambda e, o_=o_, cs=cs: e.dma_start(out=ydT.rearrange("(k p) s -> p k s", p=128)[:, :, cs], in_=o_), reads=[ro], writes=[RydT])

        def layernorm(t, rt, g_bc, b_bc, Rgb, out_ap, rout, tmp):
            st_, rst = tmp
            P.op("dve", lambda e: e.bn_stats(out=st_[:, 0:6], in_=t[:, 0:512]), [rt], [rst])
            P.op("dve", lambda e: e.bn_stats(out=st_[:, 6:12], in_=t[:, 512:1024]), [rt], [rst])
            P.op("dve", lambda e: e.bn_aggr(out=st_[:, 12:14], in_=st_[:, 0:12]), [rst], [rst])
            P.op("act", lambda e: e.activation(out=st_[:, 14:15], in_=st_[:, 13:14], func=AF.Sqrt, bias=epsb, scale=1.0), [rst, R_const], [rst])
            P.op("dve", lambda e: e.reciprocal(out=st_[:, 15:16], in_=st_[:, 14:15]), [rst], [rst])
            P.op("dve", lambda e: e.tensor_scalar(out=t, in0=t, scalar1=st_[:, 12:13], scalar2=st_[:, 15:16], op0=ALU.subtract, op1=ALU.mult), [rt, rst], [rt])
            P.op("pool", lambda e: e.tensor_tensor(out=t, in0=t, in1=g_bc, op=ALU.mult), [rt, Rgb], [rt])
            P.op("pool", lambda e: e.tensor_tensor(out=out_ap, in0=t, in1=b_bc, op=ALU.add), [rt, Rgb], [rout])

        def to_xT(src, rsrc, dstT, cols, Rdst, xbst, ostt):
            xb_, rxb = xbst.next()
            P.op("act", lambda e: e.copy(out=xb_, in_=src), [rsrc], [rxb])
            o_, ro = ostt.next()
            for half in range(2):
                pt, rpt = ptb[half]
                P.op("pe", lambda e, pt=pt, half=half: [e.transpose(out=pt[:, k * 128:(k + 1) * 128], in_=xb_[:, (half * 4 + k) * 128:(half * 4 + k + 1) * 128], identity=ident) for k in range(4)][-1], [rxb, R_const], [rpt])
                P.op("dve", lambda e, pt=pt, half=half: e.tensor_copy(out=o_[:, half * 4:half * 4 + 4, :], in_=pt.rearrange("p (k n) -> p k n", k=4)), [rpt], [ro])
            P.dma("sp", lambda e: e.dma_start(out=dstT.rearrange("(k p) s -> p k s", p=128)[:, :, cols], in_=o_), reads=[ro], writes=[Rdst])

        def phaseP(li):
            pbuf = RR([(ar.alloc([256], BF16), Res("pbuf")) for _ in range(2)])
            ost = RR([(ar.alloc([2, 128], BF16), Res("ostP")) for _ in range(2)])
            RpT = Res("pT")
            for c in range(NCH):
                b, rb = pbuf.next()
                P.dma("pool", lambda e, b=b, c=c: e.dma_start(out=b, in_=pin[li, c * 128:(c + 1) * 128, :]), writes=[rb])
                pt, rpt = ptb[c % 2]
                P.op("pe", lambda e, pt=pt, b=b: [e.transpose(out=pt[:, k * 128:(k + 1) * 128], in_=b[:, k * 128:(k + 1) * 128], identity=ident) for k in range(2)][-1], [rb, R_const], [rpt])
                o_, ro = ost.next()
                P.op("dve", lambda e, o_=o_, pt=pt: e.tensor_copy(out=o_, in_=pt[:, 0:256].rearrange("p (k n) -> p k n", k=2)), [rpt], [ro])
                P.dma("sp", lambda e, o_=o_, c=c: e.dma_start(out=pT.rearrange("(k p) s -> p k s", p=128)[:, :, c * 128:(c + 1) * 128], in_=o_), reads=[ro], writes=[RpT])

        def phase3(li):
            moe = (li % 2 == 1)
            xsrc = x if li == 0 else xres
            Rw = Res("w3")
            wg = ar.alloc([8, 4096], BF16)
            for k in range(8):
                P.dma("pool", lambda e, k=k: e.dma_start(out=wg[:, k, :], in_=w_in[li, k * 128:(k + 1) * 128, 3336:7432]), writes=[Rw])
            wb = ar.alloc([10, 1024], BF16)
            kofs = [0, 2, 4, 6]
            for b in range(4):
                nk = 4 if b == 3 else 2
                for k in range(nk):
                    P.dma("pool", lambda e, b=b, k=k: e.dma_start(out=wb[:, kofs[b] + k, :], in_=w_br[b][li, k * 128:(k + 1) * 128, :]), writes=[Rw])
            wo = ar.alloc([8, 1024], BF16)
            for k in range(8):
                P.dma("pool", lambda e, k=k: e.dma_start(out=wo[:, k, :], in_=w_out[li, k * 128:(k + 1) * 128, :]), writes=[Rw])
            g_bc = ar.alloc([1024], F32); b_bc = ar.alloc([1024], F32); Rgb = Res("ln1gb")
            P.dma("sp", lambda e: e.dma_start(out=g_bc, in_=ln1_g[li:li + 1, :].partition_broadcast(128)), writes=[Rgb])
            P.dma("sp", lambda e: e.dma_start(out=b_bc, in_=ln1_b[li:li + 1, :].partition_broadcast(128)), writes=[Rgb])
            if moe:
                wr = ar.alloc([8, 8], F32); identf = ar.alloc([128], F32)
                P.dma("sp", lambda e: e.dma_start(out=wr, in_=moe_r[0].rearrange("(k p) e -> p k e", p=128)), writes=[Rw])
                P.dma("sp", lambda e: e.dma_start(out=identf, in_=c_identf), writes=[Rw])
                x1Tf = ar.alloc([8, 128], F32); Rx1Tf = Res("x1Tf")
                rt_ = ar.alloc([64], F32); Rrt = Res("router_tmp")
            xTt = RR([(ar.alloc([8, 512], BF16), Res("xTt3")) for _ in range(2)])
            yT = RR([(ar.alloc([10, 512], BF16), Res("yT3")) for _ in range(2)])
            gsb = RR([(ar.alloc([512], F32), Res("gsb")) for _ in range(2)])
            mtmp = RR([(ar.alloc([512], F32), Res("mtmp")) for _ in range(2)])
            macc = ar.alloc([512], F32); Rmacc = Res("macc")
            mT = ar.alloc([8, 512], BF16); RmT = Res("mT")
            xr = RR([(ar.alloc([1024], F32), Res("xr")) for _ in range(1)])
            tln = RR([(ar.alloc([1024], F32), Res("tln")) for _ in range(1)])
            x1b = RR([(ar.alloc([1024], F32), Res("x1b")) for _ in range(2)])
            lntmp = RR([(ar.alloc([16], F32), Res("lntmp")) for _ in range(2)])
            xbst = RR([(ar.alloc([1024], BF16), Res("xbst")) for _ in range(2)])
            ostt = RR([(ar.alloc([8, 128], BF16), Res("ostt")) for _ in range(2)])
            pg = RR([pb[0], pb[1]]); pp = RR([pb[2], pb[3]]); po_ = RR([pb[4], pb[5]])
            Rx1T = Res("x1T"); Rx1r = Res("x1res"); Rcomb = Res("combs")
            ysrc = [(yaT, 2), (ybT, 2), (ycT, 2), (ydT, 4)]
            for t in range(NT):
                tok = slice(t * 512, (t + 1) * 512)
                xt, rxt = xTt.next()
                P.dma("sp", lambda e, xt=xt, tok=tok: e.dma_start(out=xt, in_=xT.rearrange("(k p) s -> p k s", p=128)[:, :, tok]), writes=[rxt])
                yt, ryt = yT.next()
                for b in range(4):
                    src, nk = ysrc[b]
                    P.dma("sp", lambda e, yt=yt, src=src, nk=nk, b=b, tok=tok: e.dma_start(out=yt[:, kofs[b]:kofs[b] + nk, :], in_=src.rearrange("(k p) s -> p k s", p=128)[:, :, tok]), writes=[ryt])
                for fc in range(8):
                    for b in range(4):
                        nk = 4 if b == 3 else 2
                        pg_, rpg = pg.next()
                        c0 = b * 1024 + fc * 128
                        mm_group(pg_, [(wg[:, k, c0:c0 + 128], xt[:, k, :]) for k in range(8)], [Rw, rxt], [rpg])
                        g_, rg = gsb.next()
                        P.op("act", lambda e, g_=g_, pg_=pg_: e.activation(out=g_, in_=pg_, func=AF.Sigmoid), [rpg], [rg])
                        pp_, rpp = pp.next()
                        mm_group(pp_, [(wb[:, kofs[b] + k, fc * 128:(fc + 1) * 128], yt[:, kofs[b] + k, :]) for k in range(nk)], [Rw, ryt], [rpp])
                        if b == 0:
                            P.op("dve", lambda e, g_=g_, pp_=pp_: e.tensor_tensor(out=macc, in0=g_, in1=pp_, op=ALU.mult), [rg, rpp], [Rmacc])
                        else:
                            m_, rm = mtmp.next()
                            P.op("dve", lambda e, g_=g_, pp_=pp_, m_=m_: e.tensor_tensor(out=m_, in0=g_, in1=pp_, op=ALU.mult), [rg, rpp], [rm])
                            if b < 3:
                                P.op("pool", lambda e, m_=m_: e.tensor_tensor(out=macc, in0=macc, in1=m_, op=ALU.add), [Rmacc, rm], [Rmacc])
                            else:
                                P.op("pool", lambda e, m_=m_, fc=fc: e.tensor_tensor(out=mT[:, fc, :], in0=macc, in1=m_, op=ALU.add), [Rmacc, rm], [RmT])
                for cc in range(4):
                    c = t * 4 + cc
                    rows = slice(c * 128, (c + 1) * 128)
                    xr_, rxr = xr.next()
                    P.dma("sp", lambda e, xr_=xr_, rows=rows: e.dma_start(out=xr_, in_=xsrc[rows, :]), writes=[rxr])
                    tl, rtl = tln.next()
                    for half in range(2):
                        po, rpo = po_.next()
                        hs = slice(half * 512, (half + 1) * 512)
                        mm_group(po, [(mT[:, k, cc * 128:(cc + 1) * 128], wo[:, k, hs]) for k in range(8)], [RmT, Rw], [rpo])
                        P.op("dve", lambda e, tl=tl, xr_=xr_, po=po, hs=hs: e.scalar_tensor_tensor(out=tl[:, hs], in0=xr_[:, hs], scalar=ALPHA, in1=po, op0=ALU.mult, op1=ALU.add), [rxr, rpo], [rtl])
                    x1_, rx1 = x1b.next()
                    layernorm(tl, rtl, g_bc, b_bc, Rgb, x1_, rx1, lntmp.next())
                    P.dma("sp", lambda e, x1_=x1_, rows=rows: e.dma_start(out=x1res[rows, :], in_=x1_), reads=[rx1], writes=[Rx1r])
                    to_xT(x1_, rx1, x1T, rows, Rx1T, xbst, ostt)
                    if moe:
                        for half in range(2):
                            pr_, rpr = pb[half]
                            P.op("pe", lambda e, pr_=pr_, half=half, x1_=x1_: [e.transpose(out=pr_[:, k * 128:(k + 1) * 128], in_=x1_[:, (half * 4 + k) * 128:(half * 4 + k + 1) * 128], identity=identf) for k in range(4)][-1], [rx1, Rw], [rpr])
                            P.op("act", lambda e, pr_=pr_, half=half: e.copy(out=x1Tf[:, half * 4:half * 4 + 4, :], in_=pr_.rearrange("p (k n) -> p k n", k=4)), [rpr], [Rx1Tf])
                        pl, rpl = pb[2]
                        mm_group(pl[:, 0:8], [(x1Tf[:, k, :], wr[:, k, :]) for k in range(8)], [Rx1Tf, Rw], [rpl])
                        lg = rt_[:, 0:8]; eq = rt_[:, 8:16]; lg2 = rt_[:, 16:24]; sel = rt_[:, 24:32]; ex = rt_[:, 32:40]
                        m1 = rt_[:, 40:41]; m2 = rt_[:, 41:42]; nm1 = rt_[:, 42:43]; den = rt_[:, 43:44]; cmb = rt_[:, 48:56]
                        P.op("dve", lambda e: e.tensor_copy(out=lg, in_=pl[:, 0:8]), [rpl], [Rrt])
                        P.op("dve", lambda e: e.reduce_max(out=m1, in_=lg, axis=mybir.AxisListType.X), [Rrt], [Rrt])
                        P.op("dve", lambda e: e.tensor_scalar(out=eq, in0=lg, scalar1=m1, scalar2=None, op0=ALU.is_equal), [Rrt], [Rrt])
                        P.op("dve", lambda e: e.scalar_tensor_tensor(out=lg2, in0=eq, scalar=-1e30, in1=lg, op0=ALU.mult, op1=ALU.add), [Rrt], [Rrt])
                        P.op("dve", lambda e: e.reduce_max(out=m2, in_=lg2, axis=mybir.AxisListType.X), [Rrt], [Rrt])
                        P.op("dve", lambda e: e.tensor_scalar(out=sel, in0=lg, scalar1=m2, scalar2=None, op0=ALU.is_ge), [Rrt], [Rrt])
                        P.op("dve", lambda e: e.tensor_scalar(out=nm1, in0=m1, scalar1=-1.0, scalar2=None, op0=ALU.mult), [Rrt], [Rrt])
                        P.op("act", lambda e: e.activation(out=ex, in_=lg, func=AF.Exp, bias=nm1, scale=1.0), [Rrt], [Rrt])
                        P.op("dve", lambda e: e.tensor_tensor(out=ex, in0=ex, in1=sel, op=ALU.mult), [Rrt], [Rrt])
                        P.op("dve", lambda e: e.reduce_sum(out=den, in_=ex, axis=mybir.AxisListType.X), [Rrt], [Rrt])
                        P.op("dve", lambda e: e.reciprocal(out=den, in_=den), [Rrt], [Rrt])
                        P.op("dve", lambda e: e.tensor_scalar(out=cmb, in0=ex, scalar1=den, scalar2=None, op0=ALU.mult), [Rrt], [Rrt])
                        P.dma("sp", lambda e, c=c: e.dma_start(out=combs[:, c * 8:(c + 1) * 8], in_=cmb), reads=[Rrt], writes=[Rcomb])

        def phase4(li):
            moe = (li % 2 == 1)
            last = (li == 1)
            TT = min(S, 1024)
            NCT = TT // 128
            Rw = Res("w4")
            wpg = ar.alloc([8, 1024], BF16); wpp = ar.alloc([2, 1024], BF16)
            for k in range(8):
                P.dma("pool", lambda e, k=k: e.dma_start(out=wpg[:, k, :], in_=ple_wg[li, k * 128:(k + 1) * 128, :]), writes=[Rw])
            for k in range(2):
                P.dma("pool", lambda e, k=k: e.dma_start(out=wpp[:, k, :], in_=ple_wp[li, k * 128:(k + 1) * 128, :]), writes=[Rw])
            g_bc = ar.alloc([1024], F32); b_bc = ar.alloc([1024], F32); Rgb = Res("ln2gb")
            P.dma("sp", lambda e: e.dma_start(out=g_bc, in_=ln2_g[li:li + 1, :].partition_broadcast(128)), writes=[Rgb])
            P.dma("sp", lambda e: e.dma_start(out=b_bc, in_=ln2_b[li:li + 1, :].partition_broadcast(128)), writes=[Rgb])
            acc = ar.alloc([NCT, 1024], F32); Racc = [Res(f"acc{i}") for i in range(NCT)]
            x1t = ar.alloc([8, TT], BF16); Rx1t = Res("x1t")
            pt_ = ar.alloc([2, TT], BF16); Rpt = Res("ptile")
            cmbt = ar.alloc([NCT, 8], F32); Rcm = Res("cmbt")
            wgs = RR([(ar.alloc([8, 512], BF16), Res("wgs")) for _ in range(2)])
            wus = RR([(ar.alloc([8, 512], BF16), Res("wus")) for _ in range(2)])
            wds = RR([(ar.alloc([4, 1024], BF16), Res("wds")) for _ in range(2)])
            actT = RR([(ar.alloc([4, TT], BF16), Res("actT")) for _ in range(2)])
            sgl = RR([(ar.alloc([512], F32), Res("sgl")) for _ in range(2)])
            xr = RR([(ar.alloc([1024], F32), Res("xr4")) for _ in range(2)])
            x2b = RR([(ar.alloc([1024], F32), Res("x2b")) for _ in range(2)])
            lntmp = RR([(ar.alloc([16], F32), Res("lntmp4")) for _ in range(2)])
            xbst = RR([(ar.alloc([1024], BF16), Res("xbst4")) for _ in range(2)])
            ostt = RR([(ar.alloc([8, 128], BF16), Res("ostt4")) for _ in range(2)])
            pgu = RR([(pb[0], pb[1]), (pb[2], pb[3])])
            pdn = RR([pb[4], pb[5]])
            Rout = Res("outdst"); RxTn = Res("xTnext")
            if moe:
                experts = [(moe_wg[0, e_], moe_wu[0, e_], moe_wd[0, e_], D_FFE, e_) for e_ in range(8)]
            else:
                experts = [(ffn_wg[0], ffn_wu[0], ffn_wd[0], D_FF, None)]
            for tt in range(S // TT):
                tok = slice(tt * TT, (tt + 1) * TT)
                P.dma("sp", lambda e, tok=tok: e.dma_start(out=x1t, in_=x1T.rearrange("(k p) s -> p k s", p=128)[:, :, tok]), writes=[Rx1t])
                P.dma("sp", lambda e, tok=tok: e.dma_start(out=pt_, in_=pT.rearrange("(k p) s -> p k s", p=128)[:, :, tok]), writes=[Rpt])
                if moe:
                    P.dma("sp", lambda e, tt=tt: e.dma_start(out=cmbt, in_=combs[:, tt * NCT * 8:(tt + 1) * NCT * 8].rearrange("p (c e) -> p c e", e=8)), writes=[Rcm])
                for cc in range(NCT):
                    cs = slice(cc * 128, (cc + 1) * 128)
                    for half in range(2):
                        hs = slice(half * 512, (half + 1) * 512)
                        (pa, rpa), (pb2, rpb2) = pgu.next()
                        mm_group(pa, [(x1t[:, k, cs], wpg[:, k, hs]) for k in range(8)], [Rx1t, Rw], [rpa])
                        mm_group(pb2, [(pt_[:, k, cs], wpp[:, k, hs]) for k in range(2)], [Rpt, Rw], [rpb2])
                        sg_, rsg = sgl.next()
                        P.op("act", lambda e, sg_=sg_, pa=pa: e.activation(out=sg_, in_=pa, func=AF.Sigmoid), [rpa], [rsg])
                        P.op("dve", lambda e, sg_=sg_, pb2=pb2, cc=cc, hs=hs: e.tensor_tensor(out=acc[:, cc, hs], in0=sg_, in1=pb2, op=ALU.mult), [rsg, rpb2], [Racc[cc]])
                for (wg_d, wu_d, wd_d, dff, eidx) in experts:
                    f0 = 0
                    while f0 < dff:
                        fw = min(512, dff - f0)
                        nfc = fw // 128
                        wg_, rwg = wgs.next(); wu_, rwu = wus.next(); wd_, rwd = wds.next()
                        for k in range(8):
                            P.dma("pool", lambda e, wg_=wg_, wg_d=wg_d, k=k, f0=f0, fw=fw: e.dma_start(out=wg_[:, k, 0:fw], in_=wg_d[k * 128:(k + 1) * 128, f0:f0 + fw]), writes=[rwg])
                            P.dma("pool", lambda e, wu_=wu_, wu_d=wu_d, k=k, f0=f0, fw=fw: e.dma_start(out=wu_[:, k, 0:fw], in_=wu_d[k * 128:(k + 1) * 128, f0:f0 + fw]), writes=[rwu])
                        for fc in range(nfc):
                            P.dma("pool", lambda e, wd_=wd_, wd_d=wd_d, fc=fc, f0=f0: e.dma_start(out=wd_[:, fc, :], in_=wd_d[f0 + fc * 128:f0 + (fc + 1) * 128, :]), writes=[rwd])
                        at, rat = actT.next()
                        for ts in range(TT // 512):
                            tsl = slice(ts * 512, (ts + 1) * 512)
                            for fc in range(nfc):
                                (pgt, rpgt), (put, rput) = pgu.next()
                                fs = slice(fc * 128, (fc + 1) * 128)
                                mm_group(pgt, [(wg_[:, k, fs], x1t[:, k, tsl]) for k in range(8)], [rwg, Rx1t], [rpgt])
                                mm_group(put, [(wu_[:, k, fs], x1t[:, k, tsl]) for k in range(8)], [rwu, Rx1t], [rput])
                                sg_, rsg = sgl.next()
                                P.op("act", lambda e, sg_=sg_, pgt=pgt: e.activation(out=sg_, in_=pgt, func=AF.Silu), [rpgt], [rsg])
                                P.op("dve", lambda e, sg_=sg_, put=put, at=at, fc=fc, tsl=tsl: e.tensor_tensor(out=at[:, fc, tsl], in0=sg_, in1=put, op=ALU.mult), [rsg, rput], [rat])
                        for cc in range(NCT):
                            cs = slice(cc * 128, (cc + 1) * 128)
                            for half in range(2):
                                hs = slice(half * 512, (half + 1) * 512)
                                pd, rpd = pdn.next()
                                mm_group(pd, [(at[:, fc, cs], wd_[:, fc, hs]) for fc in range(nfc)], [rat, rwd], [rpd])
                                if eidx is None:
                                    P.op("dve", lambda e, pd=pd, cc=cc, hs=hs: e.tensor_tensor(out=acc[:, cc, hs], in0=acc[:, cc, hs], in1=pd, op=ALU.add), [rpd, Racc[cc]], [Racc[cc]])
                                else:
                                    P.op("dve", lambda e, pd=pd, cc=cc, hs=hs, eidx=eidx: e.scalar_tensor_tensor(out=acc[:, cc, hs], in0=pd, scalar=cmbt[:, cc, eidx:eidx + 1], in1=acc[:, cc, hs], op0=ALU.mult, op1=ALU.add), [rpd, Racc[cc], Rcm], [Racc[cc]])
                        f0 += fw
                for cc in range(NCT):
                    c = tt * NCT + cc
                    rows = slice(c * 128, (c + 1) * 128)
                    xr_, rxr = xr.next()
                    P.dma("sp", lambda e, xr_=xr_, rows=rows: e.dma_start(out=xr_, in_=x1res[rows, :]), writes=[rxr])
                    P.op("dve", lambda e, xr_=xr_, cc=cc: e.scalar_tensor_tensor(out=acc[:, cc, :], in0=xr_, scalar=ALPHA, in1=acc[:, cc, :], op0=ALU.mult, op1=ALU.add), [rxr, Racc[cc]], [Racc[cc]])
                    x2_, rx2 = x2b.next()
                    layernorm(acc[:, cc, :], Racc[cc], g_bc, b_bc, Rgb, x2_, rx2, lntmp.next())
                    dst = out if last else xres
                    o = P.dma("sp", lambda e, x2_=x2_, rows=rows, dst=dst: e.dma_start(out=dst[rows, :], in_=x2_), reads=[rx2], writes=[Rout])
                    if not last:
                        to_xT(x2_, rx2, xT, rows, RxTn, xbst, ostt)

        if "p0" in phases:
            phase0()
            P.barrier()
            ar.top = base_top
        for li in (0, 1):
            if f"l{li}" not in phases:
                continue
            if "1" in subph:
                phase1(li)
                P.barrier()
                ar.top = base_top
            if "A" in subph:
                phaseA()
                P.barrier()
                ar.top = base_top
            if "C" in subph:
                phaseC(li)
                P.barrier()
                ar.top = base_top
            if "D" in subph:
                phaseD(li)
                P.barrier()
                ar.top = base_top
            if "34" in subph:
                phaseP(li)
                P.barrier()
                ar.top = base_top
                phase3(li)
                P.barrier()
                ar.top = base_top
                phase4(li)
                P.barrier()
                ar.top = base_top

        P.barrier()
        lasts = list(P.dma_last.values())
        P.emit(final_ops=lasts)
    P.inputs = I
    return nc, P


def host_consts():
    r = np.arange(128)
    U = (r[:, None] <= r[None, :]).astype(np.float32)
    Lm8 = -8.0 * (r[:, None] >= r[None, :]).astype(np.float32)
    t = np.arange(512)
    maskd = np.zeros((128, 4, 512), np.float32)
    for i in range(4):
        maskd[:, i, :] = (r[:, None] + 128 * i < t[None, :])
    return {"c_ident": np.eye(128, dtype=np.float32), "c_U": U, "c_Lm8": Lm8, "c_maskd": maskd}


def host_layout(inputs, b, S):
    f = lambda a: np.ascontiguousarray(np.asarray(a, dtype=np.float32))
    m = {}
    m["x"] = f(inputs["x"][b, :S])
    m["p"] = f(inputs["p"][:, b, :S])
    for k in ["w_in", "w_br_a", "w_br_b", "w_br_c", "w_br_d", "w_out", "sg_ln_g", "sg_ln_b",
              "ssd_dt_bias", "ssd_a_log", "ssd_d", "ssd_norm_g", "ln1_g", "ln1_b",
              "ffn_w_gate", "ffn_w_up", "ffn_w_down", "moe_router", "moe_w_gate", "moe_w_up",
              "moe_w_down", "ple_w_gate", "ple_w_proj", "ln2_g", "ln2_b"]:
        m[k] = f(inputs[k])
    m["sg_wT"] = f(np.transpose(np.asarray(inputs["sg_w"]), (0, 3, 1, 2)))
    m["sg_bT"] = f(np.transpose(np.asarray(inputs["sg_b"]), (0, 2, 1)))
    rb = np.asarray(inputs["ca_rel_bias"], dtype=np.float32)
    j = np.arange(128)[:, None, None]
    o = np.arange(5)[None, :, None]
    i = np.arange(128)[None, None, :]
    tq = 512 + i
    sk = o * 128 + j
    rel = tq - sk
    idx = np.clip(rel, -63, 256) + 63
    diff = tq // 64 - sk // 64
    ok = (diff >= 0) & (diff <= 8)
    g = rb[:, :, idx]
    g = np.where(ok[None, None], g, np.float32(NEG))
    m["ca_biasT"] = f(np.transpose(g, (0, 2, 1, 3, 4)))
    cw = np.asarray(inputs["ssd_conv_w"], dtype=np.float32)
    m["conv_wT"] = f(np.transpose(cw.reshape(2, 4, 6, 128), (0, 3, 2, 1)))
    cb = np.asarray(inputs["ssd_conv_b"], dtype=np.float32)
    m["conv_bT"] = f(np.transpose(cb.reshape(2, 6, 128), (0, 2, 1)))
    m.update(host_consts())
    return m


def kernel(**inputs):
    S = 8192
    nc, _ = build(S)
    in_maps = [host_layout(inputs, b, S) for b in range(8)]
    res = run_bass_kernel_spmd(nc, in_maps, core_ids=list(range(8)))
    return np.stack([np.asarray(r["out"], dtype=np.float32) for r in res.results], axis=0)
```

```python
import contextlib
import numpy as np
import concourse.bass as bass
import concourse.mybir as mybir
from concourse.bass_utils import run_bass_kernel_spmd

F32 = mybir.dt.float32
BF16 = mybir.dt.bfloat16
AF = mybir.ActivationFunctionType
ALU = mybir.AluOpType

NDMA_SEM = 14
ALPHA = 4 ** 0.25
LN_EPS = 1e-5
D_FF = 2816
D_FFE = 3584
NEG = -30000.0


class Res:
    __slots__ = ("name", "w", "r", "excl")

    def __init__(self, name, excl=False):
        self.name = name
        self.w = None
        self.r = []
        self.excl = excl


class Op:
    __slots__ = ("eng", "fn", "deps", "signal", "val", "semkey", "isdma")

    def __init__(self, eng, fn):
        self.eng = eng
        self.fn = fn
        self.deps = []
        self.signal = False
        self.val = None
        self.semkey = None
        self.isdma = False


class Prog:
    def __init__(self, nc):
        self.nc = nc
        self.ops = {e: [] for e in ("pe", "act", "dve", "pool", "sp")}
        self.dma_rr = {"sp": 0, "pool": 0}
        self.dma_last = {}

    def _deps(self, op, reads, writes):
        ex = [r for r in reads if r.excl]
        if ex:
            reads = [r for r in reads if not r.excl]
            writes = list(writes) + ex
        deps = []
        for r in reads:
            if r.w is not None:
                deps.append(r.w)
        for r in writes:
            if r.w is not None:
                deps.append(r.w)
            deps.extend(r.r)
        for r in reads:
            r.r.append(op)
        for r in writes:
            r.w = op
            r.r = []
        seen = set()
        out = []
        for d in deps:
            if id(d) not in seen and d is not op:
                seen.add(id(d))
                out.append(d)
        return out

    def op(self, eng, fn, reads=(), writes=()):
        o = Op(eng, fn)
        o.semkey = eng
        o.deps = self._deps(o, reads, writes)
        if eng == "pe":
            o.deps = [d for d in o.deps if not (d.eng == "pe" and not d.isdma)]
        self.ops[eng].append(o)
        return o

    def dma(self, queue, fn, reads=(), writes=()):
        o = Op(queue, fn)
        o.isdma = True
        slot = self.dma_rr[queue]
        self.dma_rr[queue] = (slot + 1) % NDMA_SEM
        o.semkey = (queue, slot)
        o.deps = self._deps(o, reads, writes)
        prev = self.dma_last.get(o.semkey)
        if prev is not None:
            o.deps.append(prev)
        self.dma_last[o.semkey] = o
        o.signal = True
        self.ops[queue].append(o)
        return o

    def barrier(self):
        lasts = []
        for e, lst in self.ops.items():
            if lst:
                lasts.append(lst[-1])
        for k, o in self.dma_last.items():
            lasts.append(o)
        for e in ("pe", "act", "dve", "pool", "sp"):
            o = Op(e, lambda eng: eng.nop())
            o.semkey = e
            o.deps = [d for d in lasts if d.eng != e or d.isdma]
            self.ops[e].append(o)

    def emit(self, final_ops=()):
        nc = self.nc
        for e, lst in self.ops.items():
            for o in lst:
                for d in o.deps:
                    d.signal = True
        for o in final_ops:
            o.signal = True
        cnt = {}
        for e, lst in self.ops.items():
            for o in lst:
                if o.signal:
                    inc = 16 if o.isdma else 1
                    cnt[o.semkey] = cnt.get(o.semkey, 0) + inc
                    o.val = cnt[o.semkey]
        self.maxcnt = dict(cnt)
        with contextlib.ExitStack() as st:
            sems = {}
            for k in cnt:
                nm = k if isinstance(k, str) else f"{k[0]}{k[1]}"
                sems[k] = st.enter_context(nc.semaphore("s_" + nm))
            block = st.enter_context(nc.Block())
            engmap = {"pe": block.tensor, "act": block.scalar, "dve": block.vector,
                      "pool": block.gpsimd, "sp": block.sync}

            def make(e):
                lst = self.ops[e]

                def body(eng):
                    known = {}
                    for o in lst:
                        need = {}
                        for d in o.deps:
                            if d.val > known.get(d.semkey, 0):
                                need[d.semkey] = max(need.get(d.semkey, 0), d.val)
                        for k, v in need.items():
                            eng.wait_ge(sems[k], v)
                            known[k] = v
                        ins = o.fn(eng)
                        if o.signal:
                            ins.then_inc(sems[o.semkey], 16 if o.isdma else 1)
                    if e == "sp":
                        for o in final_ops:
                            if o.val > known.get(o.semkey, 0):
                                eng.wait_ge(sems[o.semkey], o.val)
                                known[o.semkey] = o.val
                return body

            for e in ("pe", "act", "dve", "pool", "sp"):
                engmap[e](make(e))


class Arena:
    def __init__(self, nc, st, nbytes):
        self.n4 = nbytes // 4
        self.t = st.enter_context(nc.sbuf_tensor("arena", [128, self.n4], F32))
        self.top = 0

    def alloc(self, shape, dt):
        esz = 2 if dt == BF16 else 4
        n = 1
        for s in shape:
            n *= s
        nb = (n * esz + 63) // 64 * 64
        off4 = self.top // 4
        self.top += nb
        assert self.top // 4 <= self.n4, ("arena overflow", self.top)
        v = self.t[:, off4:off4 + nb // 4]
        if dt != F32:
            v = v.bitcast(dt)
        v = v[:, 0:n]
        if len(shape) == 2:
            v = v.rearrange("p (a b) -> p a b", a=shape[0])
        elif len(shape) == 3:
            v = v.rearrange("p (a b c) -> p a b c", a=shape[0], b=shape[1])
        return v


class RR:
    def __init__(self, items):
        self.items = items
        self.i = 0

    def next(self):
        it = self.items[self.i % len(self.items)]
        self.i += 1
        return it


def build(S, dbg=(), phases=("p0", "l0", "l1"), subph=("1", "A", "C", "D", "34")):
    nc = bass.Bass("TRN2", target_bir_lowering=False)
    NT = S // 512
    NCH = S // 128
    I = {}

    def din(name, shape, dt=F32):
        I[name] = nc.dram_tensor(name, list(shape), dt, kind="ExternalInput").ap()
        return I[name]

    def dscr(name, shape, dt):
        kind = "ExternalOutput" if name in dbg else "Internal"
        return nc.dram_tensor(name, list(shape), dt, kind=kind).ap()

    x = din("x", [S, 1024])
    pin = din("p", [2, S, 256])
    w_in = din("w_in", [2, 1024, 7432])
    w_br = [din("w_br_a", [2, 256, 1024]), din("w_br_b", [2, 256, 1024]),
            din("w_br_c", [2, 256, 1024]), din("w_br_d", [2, 512, 1024])]
    w_out = din("w_out", [2, 1024, 1024])
    sg_ln_g = din("sg_ln_g", [2, 256]); sg_ln_b = din("sg_ln_b", [2, 256])
    sg_wT = din("sg_wT", [2, 128, 4, 128])
    sg_bT = din("sg_bT", [2, 128, 4])
    ca_biasT = din("ca_biasT", [2, 128, 4, 5, 128])
    conv_wT = din("conv_wT", [2, 128, 6, 4])
    conv_bT = din("conv_bT", [2, 128, 6])
    dt_bias = din("ssd_dt_bias", [2, 8]); a_log = din("ssd_a_log", [2, 8])
    ssd_d = din("ssd_d", [2, 8]); ssd_norm_g = din("ssd_norm_g", [2, 512])
    ln1_g = din("ln1_g", [2, 1024]); ln1_b = din("ln1_b", [2, 1024])
    if "34" in subph:
        ffn_wg = din("ffn_w_gate", [1, 1024, D_FF]); ffn_wu = din("ffn_w_up", [1, 1024, D_FF])
        ffn_wd = din("ffn_w_down", [1, D_FF, 1024])
        moe_r = din("moe_router", [1, 1024, 8])
        moe_wg = din("moe_w_gate", [1, 8, 1024, D_FFE]); moe_wu = din("moe_w_up", [1, 8, 1024, D_FFE])
        moe_wd = din("moe_w_down", [1, 8, D_FFE, 1024])
    ple_wg = din("ple_w_gate", [2, 1024, 1024]); ple_wp = din("ple_w_proj", [2, 256, 1024])
    ln2_g = din("ln2_g", [2, 1024]); ln2_b = din("ln2_b", [2, 1024])
    c_ident = din("c_ident", [128, 128])
    c_U = din("c_U", [128, 128])
    c_Lm8 = din("c_Lm8", [128, 128])
    c_maskd = din("c_maskd", [128, 4, 512])
    c_identf = c_ident

    out = nc.dram_tensor("out", [S, 1024], F32, kind="ExternalOutput").ap()

    xT = dscr("xT", [1024, S], BF16)
    xres = dscr("xres", [S, 1024], F32)
    qkTa = dscr("qkTa", [512, S], BF16)
    qkTc = dscr("qkTc", [512, S], BF16)
    xbcT = dscr("xbcT", [768, S], F32)
    v_a = dscr("v_a", [S, 256], BF16)
    v_c = dscr("v_c", [S, 256], BF16)
    zs = dscr("zs", [S, 512], F32)
    dtr = dscr("dtr", [128, NCH * 8], F32)
    ybT = dscr("ybT", [256, S], BF16)
    yaT = dscr("yaT", [256, S], BF16)
    ycT = dscr("ycT", [256, S], BF16)
    ydT = dscr("ydT", [512, S], BF16)
    x1T = dscr("x1T", [1024, S], BF16)
    x1res = dscr("x1res", [S, 1024], F32)
    pT = dscr("pT", [256, S], BF16)
    combs = dscr("combs", [128, NCH * 8], F32)

    st = contextlib.ExitStack()
    with st:
        P = Prog(nc)
        ar = Arena(nc, st, 200 * 1024)
        pbw = []
        for i in range(3):
            pbw.append((st.enter_context(nc.psum_tensor(f"pbw{i}", [128, 1024], F32))[:], Res(f"pbw{i}", True)))
        pb = []
        for i in range(3):
            for hlf in range(2):
                pb.append((pbw[i][0][:, hlf * 512:(hlf + 1) * 512], Res(f"pb{2 * i + hlf}", True)))
        pb.append((st.enter_context(nc.psum_tensor("pb6", [128, 512], F32))[:], Res("pb6", True)))
        ptb_t = st.enter_context(nc.psum_tensor("ptb", [128, 1024], BF16))
        R_ptb = Res("ptb", True)
        ptb = [(ptb_t[:, 0:512], R_ptb), (ptb_t[:, 512:1024], R_ptb)]

        def mm_group(out_ap, pairs, reads, writes):
            def fn(e):
                n = len(pairs)
                ins = None
                for i, (l, r) in enumerate(pairs):
                    ins = e.matmul(out_ap, lhsT=l, rhs=r, start=(i == 0), stop=(i == n - 1))
                return ins
            return P.op("pe", fn, reads, writes)

        ident = ar.alloc([128], BF16); R_const = Res("const")
        U_f = ar.alloc([128], F32)
        U_bf = ar.alloc([128], BF16)
        Lm8 = ar.alloc([128], BF16)
        onesm8 = ar.alloc([128], BF16)
        ones_f = ar.alloc([128], F32)
        epsb = ar.alloc([1], F32)
        P.dma("pool", lambda e: e.dma_start(out=ident, in_=c_ident), writes=[R_const])
        P.dma("sp", lambda e: e.dma_start(out=U_f, in_=c_U), writes=[R_const])
        P.dma("pool", lambda e: e.dma_start(out=U_bf, in_=c_U), writes=[R_const])
        P.dma("pool", lambda e: e.dma_start(out=Lm8, in_=c_Lm8), writes=[R_const])
        P.op("pool", lambda e: e.memset(onesm8, -8.0), writes=[R_const])
        P.op("pool", lambda e: e.memset(ones_f, 1.0), writes=[R_const])
        P.op("pool", lambda e: e.memset(epsb, LN_EPS), writes=[R_const])
        base_top = ar.top
        final_ops = []

        def phase0():
            xb = RR([(ar.alloc([1024], BF16), Res("xb")) for _ in range(2)])
            xs = RR([(ar.alloc([8, 128], BF16), Res("xs")) for _ in range(2)])
            Rx = Res("xT")
            for c in range(NCH):
                b, rb = xb.next()
                P.dma("pool", lambda e, b=b, c=c: e.dma_start(out=b, in_=x[c * 128:(c + 1) * 128, :]), writes=[rb])
                for half in range(2):
                    pt, rpt = ptb[half]
                    P.op("pe", lambda e, b=b, pt=pt, half=half: [e.transpose(out=pt[:, k * 128:(k + 1) * 128], in_=b[:, (half * 4 + k) * 128:(half * 4 + k + 1) * 128], identity=ident) for k in range(4)][-1],
                         reads=[rb, R_const], writes=[rpt])
                    s_, rs = xs.items[xs.i % 2]
                    eng = "dve" if half == 0 else "act"
                    if eng == "dve":
                        P.op("dve", lambda e, s_=s_, pt=pt, half=half: e.tensor_copy(out=s_[:, half * 4:half * 4 + 4, :], in_=pt.rearrange("p (k n) -> p k n", k=4)), reads=[rpt], writes=[rs])
                    else:
                        P.op("act", lambda e, s_=s_, pt=pt, half=half: e.copy(out=s_[:, half * 4:half * 4 + 4, :], in_=pt.rearrange("p (k n) -> p k n", k=4)), reads=[rpt], writes=[rs])
                s_, rs = xs.next()
                P.dma("sp", lambda e, s_=s_, c=c: e.dma_start(out=xT.rearrange("(k p) s -> p k s", p=128)[:, :, c * 128:(c + 1) * 128], in_=s_), reads=[rs], writes=[Rx])

        def phase1(li):
            w1 = ar.alloc([8, 3336], BF16); Rw1 = Res("w1")
            for k in range(8):
                P.dma("pool", lambda e, k=k: e.dma_start(out=w1[:, k, :], in_=w_in[li, k * 128:(k + 1) * 128, 0:3336]), writes=[Rw1])
            lng = ar.alloc([256], F32); lnb = ar.alloc([256], F32)
            sgw = ar.alloc([4, 128], BF16); sgw_f = ar.alloc([4, 128], F32); sgb = ar.alloc([4], F32)
            Rc = Res("p1const")
            P.dma("sp", lambda e: e.dma_start(out=lng, in_=sg_ln_g[li:li + 1, :].partition_broadcast(128)), writes=[Rc])
            P.dma("sp", lambda e: e.dma_start(out=lnb, in_=sg_ln_b[li:li + 1, :].partition_broadcast(128)), writes=[Rc])
            P.dma("sp", lambda e: e.dma_start(out=sgw_f, in_=sg_wT[li]), writes=[Rc])
            P.dma("sp", lambda e: e.dma_start(out=sgb, in_=sg_bT[li]), writes=[Rc])
            P.op("dve", lambda e: e.tensor_tensor(out=sgw, in0=sgw_f, in1=U_f.unsqueeze(1).to_broadcast([128, 4, 128]), op=ALU.mult), reads=[Rc, R_const], writes=[Rc])
            xTt = RR([(ar.alloc([8, 512], BF16), Res("xTt")) for _ in range(2)])
            stA = RR([(ar.alloc([4, 512], BF16), Res("stA")) for _ in range(2)])
            stC = RR([(ar.alloc([4, 512], BF16), Res("stC")) for _ in range(2)])
            stD = RR([(ar.alloc([6, 512], F32), Res("stD")) for _ in range(2)])
            stV = RR([(ar.alloc([512], BF16), Res("stV")) for _ in range(2)])
            stZ = RR([(ar.alloc([512], F32), Res("stZ")) for _ in range(2)])
            stDt = RR([(ar.alloc([8], F32), Res("stDt")) for _ in range(2)])
            uvg = RR([(ar.alloc([512], F32), Res("uvg")) for _ in range(2)])
            stat = RR([(ar.alloc([8], F32), Res("stat")) for _ in range(2)])
            rstd = RR([(ar.alloc([2], F32), Res("rstd")) for _ in range(2)])
            vn = RR([(ar.alloc([256], F32), Res("vn")) for _ in range(2)])
            vnb = RR([(ar.alloc([256], BF16), Res("vnb")) for _ in range(2)])
            ybt = RR([(ar.alloc([256], BF16), Res("ybt")) for _ in range(2)])
            stB = RR([(ar.alloc([2, 512], BF16), Res("stB")) for _ in range(2)])
            fmps = RR([pb[0], pb[1]])
            R_scr = {n: Res(n) for n in ["qkTa", "qkTc", "xbcT", "v_a", "v_c", "zs", "dtr", "ybT"]}
            RxT = Res("xT_r")
            evi = [0]
            pendB = []

            def evac(out_ap, in_ap, reads, writes, func=None):
                if func is not None:
                    return P.op("act", lambda e: e.activation(out=out_ap, in_=in_ap, func=func), reads, writes)
                evi[0] += 1
                if evi[0] % 2:
                    return P.op("dve", lambda e: e.tensor_copy(out=out_ap, in_=in_ap), reads, writes)
                return P.op("act", lambda e: e.copy(out=out_ap, in_=in_ap), reads, writes)

            for t in range(NT):
                xt, rxt = xTt.next()
                tok = slice(t * 512, (t + 1) * 512)
                P.dma("sp", lambda e, xt=xt, tok=tok: e.dma_start(out=xt, in_=xT.rearrange("(k p) s -> p k s", p=128)[:, :, tok]), writes=[rxt])
                for (col0, nchk, stg, dst, rname) in ((0, 4, stA, qkTa, "qkTa"), (1280, 4, stC, qkTc, "qkTc"), (2560, 6, stD, xbcT, "xbcT")):
                    sg_, rsg = stg.next()
                    for j in range(nchk):
                        ps, rps = fmps.next()
                        c0 = col0 + j * 128
                        mm_group(ps, [(w1[:, k, c0:c0 + 128], xt[:, k, :]) for k in range(8)], [Rw1, rxt], [rps])
                        evac(sg_[:, j, :], ps, [rps], [rsg])
                    P.dma("sp", lambda e, sg_=sg_, dst=dst, tok=tok: e.dma_start(out=dst.rearrange("(k p) s -> p k s", p=128)[:, :, tok], in_=sg_), reads=[rsg], writes=[R_scr[rname]])
                sB, rsB = stB.next()
                for cc in range(4):
                    rows = slice(t * 512 + cc * 128, t * 512 + (cc + 1) * 128)
                    lh = [xt[:, k, cc * 128:(cc + 1) * 128] for k in range(8)]
                    psV, rV = pb[2]; psB, rB = pb[3]; psZ, rZ = pb[4]; psM, rM = pb[5]; psD, rD = pb[6]
                    mm_group(psV[:, 0:256], [(lh[k], w1[:, k, 512:768]) for k in range(8)], [Rw1, rxt], [rV])
                    mm_group(psV[:, 256:512], [(lh[k], w1[:, k, 1792:2048]) for k in range(8)], [Rw1, rxt], [rV])
                    mm_group(psB, [(lh[k], w1[:, k, 768:1280]) for k in range(8)], [Rw1, rxt], [rB])
                    mm_group(psZ, [(lh[k], w1[:, k, 2048:2560]) for k in range(8)], [Rw1, rxt], [rZ])
                    mm_group(psD[:, 0:8], [(lh[k], w1[:, k, 3328:3336]) for k in range(8)], [Rw1, rxt], [rD])
                    while pendB:
                        pendB.pop(0)()
                    sv, rsv = stV.next()
                    evac(sv, psV, [rV], [rsv])
                    P.dma("sp", lambda e, sv=sv, rows=rows: e.dma_start(out=v_a[rows, :], in_=sv[:, 0:256]), reads=[rsv], writes=[R_scr["v_a"]])
                    P.dma("sp", lambda e, sv=sv, rows=rows: e.dma_start(out=v_c[rows, :], in_=sv[:, 256:512]), reads=[rsv], writes=[R_scr["v_c"]])
                    sz, rsz = stZ.next()
                    evac(sz, psZ, [rZ], [rsz], func=AF.Silu)
                    P.dma("sp", lambda e, sz=sz, rows=rows: e.dma_start(out=zs[rows, :], in_=sz), reads=[rsz], writes=[R_scr["zs"]])
                    sd, rsd = stDt.next()
                    P.op("dve", lambda e, sd=sd, psD=psD: e.tensor_copy(out=sd, in_=psD[:, 0:8]), [rD], [rsd])
                    P.dma("sp", lambda e, sd=sd, t=t, cc=cc: e.dma_start(out=dtr[:, (t * 4 + cc) * 8:(t * 4 + cc + 1) * 8], in_=sd), reads=[rsd], writes=[R_scr["dtr"]])
                    ug, rug = uvg.next()
                    P.op("act", lambda e, ug=ug, psB=psB: e.activation(out=ug, in_=psB, func=AF.Gelu_apprx_tanh), [rB], [rug])
                    sta, rsta = stat.next()
                    P.op("dve", lambda e, sta=sta, ug=ug: e.bn_stats(out=sta[:, 0:6], in_=ug[:, 256:512]), [rug], [rsta])
                    P.op("dve", lambda e, sta=sta: e.bn_aggr(out=sta[:, 6:8], in_=sta[:, 0:6]), [rsta], [rsta])
                    rs_, rrs = rstd.next()
                    P.op("act", lambda e, rs_=rs_, sta=sta: e.activation(out=rs_[:, 0:1], in_=sta[:, 7:8], func=AF.Sqrt, bias=epsb, scale=1.0), [rsta, R_const], [rrs])
                    P.op("dve", lambda e, rs_=rs_: e.reciprocal(out=rs_[:, 1:2], in_=rs_[:, 0:1]), [rrs], [rrs])
                    v_, rv_ = vn.next()
                    P.op("dve", lambda e, v_=v_, ug=ug, sta=sta, rs_=rs_: e.tensor_scalar(out=v_, in0=ug[:, 256:512], scalar1=sta[:, 6:7], scalar2=rs_[:, 1:2], op0=ALU.subtract, op1=ALU.mult), [rug, rsta, rrs], [rv_])
                    P.op("pool", lambda e, v_=v_: e.tensor_tensor(out=v_, in0=v_, in1=lng, op=ALU.mult), [rv_, Rc], [rv_])
                    vb, rvb = vnb.next()
                    P.op("pool", lambda e, v_=v_, vb=vb: e.tensor_tensor(out=vb, in0=v_, in1=lnb, op=ALU.add), [rv_, Rc], [rvb])

                    def b2(vb=vb, rvb=rvb, ug=ug, rug=rug, psM=psM, rM=rM, sB=sB, rsB=rsB, cc=cc, tok=tok):
                        def mixfn(e):
                            ins = None
                            for g in range(4):
                                ins = e.matmul(psM[:, g * 64:(g + 1) * 64], lhsT=sgw[:, g, :], rhs=vb[:, g * 64:(g + 1) * 64], start=True, stop=True)
                            return ins
                        P.op("pe", mixfn, [rvb, Rc], [rM])
                        yb, ryb = ybt.next()

                        def gatefn(e):
                            ins = None
                            for g in range(4):
                                ins = e.scalar_tensor_tensor(out=yb[:, g * 64:(g + 1) * 64], in0=psM[:, g * 64:(g + 1) * 64], scalar=sgb[:, g:g + 1], in1=ug[:, g * 64:(g + 1) * 64], op0=ALU.add, op1=ALU.mult)
                            return ins
                        P.op("dve", gatefn, [rM, rug, Rc], [ryb])
                        pt, rpt = ptb[cc % 2]
                        P.op("pe", lambda e: [e.transpose(out=pt[:, k * 128:(k + 1) * 128], in_=yb[:, k * 128:(k + 1) * 128], identity=ident) for k in range(2)][-1], [ryb, R_const], [rpt])
                        P.op("act", lambda e: e.copy(out=sB[:, :, cc * 128:(cc + 1) * 128], in_=pt[:, 0:256].rearrange("p (k n) -> p k n", k=2)), [rpt], [rsB])
                        if cc == 3:
                            P.dma("sp", lambda e: e.dma_start(out=ybT.rearrange("(k p) s -> p k s", p=128)[:, :, tok], in_=sB), reads=[rsB], writes=[R_scr["ybT"]])
                    pendB.append(b2)
            while pendB:
                pendB.pop(0)()

        def phaseA():
            qk = ar.alloc([4, S], BF16); Rqk = Res("qkA")
            va = ar.alloc([NCH, 256], BF16); Rva = Res("vaA")
            maskd = ar.alloc([4, 512], BF16); Rm = Res("maskd")
            for k in range(4):
                P.dma("sp", lambda e, k=k: e.dma_start(out=qk[:, k, :], in_=qkTa[k * 128:(k + 1) * 128, :]), writes=[Rqk])
            P.dma("sp", lambda e: e.dma_start(out=va, in_=v_a.rearrange("(c p) f -> p c f", p=128)), writes=[Rva])
            P.dma("pool", lambda e: e.dma_start(out=maskd, in_=c_maskd), writes=[Rm])
            eb = RR([(ar.alloc([1024], F32), Res("eb")) for _ in range(2)])
            spb = RR([(ar.alloc([1024], BF16), Res("spb")) for _ in range(4)])
            ab = RR([(ar.alloc([1024], BF16), Res("ab")) for _ in range(3)])
            racc = ar.alloc([1024], F32); Rracc = Res("racc")
            raccb = RR([(ar.alloc([1024], BF16), Res("raccb")) for _ in range(3)])
            ost = RR([(ar.alloc([512], BF16), Res("ostA")) for _ in range(4)])
            zsets = RR(pbw)
            pos = [pb[6], (ptb_t[:].bitcast(F32), R_ptb)]
            RyaT = Res("yaT")
            steps = []
            for hp in range(2):
                for qt in range(NT):
                    kbs = list(range(4 * qt + 3, -1, -1))
                    for n, kb in enumerate(kbs):
                        steps.append(dict(hp=hp, qt=qt, n=n, kb=kb, diag=kb - 4 * qt, last=(n == len(kbs) - 1)))
            for i, sd in enumerate(steps):
                sd["rbp"] = None

            def v2(ap):
                return ap.rearrange("p (a b) -> p a b", a=2)

            def S1(i):
                sd = steps[i]
                hp, qt, kb, n, diag = sd["hp"], sd["qt"], sd["kb"], sd["n"], sd["diag"]
                zw, rzw = zsets.next(); sd["z"] = (zw, rzw)

                def zfn(e):
                    ins = None
                    for j in range(2):
                        pr = slice(j * 64, j * 64 + 64)
                        ins = e.matmul(zw[:, j * 512:(j + 1) * 512], lhsT=qk[pr, 2 + hp, kb * 128:(kb + 1) * 128], rhs=qk[pr, hp, qt * 512:(qt + 1) * 512], start=True, stop=True)
                    return ins
                P.op("pe", zfn, [Rqk], [rzw])
                e_, re_ = eb.next()
                P.op("act", lambda e: e.activation(out=e_, in_=zw, func=AF.Exp, scale=0.125), [rzw], [re_])
                sp_, rsp = spb.next(); sd["sp"] = (sp_, rsp)
                P.op("act", lambda e: e.activation(out=sp_, in_=e_, func=AF.Ln, bias=1.0, scale=1.0), [re_], [rsp])
                if diag >= 0:
                    P.op("pool", lambda e: e.tensor_tensor(out=v2(sp_), in0=v2(sp_), in1=maskd[:, diag, :].unsqueeze(1).to_broadcast([128, 2, 512]), op=ALU.mult), [rsp, Rm], [rsp])
                if not sd["last"]:
                    if n == 0:
                        P.op("dve", lambda e: e.tensor_copy(out=racc, in_=sp_), [rsp], [Rracc])
                    else:
                        P.op("dve", lambda e: e.tensor_tensor(out=racc, in0=racc, in1=sp_, op=ALU.add), [rsp, Rracc], [Rracc])
                    rb_, rrb = raccb.next()
                    P.op("dve", lambda e: e.tensor_copy(out=rb_, in_=racc), [Rracc], [rrb])
                    steps[i + 1]["rbp"] = (rb_, rrb)

            def S2(i):
                sd = steps[i]
                diag = sd["diag"]
                zw, rzw = sd["z"]; sp_, rsp = sd["sp"]; rbp = sd["rbp"]

                def accfn(e):
                    ins = None
                    for j in range(2):
                        o = zw[:, j * 512:(j + 1) * 512]
                        ins = e.matmul(o, lhsT=Lm8, rhs=sp_[:, j * 512:(j + 1) * 512], start=False, stop=(rbp is None))
                        if rbp is not None:
                            ins = e.matmul(o, lhsT=onesm8, rhs=rbp[0][:, j * 512:(j + 1) * 512], start=False, stop=True)
                    return ins
                rd = [rsp, R_const] + ([rbp[1]] if rbp is not None else [])
                P.op("pe", accfn, rd, [rzw])
                a_, ra = ab.next(); sd["a"] = (a_, ra)
                P.op("act", lambda e: e.activation(out=a_, in_=zw, func=AF.Exp, scale=0.125), [rzw], [ra])
                if diag >= 0:
                    P.op("pool", lambda e: e.tensor_tensor(out=v2(a_), in0=v2(a_), in1=maskd[:, diag, :].unsqueeze(1).to_broadcast([128, 2, 512]), op=ALU.mult), [ra, Rm], [ra])

            def S3(i):
                sd = steps[i]
                hp, qt, kb, n, last = sd["hp"], sd["qt"], sd["kb"], sd["n"], sd["last"]
                a_, ra = sd["a"]

                def avfn(e):
                    ins = None
                    for j in range(2):
                        h = 2 * hp + j
                        ins = e.matmul(pos[j][0][0:64, :], lhsT=va[:, kb, h * 64:(h + 1) * 64], rhs=a_[:, j * 512:(j + 1) * 512], start=(n == 0), stop=last)
                    return ins
                P.op("pe", avfn, [Rva, ra], [pos[0][1], pos[1][1]])
                if last:
                    for j in range(2):
                        h = 2 * hp + j
                        o_, ro = ost.next()
                        P.op("dve", lambda e, o_=o_, j=j: e.tensor_copy(out=o_[0:64, :], in_=pos[j][0][0:64, :]), [pos[j][1]], [ro])
                        P.dma("sp", lambda e, o_=o_, h=h: e.dma_start(out=yaT[h * 64:(h + 1) * 64, qt * 512:(qt + 1) * 512], in_=o_[0:64, :]), reads=[ro], writes=[RyaT])
                sd.clear()

            NS = len(steps)
            for s in range(NS + 2):
                if s < NS:
                    S1(s)
                if 1 <= s <= NS:
                    S2(s - 1)
                if s >= 2:
                    S3(s - 2)

        def phaseC(li):
            qk = ar.alloc([4, S], BF16); Rqk = Res("qkC")
            v1 = ar.alloc([NCH, 4, 65], BF16); Rv1 = Res("v1C")
            bias = ar.alloc([4, 5, 128], F32); Rb = Res("biasC")
            for k in range(4):
                P.dma("sp", lambda e, k=k: e.dma_start(out=qk[:, k, :], in_=qkTc[k * 128:(k + 1) * 128, :]), writes=[Rqk])
            P.op("pool", lambda e: e.memset(v1, 1.0), writes=[Rv1])
            for hh in range(4):
                P.dma("sp", lambda e, hh=hh: e.dma_start(out=v1[:, :, hh, 0:64], in_=v_c.rearrange("(c p) f -> p c f", p=128)[:, :, hh * 64:(hh + 1) * 64]), writes=[Rv1])
            P.dma("sp", lambda e: e.dma_start(out=bias, in_=ca_biasT[li]), writes=[Rb])
            sbuf_s = RR([(ar.alloc([5, 128], F32), Res("sC")) for _ in range(3)])
            pT = RR([(ar.alloc([5, 128], BF16), Res("pT")) for _ in range(4)])
            rec = RR([(ar.alloc([4], F32), Res("recC")) for _ in range(2)])
            yc = RR([(ar.alloc([4, 64], BF16), Res("ycC")) for _ in range(2)])
            ost = RR([(ar.alloc([2, 128], BF16), Res("ostC")) for _ in range(2)])
            psA = RR([pb[0], pb[2]])
            psB = RR([pb[1], pb[3]])
            psO = RR([pb[4], pb[5]])
            RycT = Res("ycT")
            cur = {}

            def C1(qc, h):
                os_ = [o for o in range(5) if qc - 4 + o >= 0]
                pr = slice((h % 2) * 64, (h % 2) * 64 + 64)
                q_ap = qk[pr, h // 2, qc * 128:(qc + 1) * 128]
                pa, rpa = psA.next()
                pb_, rpb = psB.next()

                def scfn(e):
                    ins = None
                    for o in os_:
                        kb = qc - 4 + o
                        dst = pa[:, o * 128:(o + 1) * 128] if o < 4 else pb_[:, 0:128]
                        ins = e.matmul(dst, lhsT=qk[pr, 2 + h // 2, kb * 128:(kb + 1) * 128], rhs=q_ap, start=True, stop=True)
                    return ins
                P.op("pe", scfn, [Rqk], [rpa, rpb])
                s_, rs = sbuf_s.next()
                o_lo = [o for o in os_ if o < 4]
                if o_lo:
                    a0, a1 = o_lo[0], o_lo[-1] + 1
                    P.op("dve", lambda e: e.scalar_tensor_tensor(out=s_[:, a0:a1, :], in0=pa[:, a0 * 128:a1 * 128].rearrange("p (o n) -> p o n", n=128), scalar=0.125, in1=bias[:, h, a0:a1, :], op0=ALU.mult, op1=ALU.add), [rpa, Rb], [rs])
                P.op("dve", lambda e: e.scalar_tensor_tensor(out=s_[:, 4, :], in0=pb_[:, 0:128], scalar=0.125, in1=bias[:, h, 4, :], op0=ALU.mult, op1=ALU.add), [rpb, Rb], [rs])
                p_, rp = pT.next()
                o0 = os_[0]
                P.op("act", lambda e: e.activation(out=p_[:, o0:5, :], in_=s_[:, o0:5, :], func=AF.Exp), [rs], [rp])
                cur[(qc, h)] = (p_, rp, os_)

            def C2(qc, h):
                p_, rp, os_ = cur.pop((qc, h))
                if h == 0:
                    cur["po"] = psO.next()
                po, rpo = cur["po"]

                def avfn(e):
                    ins = None
                    for n, o in enumerate(os_):
                        kb = qc - 4 + o
                        ins = e.matmul(po[:, h * 65:(h + 1) * 65], lhsT=p_[:, o, :], rhs=v1[:, kb, h, :], start=(n == 0), stop=(n == len(os_) - 1))
                    return ins
                P.op("pe", avfn, [rp, Rv1], [rpo])
                if h < 3:
                    return
                r_, rr = rec.next()
                pov = po[:, 0:260].rearrange("p (h d) -> p h d", h=4)
                P.op("dve", lambda e: e.reciprocal(out=r_, in_=pov[:, :, 64]), [rpo], [rr])
                y_, ry = yc.next()
                P.op("dve", lambda e: e.tensor_tensor(out=y_, in0=pov[:, :, 0:64], in1=r_.unsqueeze(2).to_broadcast([128, 4, 64]), op=ALU.mult), [rpo, rr], [ry])
                pt, rpt = ptb[qc % 2]
                P.op("pe", lambda e: [e.transpose(out=pt[:, k * 128:(k + 1) * 128], in_=y_[:, 2 * k:2 * k + 2, :].rearrange("p a b -> p (a b)"), identity=ident) for k in range(2)][-1], [ry, R_const], [rpt])
                o_, ro = ost.next()
                P.op("act", lambda e: e.copy(out=o_, in_=pt[:, 0:256].rearrange("p (k n) -> p k n", k=2)), [rpt], [ro])
                P.dma("sp", lambda e: e.dma_start(out=ycT.rearrange("(k p) s -> p k s", p=128)[:, :, qc * 128:(qc + 1) * 128], in_=o_), reads=[ro], writes=[RycT])

            units = [(qc, h) for qc in range(NCH) for h in range(4)]
            for u in range(len(units) + 2):
                if u < len(units):
                    C1(*units[u])
                if u >= 2:
                    C2(*units[u - 2])

        def phaseD(li):
            cw = ar.alloc([6, 4], F32); cb = ar.alloc([6], F32); Rc = Res("dconst")
            dtb = ar.alloc([8], F32); alog = ar.alloc([8], F32); dsk = ar.alloc([8], F32)
            ng = ar.alloc([512], F32)
            P.dma("sp", lambda e: e.dma_start(out=cw, in_=conv_wT[li]), writes=[Rc])
            P.dma("sp", lambda e: e.dma_start(out=cb, in_=conv_bT[li]), writes=[Rc])
            P.dma("sp", lambda e: e.dma_start(out=dtb, in_=dt_bias[li:li + 1, :].partition_broadcast(128)), writes=[Rc])
            P.dma("sp", lambda e: e.dma_start(out=alog, in_=a_log[li:li + 1, :].partition_broadcast(128)), writes=[Rc])
            P.dma("sp", lambda e: e.dma_start(out=dsk, in_=ssd_d[li:li + 1, :].partition_broadcast(128)), writes=[Rc])
            P.dma("sp", lambda e: e.dma_start(out=ng, in_=ssd_norm_g[li:li + 1, :].partition_broadcast(128)), writes=[Rc])
            P.op("act", lambda e: e.activation(out=alog, in_=alog, func=AF.Exp), [Rc], [Rc])
            P.op("dve", lambda e: e.tensor_scalar(out=alog, in0=alog, scalar1=-1.0, scalar2=None, op0=ALU.mult), [Rc], [Rc])
            dta = ar.alloc([NCH, 8], F32); dtA = ar.alloc([NCH, 8], F32); Rdt = Res("dt")
            P.dma("sp", lambda e: e.dma_start(out=dta, in_=dtr.rearrange("p (c h) -> p c h", h=8)), writes=[Rdt])
            P.op("dve", lambda e: e.tensor_tensor(out=dta, in0=dta, in1=dtb.unsqueeze(1).to_broadcast([128, NCH, 8]), op=ALU.add), [Rdt, Rc], [Rdt])
            P.op("act", lambda e: e.activation(out=dta, in_=dta, func=AF.Exp), [Rdt], [Rdt])
            P.op("act", lambda e: e.activation(out=dta, in_=dta, func=AF.Ln, bias=1.0, scale=1.0), [Rdt], [Rdt])
            P.op("dve", lambda e: e.tensor_tensor(out=dtA, in0=dta, in1=alog.unsqueeze(1).to_broadcast([128, NCH, 8]), op=ALU.mult), [Rdt, Rc], [Rdt])
            cvo = ar.alloc([6, S], BF16); Rcv = Res("cvo")
            SEG = min(S, 2048)
            xin = RR([(ar.alloc([SEG + 3], F32), Res("xin")) for _ in range(2)])
            acc = RR([(ar.alloc([SEG], F32), Res("acc")) for _ in range(2)])
            for fc in range(6):
                for sg in range(S // SEG):
                    xi, rxi = xin.next()
                    if sg == 0:
                        P.op("pool", lambda e, xi=xi: e.memset(xi[:, 0:3], 0.0), writes=[rxi])
                        P.dma("sp", lambda e, xi=xi, fc=fc: e.dma_start(out=xi[:, 3:SEG + 3], in_=xbcT[fc * 128:(fc + 1) * 128, 0:SEG]), writes=[rxi])
                    else:
                        P.dma("sp", lambda e, xi=xi, fc=fc, sg=sg: e.dma_start(out=xi, in_=xbcT[fc * 128:(fc + 1) * 128, sg * SEG - 3:(sg + 1) * SEG]), writes=[rxi])
                    ac, rac = acc.next()
                    P.op("dve", lambda e, ac=ac, xi=xi, fc=fc: e.tensor_scalar(out=ac, in0=xi[:, 0:SEG], scalar1=cw[:, fc, 0:1], scalar2=cb[:, fc:fc + 1], op0=ALU.mult, op1=ALU.add), [rxi, Rc], [rac])
                    for k in (1, 2, 3):
                        eng = "dve"
                        P.op(eng, lambda e, ac=ac, xi=xi, fc=fc, k=k: e.scalar_tensor_tensor(out=ac, in0=xi[:, k:SEG + k], scalar=cw[:, fc, k:k + 1], in1=ac, op0=ALU.mult, op1=ALU.add), [rxi, Rc, rac], [rac])
                    P.op("act", lambda e, ac=ac, fc=fc, sg=sg: e.activation(out=cvo[:, fc, sg * SEG:(sg + 1) * SEG], in_=ac, func=AF.Silu), [rac], [Rcv])
            hst = ar.alloc([8, 64], F32); hsb = ar.alloc([8, 64], BF16); Rh = Res("hst"); Rhb = Res("hsb")
            P.op("pool", lambda e: e.memset(hst, 0.0), writes=[Rh])
            P.op("pool", lambda e: e.memset(hsb, 0.0), writes=[Rhb])
            fbs = RR([dict(xs_tm=ar.alloc([8, 64], F32), Rxs=Res("xs_tm"), b_tm=ar.alloc([128], BF16), Rbt=Res("b_tm"),
                           eacs=ar.alloc([8], F32), Reacs=Res("eacs"), cd=ar.alloc([8], F32), Rcd=Res("cd"),
                           MT=ar.alloc([8, 128], BF16), RMT=Res("MT"), xdt=ar.alloc([8, 64], BF16), Rxdt=Res("xdt"),
                           xdtd=ar.alloc([8, 64], BF16), Rxdtd=Res("xdtd")) for _ in range(2)])
            rhsM = ar.alloc([8, 128], F32); RrM = Res("rhsM")
            acsc = ar.alloc([8], F32); Racs = Res("acsc")
            E = ar.alloc([8, 128], F32); RE = Res("E")
            cbm = ar.alloc([2, 128], F32); Rcbm = Res("cbm")
            t1 = ar.alloc([8, 64], F32); Rt1 = Res("t1")
            t2 = ar.alloc([8, 64], F32); Rt2 = Res("t2")
            htmp = ar.alloc([8, 64], F32); Rht = Res("htmp")
            zsb = RR([(ar.alloc([512], F32), Res("zsb")) for _ in range(3)])
            junk = ar.alloc([256], F32); Rj = Res("junkD")
            ssq = ar.alloc([4], F32); Rssq = Res("ssq")
            yd = ar.alloc([512], BF16); Ryd = Res("yd")
            ost = RR([(ar.alloc([4, 128], BF16), Res("ostD")) for _ in range(2)])
            RydT = Res("ydT")
            p_r0, r_r0 = pb[1]; p_r1, r_r1 = pb[2]; p_cb, r_cb = pb[3]
            p_acs = p_cb[:, 256:264]; r_acs = r_cb
            p_yd, r_yd = pb[4]; p_yo, r_yo = pb[5]; p_st, r_st = pb[6]; p_yo1, r_yo1 = pb[0]
            ptw = ptb_t[:]
            curD = {}

            def front(c):
                B = fbs.next()
                xs_tm, Rxs, b_tm, Rbt, eacs, Reacs, cd, Rcd = B["xs_tm"], B["Rxs"], B["b_tm"], B["Rbt"], B["eacs"], B["Reacs"], B["cd"], B["Rcd"]
                MT, RMT, xdt, Rxdt, xdtd, Rxdtd = B["MT"], B["RMT"], B["xdt"], B["Rxdt"], B["xdtd"], B["Rxdtd"]
                cs = slice(c * 128, (c + 1) * 128)
                zb, rzb = zsb.next()
                curD[c] = (B, zb, rzb)
                P.dma("sp", lambda e: e.dma_start(out=zb, in_=zs[cs, :]), writes=[rzb])
                P.op("pe", lambda e: [e.transpose(out=ptw[:, k * 128:(k + 1) * 128], in_=cvo[:, k, cs], identity=ident) for k in range(5)][-1], [Rcv, R_const], [R_ptb])
                P.op("act", lambda e: e.copy(out=xs_tm.rearrange("p h d -> p (h d)"), in_=ptw[:, 0:512]), [R_ptb], [Rxs])
                P.op("dve", lambda e: e.tensor_copy(out=b_tm, in_=ptw[:, 512:640]), [R_ptb], [Rbt])
                P.op("pe", lambda e: e.matmul(p_acs, lhsT=U_f, rhs=dtA[:, c, :], start=True, stop=True), [Rdt, R_const], [r_acs])
                P.op("pool", lambda e: e.tensor_tensor(out=rhsM, in0=U_f.unsqueeze(1).to_broadcast([128, 8, 128]), in1=dtA[:, c, :].unsqueeze(2).to_broadcast([128, 8, 128]), op=ALU.mult), [Rdt, R_const], [RrM])
                P.op("pe", lambda e: e.matmul(p_r0, lhsT=ones_f, rhs=rhsM[:, 0:4, :].rearrange("p a b -> p (a b)"), start=True, stop=True), [RrM, R_const], [r_r0])
                P.op("pe", lambda e: e.matmul(p_r1, lhsT=ones_f, rhs=rhsM[:, 4:8, :].rearrange("p a b -> p (a b)"), start=True, stop=True), [RrM, R_const], [r_r1])
                P.op("dve", lambda e: e.tensor_copy(out=acsc, in_=p_acs), [r_acs], [Racs])
                P.op("act", lambda e: e.activation(out=eacs, in_=p_acs, func=AF.Exp), [r_acs], [Reacs])
                for j, (pr_, rr_) in enumerate(((p_r0, r_r0), (p_r1, r_r1))):
                    P.op("dve", lambda e, pr_=pr_, j=j: e.tensor_tensor(out=E[:, 4 * j:4 * j + 4, :], in0=pr_.rearrange("p (a b) -> p a b", a=4), in1=acsc[:, 4 * j:4 * j + 4].unsqueeze(2).to_broadcast([128, 4, 128]), op=ALU.subtract), [rr_, Racs], [RE])
                    P.op("act", lambda e, pr_=pr_, j=j: e.activation(out=cd[:, 4 * j:4 * j + 4], in_=pr_.rearrange("p (a b) -> p a b", a=4)[:, :, 127], func=AF.Exp), [rr_], [Rcd])
                P.op("act", lambda e: e.activation(out=E, in_=E, func=AF.Exp), [RE], [RE])
                cb_dst = ((p_cb, r_cb), (p_r1, r_r1))
                for g in range(2):
                    pc_, rc_ = cb_dst[g]
                    P.op("pe", lambda e, g=g, pc_=pc_: e.matmul(pc_[:, 0:128], lhsT=cvo[g * 64:(g + 1) * 64, 4, cs], rhs=cvo[g * 64:(g + 1) * 64, 5, cs], start=True, stop=True), [Rcv], [rc_])
                    P.op("dve", lambda e, g=g, pc_=pc_: e.tensor_tensor(out=cbm[:, g, :], in0=pc_[:, 0:128], in1=U_f, op=ALU.mult), [rc_, R_const], [Rcbm])
                for g in range(2):
                    P.op("dve", lambda e, g=g: e.scalar_tensor_tensor(out=MT[:, 4 * g:4 * g + 4, :], in0=E[:, 4 * g:4 * g + 4, :], scalar=1.0, in1=cbm[:, g, :].unsqueeze(1).to_broadcast([128, 4, 128]), op0=ALU.min, op1=ALU.mult), [RE, Rcbm], [RMT])
                P.op("pool", lambda e: e.tensor_tensor(out=xdt, in0=xs_tm, in1=dta[:, c, :].unsqueeze(2).to_broadcast([128, 8, 64]), op=ALU.mult), [Rxs, Rdt], [Rxdt])
                P.op("dve", lambda e: e.tensor_tensor(out=xdtd, in0=xdt, in1=E[:, :, 127].unsqueeze(2).to_broadcast([128, 8, 64]), op=ALU.mult), [Rxdt, RE], [Rxdtd])

            def back(c):
                B, zb, rzb = curD.pop(c)
                xs_tm, Rxs, b_tm, Rbt, eacs, Reacs, cd, Rcd = B["xs_tm"], B["Rxs"], B["b_tm"], B["Rbt"], B["eacs"], B["Reacs"], B["cd"], B["Rcd"]
                MT, RMT, xdt, Rxdt, xdtd, Rxdtd = B["MT"], B["RMT"], B["xdt"], B["Rxdt"], B["xdtd"], B["Rxdtd"]
                cs = slice(c * 128, (c + 1) * 128)

                def ydfn(e):
                    ins = None
                    for h in range(8):
                        ins = e.matmul(p_yd[:, h * 64:(h + 1) * 64], lhsT=MT[:, h, :], rhs=xdt[:, h, :], start=True, stop=True)
                    return ins
                P.op("pe", ydfn, [RMT, Rxdt], [r_yd])
                yo_dst = ((p_yo, r_yo), (p_yo1, r_yo1))
                for g in range(2):
                    py_, ry_ = yo_dst[g]
                    P.op("pe", lambda e, g=g, py_=py_: e.matmul(py_[:, 0:256], lhsT=cvo[g * 64:(g + 1) * 64, 5, cs], rhs=hsb[g * 64:(g + 1) * 64, 4 * g:4 * g + 4, :].rearrange("p a b -> p (a b)"), start=True, stop=True), [Rcv, Rhb], [ry_])
                P.op("pe", lambda e: e.matmul(p_st, lhsT=b_tm, rhs=xdtd.rearrange("p a b -> p (a b)"), start=True, stop=True), [Rbt, Rxdtd], [r_st])
                P.op("pool", lambda e: e.tensor_tensor(out=htmp, in0=hst, in1=cd.unsqueeze(2).to_broadcast([128, 8, 64]), op=ALU.mult), [Rh, Rcd], [Rht])
                P.op("dve", lambda e: e.tensor_tensor(out=hst, in0=htmp, in1=p_st.rearrange("p (a b) -> p a b", a=8), op=ALU.add), [Rht, r_st], [Rh])
                P.op("act", lambda e: e.copy(out=hsb, in_=hst), [Rh], [Rhb])
                for g in range(2):
                    py_, ry_ = yo_dst[g]
                    P.op("dve", lambda e, g=g, py_=py_: e.tensor_tensor(out=t1[:, 4 * g:4 * g + 4, :], in0=py_[:, 0:256].rearrange("p (a b) -> p a b", a=4), in1=eacs[:, 4 * g:4 * g + 4].unsqueeze(2).to_broadcast([128, 4, 64]), op=ALU.mult), [ry_, Reacs], [Rt1])
                P.op("dve", lambda e: e.tensor_tensor(out=t1, in0=t1, in1=p_yd.rearrange("p (a b) -> p a b", a=8), op=ALU.add), [Rt1, r_yd], [Rt1])
                P.op("pool", lambda e: e.tensor_tensor(out=t2, in0=xs_tm, in1=dsk.unsqueeze(2).to_broadcast([128, 8, 64]), op=ALU.mult), [Rxs, Rc], [Rt2])
                P.op("pool", lambda e: e.tensor_tensor(out=t1, in0=t1, in1=t2, op=ALU.add), [Rt1, Rt2], [Rt1])
                P.op("pool", lambda e: e.tensor_tensor(out=t1.rearrange("p a b -> p (a b)"), in0=t1.rearrange("p a b -> p (a b)"), in1=zb, op=ALU.mult), [Rt1, rzb], [Rt1])
                t1f = t1.rearrange("p a b -> p (a b)")
                for g in range(2):
                    P.op("act", lambda e, g=g: e.activation(out=junk, in_=t1f[:, g * 256:(g + 1) * 256], func=AF.Square, accum_out=ssq[:, g:g + 1]), [Rt1], [Rj, Rssq])
                P.op("act", lambda e: e.activation(out=ssq[:, 2:4], in_=ssq[:, 0:2], func=AF.Sqrt, bias=epsb, scale=1.0 / 256), [Rssq, R_const], [Rssq])
                P.op("dve", lambda e: e.reciprocal(out=ssq[:, 2:4], in_=ssq[:, 2:4]), [Rssq], [Rssq])
                P.op("dve", lambda e: e.tensor_tensor(out=t2.rearrange("p (g a) b -> p g (a b)", g=2), in0=t1.rearrange("p (g a) b -> p g (a b)", g=2), in1=ssq[:, 2:4].unsqueeze(2).to_broadcast([128, 2, 256]), op=ALU.mult), [Rt1, Rssq], [Rt2])
                P.op("pool", lambda e: e.tensor_tensor(out=yd, in0=t2.rearrange("p a b -> p (a b)"), in1=ng, op=ALU.mult), [Rt2, Rc], [Ryd])
                P.op("pe", lambda e: [e.transpose(out=ptw[:, k * 128:(k + 1) * 128], in_=yd[:, k * 128:(k + 1) * 128], identity=ident) for k in range(4)][-1], [Ryd, R_const], [R_ptb])
                o_, ro = ost.next()
                P.op("act", lambda e: e.copy(out=o_, in_=ptw[:, 0:512].rearrange("p (k n) -> p k n", k=4)), [R_ptb], [ro])
                P.dma("sp", lambda e: e.dma_start(out=ydT.rearrange("(k p) s -> p k s", p=128)[:, :, cs], in_=o_), reads=[ro], writes=[RydT])

            for c in range(NCH + 1):
                if c < NCH:
                    front(c)
                if c >= 1:
                    back(c - 1)

        def layernorm(t, rt, g_bc, b_bc, Rgb, out_ap, rout, tmp):
            st_, rst = tmp
            P.op("dve", lambda e: e.bn_stats(out=st_[:, 0:6], in_=t[:, 0:512]), [rt], [rst])
            P.op("dve", lambda e: e.bn_stats(out=st_[:, 6:12], in_=t[:, 512:1024]), [rt], [rst])
            P.op("dve", lambda e: e.bn_aggr(out=st_[:, 12:14], in_=st_[:, 0:12]), [rst], [rst])
            P.op("act", lambda e: e.activation(out=st_[:, 14:15], in_=st_[:, 13:14], func=AF.Sqrt, bias=epsb, scale=1.0), [rst, R_const], [rst])
            P.op("dve", lambda e: e.reciprocal(out=st_[:, 15:16], in_=st_[:, 14:15]), [rst], [rst])
            P.op("dve", lambda e: e.tensor_scalar(out=t, in0=t, scalar1=st_[:, 12:13], scalar2=st_[:, 15:16], op0=ALU.subtract, op1=ALU.mult), [rt, rst], [rt])
            P.op("dve", lambda e: e.tensor_tensor(out=t, in0=t, in1=g_bc, op=ALU.mult), [rt, Rgb], [rt])
            P.op("pool", lambda e: e.tensor_tensor(out=out_ap, in0=t, in1=b_bc, op=ALU.add), [rt, Rgb], [rout])

        def to_xT(src, rsrc, dstT, cols, Rdst, xbst, ostt):
            xb_, rxb = xbst.next()
            P.op("act", lambda e: e.copy(out=xb_, in_=src), [rsrc], [rxb])
            o_, ro = ostt.next()
            for half in range(2):
                pt, rpt = ptb[half]
                P.op("pe", lambda e, pt=pt, half=half: [e.transpose(out=pt[:, k * 128:(k + 1) * 128], in_=xb_[:, (half * 4 + k) * 128:(half * 4 + k + 1) * 128], identity=ident) for k in range(4)][-1], [rxb, R_const], [rpt])
                P.op("dve", lambda e, pt=pt, half=half: e.tensor_copy(out=o_[:, half * 4:half * 4 + 4, :], in_=pt.rearrange("p (k n) -> p k n", k=4)), [rpt], [ro])
            P.dma("sp", lambda e: e.dma_start(out=dstT.rearrange("(k p) s -> p k s", p=128)[:, :, cols], in_=o_), reads=[ro], writes=[Rdst])

        def phaseP(li):
            pbuf = RR([(ar.alloc([256], BF16), Res("pbuf")) for _ in range(2)])
            ost = RR([(ar.alloc([2, 128], BF16), Res("ostP")) for _ in range(2)])
            RpT = Res("pT")
            for c in range(NCH):
                b, rb = pbuf.next()
                P.dma("pool", lambda e, b=b, c=c: e.dma_start(out=b, in_=pin[li, c * 128:(c + 1) * 128, :]), writes=[rb])
                pt, rpt = ptb[c % 2]
                P.op("pe", lambda e, pt=pt, b=b: [e.transpose(out=pt[:, k * 128:(k + 1) * 128], in_=b[:, k * 128:(k + 1) * 128], identity=ident) for k in range(2)][-1], [rb, R_const], [rpt])
                o_, ro = ost.next()
                P.op("dve", lambda e, o_=o_, pt=pt: e.tensor_copy(out=o_, in_=pt[:, 0:256].rearrange("p (k n) -> p k n", k=2)), [rpt], [ro])
                P.dma("sp", lambda e, o_=o_, c=c: e.dma_start(out=pT.rearrange("(k p) s -> p k s", p=128)[:, :, c * 128:(c + 1) * 128], in_=o_), reads=[ro], writes=[RpT])

        def phase3(li):
            moe = (li % 2 == 1)
            xsrc = x if li == 0 else xres
            Rw = Res("w3")
            wg = ar.alloc([8, 4096], BF16)
            for k in range(8):
                P.dma("pool", lambda e, k=k: e.dma_start(out=wg[:, k, :], in_=w_in[li, k * 128:(k + 1) * 128, 3336:7432]), writes=[Rw])
            wb = ar.alloc([10, 1024], BF16)
            kofs = [0, 2, 4, 6]
            for b in range(4):
                nk = 4 if b == 3 else 2
                for k in range(nk):
                    P.dma("pool", lambda e, b=b, k=k: e.dma_start(out=wb[:, kofs[b] + k, :], in_=w_br[b][li, k * 128:(k + 1) * 128, :]), writes=[Rw])
            wo = ar.alloc([8, 1024], BF16)
            for k in range(8):
                P.dma("pool", lambda e, k=k: e.dma_start(out=wo[:, k, :], in_=w_out[li, k * 128:(k + 1) * 128, :]), writes=[Rw])
            g_bc = ar.alloc([1024], F32); b_bc = ar.alloc([1024], F32); Rgb = Res("ln1gb")
            P.dma("sp", lambda e: e.dma_start(out=g_bc, in_=ln1_g[li:li + 1, :].partition_broadcast(128)), writes=[Rgb])
            P.dma("sp", lambda e: e.dma_start(out=b_bc, in_=ln1_b[li:li + 1, :].partition_broadcast(128)), writes=[Rgb])
            if moe:
                wr = ar.alloc([8, 8], F32); identf = ar.alloc([128], F32)
                P.dma("sp", lambda e: e.dma_start(out=wr, in_=moe_r[0].rearrange("(k p) e -> p k e", p=128)), writes=[Rw])
                P.dma("sp", lambda e: e.dma_start(out=identf, in_=c_identf), writes=[Rw])
                x1Tf = ar.alloc([8, 128], F32); Rx1Tf = Res("x1Tf")
                rt_ = ar.alloc([64], F32); Rrt = Res("router_tmp")
            xTt = RR([(ar.alloc([8, 512], BF16), Res("xTt3")) for _ in range(2)])
            yT = RR([(ar.alloc([10, 512], BF16), Res("yT3")) for _ in range(2)])
            gsb = RR([(ar.alloc([512], F32), Res("gsb")) for _ in range(2)])
            mtmp = RR([(ar.alloc([512], F32), Res("mtmp")) for _ in range(2)])
            macc = ar.alloc([512], F32); Rmacc = Res("macc")
            mT = ar.alloc([8, 512], BF16); RmT = Res("mT")
            xr = RR([(ar.alloc([1024], F32), Res("xr")) for _ in range(1)])
            tln = RR([(ar.alloc([1024], F32), Res("tln")) for _ in range(1)])
            x1b = RR([(ar.alloc([1024], F32), Res("x1b")) for _ in range(2)])
            lntmp = RR([(ar.alloc([16], F32), Res("lntmp")) for _ in range(2)])
            xbst = RR([(ar.alloc([1024], BF16), Res("xbst")) for _ in range(2)])
            ostt = RR([(ar.alloc([8, 128], BF16), Res("ostt")) for _ in range(2)])
            pg = RR([pb[0], pb[1]]); pp = RR([pb[2], pb[3]]); po_ = RR([pb[4], pb[5]])
            Rx1T = Res("x1T"); Rx1r = Res("x1res"); Rcomb = Res("combs")
            ysrc = [(yaT, 2), (ybT, 2), (ycT, 2), (ydT, 4)]
            for t in range(NT):
                tok = slice(t * 512, (t + 1) * 512)
                xt, rxt = xTt.next()
                P.dma("sp", lambda e, xt=xt, tok=tok: e.dma_start(out=xt, in_=xT.rearrange("(k p) s -> p k s", p=128)[:, :, tok]), writes=[rxt])
                yt, ryt = yT.next()
                for b in range(4):
                    src, nk = ysrc[b]
                    P.dma("sp", lambda e, yt=yt, src=src, nk=nk, b=b, tok=tok: e.dma_start(out=yt[:, kofs[b]:kofs[b] + nk, :], in_=src.rearrange("(k p) s -> p k s", p=128)[:, :, tok]), writes=[ryt])
                for fc in range(8):
                    for b in range(4):
                        nk = 4 if b == 3 else 2
                        pg_, rpg = pg.next()
                        c0 = b * 1024 + fc * 128
                        mm_group(pg_, [(wg[:, k, c0:c0 + 128], xt[:, k, :]) for k in range(8)], [Rw, rxt], [rpg])
                        g_, rg = gsb.next()
                        P.op("act", lambda e, g_=g_, pg_=pg_: e.activation(out=g_, in_=pg_, func=AF.Sigmoid), [rpg], [rg])
                        pp_, rpp = pp.next()
                        mm_group(pp_, [(wb[:, kofs[b] + k, fc * 128:(fc + 1) * 128], yt[:, kofs[b] + k, :]) for k in range(nk)], [Rw, ryt], [rpp])
                        if b == 0:
                            P.op("dve", lambda e, g_=g_, pp_=pp_: e.tensor_tensor(out=macc, in0=g_, in1=pp_, op=ALU.mult), [rg, rpp], [Rmacc])
                        else:
                            m_, rm = mtmp.next()
                            P.op("dve", lambda e, g_=g_, pp_=pp_, m_=m_: e.tensor_tensor(out=m_, in0=g_, in1=pp_, op=ALU.mult), [rg, rpp], [rm])
                            if b < 3:
                                P.op("pool", lambda e, m_=m_: e.tensor_tensor(out=macc, in0=macc, in1=m_, op=ALU.add), [Rmacc, rm], [Rmacc])
                            else:
                                P.op("pool", lambda e, m_=m_, fc=fc: e.tensor_tensor(out=mT[:, fc, :], in0=macc, in1=m_, op=ALU.add), [Rmacc, rm], [RmT])
                for cc in range(4):
                    c = t * 4 + cc
                    rows = slice(c * 128, (c + 1) * 128)
                    xr_, rxr = xr.next()
                    P.dma("sp", lambda e, xr_=xr_, rows=rows: e.dma_start(out=xr_, in_=xsrc[rows, :]), writes=[rxr])
                    tl, rtl = tln.next()
                    for half in range(2):
                        po, rpo = po_.next()
                        hs = slice(half * 512, (half + 1) * 512)
                        mm_group(po, [(mT[:, k, cc * 128:(cc + 1) * 128], wo[:, k, hs]) for k in range(8)], [RmT, Rw], [rpo])
                        P.op("dve", lambda e, tl=tl, xr_=xr_, po=po, hs=hs: e.scalar_tensor_tensor(out=tl[:, hs], in0=xr_[:, hs], scalar=ALPHA, in1=po, op0=ALU.mult, op1=ALU.add), [rxr, rpo], [rtl])
                    x1_, rx1 = x1b.next()
                    layernorm(tl, rtl, g_bc, b_bc, Rgb, x1_, rx1, lntmp.next())
                    P.dma("sp", lambda e, x1_=x1_, rows=rows: e.dma_start(out=x1res[rows, :], in_=x1_), reads=[rx1], writes=[Rx1r])
                    to_xT(x1_, rx1, x1T, rows, Rx1T, xbst, ostt)
                    if moe:
                        for half in range(2):
                            pr_, rpr = pb[half]
                            P.op("pe", lambda e, pr_=pr_, half=half, x1_=x1_: [e.transpose(out=pr_[:, k * 128:(k + 1) * 128], in_=x1_[:, (half * 4 + k) * 128:(half * 4 + k + 1) * 128], identity=identf) for k in range(4)][-1], [rx1, Rw], [rpr])
                            P.op("act", lambda e, pr_=pr_, half=half: e.copy(out=x1Tf[:, half * 4:half * 4 + 4, :], in_=pr_.rearrange("p (k n) -> p k n", k=4)), [rpr], [Rx1Tf])
                        pl, rpl = pb[2]
                        mm_group(pl[:, 0:8], [(x1Tf[:, k, :], wr[:, k, :]) for k in range(8)], [Rx1Tf, Rw], [rpl])
                        lg = rt_[:, 0:8]; eq = rt_[:, 8:16]; lg2 = rt_[:, 16:24]; sel = rt_[:, 24:32]; ex = rt_[:, 32:40]
                        m1 = rt_[:, 40:41]; m2 = rt_[:, 41:42]; nm1 = rt_[:, 42:43]; den = rt_[:, 43:44]; cmb = rt_[:, 48:56]
                        P.op("dve", lambda e: e.tensor_copy(out=lg, in_=pl[:, 0:8]), [rpl], [Rrt])
                        P.op("dve", lambda e: e.reduce_max(out=m1, in_=lg, axis=mybir.AxisListType.X), [Rrt], [Rrt])
                        P.op("dve", lambda e: e.tensor_scalar(out=eq, in0=lg, scalar1=m1, scalar2=None, op0=ALU.is_equal), [Rrt], [Rrt])
                        P.op("dve", lambda e: e.scalar_tensor_tensor(out=lg2, in0=eq, scalar=-1e30, in1=lg, op0=ALU.mult, op1=ALU.add), [Rrt], [Rrt])
                        P.op("dve", lambda e: e.reduce_max(out=m2, in_=lg2, axis=mybir.AxisListType.X), [Rrt], [Rrt])
                        P.op("dve", lambda e: e.tensor_scalar(out=sel, in0=lg, scalar1=m2, scalar2=None, op0=ALU.is_ge), [Rrt], [Rrt])
                        P.op("dve", lambda e: e.tensor_scalar(out=nm1, in0=m1, scalar1=-1.0, scalar2=None, op0=ALU.mult), [Rrt], [Rrt])
                        P.op("act", lambda e: e.activation(out=ex, in_=lg, func=AF.Exp, bias=nm1, scale=1.0), [Rrt], [Rrt])
                        P.op("dve", lambda e: e.tensor_tensor(out=ex, in0=ex, in1=sel, op=ALU.mult), [Rrt], [Rrt])
                        P.op("dve", lambda e: e.reduce_sum(out=den, in_=ex, axis=mybir.AxisListType.X), [Rrt], [Rrt])
                        P.op("dve", lambda e: e.reciprocal(out=den, in_=den), [Rrt], [Rrt])
                        P.op("dve", lambda e: e.tensor_scalar(out=cmb, in0=ex, scalar1=den, scalar2=None, op0=ALU.mult), [Rrt], [Rrt])
                        P.dma("sp", lambda e, c=c: e.dma_start(out=combs[:, c * 8:(c + 1) * 8], in_=cmb), reads=[Rrt], writes=[Rcomb])

        def phase4(li):
            moe = (li % 2 == 1)
            last = (li == 1)
            TT = min(S, 1024)
            NCT = TT // 128
            Rw = Res("w4")
            wpg = ar.alloc([8, 1024], BF16); wpp = ar.alloc([2, 1024], BF16)
            for k in range(8):
                P.dma("pool", lambda e, k=k: e.dma_start(out=wpg[:, k, :], in_=ple_wg[li, k * 128:(k + 1) * 128, :]), writes=[Rw])
            for k in range(2):
                P.dma("pool", lambda e, k=k: e.dma_start(out=wpp[:, k, :], in_=ple_wp[li, k * 128:(k + 1) * 128, :]), writes=[Rw])
            g_bc = ar.alloc([1024], F32); b_bc = ar.alloc([1024], F32); Rgb = Res("ln2gb")
            P.dma("sp", lambda e: e.dma_start(out=g_bc, in_=ln2_g[li:li + 1, :].partition_broadcast(128)), writes=[Rgb])
            P.dma("sp", lambda e: e.dma_start(out=b_bc, in_=ln2_b[li:li + 1, :].partition_broadcast(128)), writes=[Rgb])
            acc = ar.alloc([NCT, 1024], F32); Racc = [Res(f"acc{i}") for i in range(NCT)]
            tin = RR([(ar.alloc([8, TT], BF16), ar.alloc([2, TT], BF16), ar.alloc([NCT, 8], F32), Res(f"tin{i}")) for i in range(2)])
            wgs = RR([(ar.alloc([8, 512], BF16), Res("wgs")) for _ in range(2)])
            wus = RR([(ar.alloc([8, 512], BF16), Res("wus")) for _ in range(2)])
            wds = RR([(ar.alloc([4, 1024], BF16), Res("wds")) for _ in range(2)])
            actT = RR([(ar.alloc([4, TT], BF16), Res("actT")) for _ in range(2)])
            sgl = RR([(ar.alloc([512], F32), Res("sgl")) for _ in range(2)])
            xr = RR([(ar.alloc([1024], F32), Res("xr4")) for _ in range(2)])
            x2b = RR([(ar.alloc([1024], F32), Res("x2b")) for _ in range(2)])
            lntmp = RR([(ar.alloc([16], F32), Res("lntmp4")) for _ in range(2)])
            xbst = RR([(ar.alloc([1024], BF16), Res("xbst4")) for _ in range(2)])
            ostt = RR([(ar.alloc([8, 128], BF16), Res("ostt4")) for _ in range(2)])
            pgu = RR([(pb[0], pb[1]), (pb[2], pb[3])])
            pdn = RR([pb[4], pb[5]])
            Rout = Res("outdst"); RxTn = Res("xTnext")
            if moe:
                experts = [(moe_wg[0, e_], moe_wu[0, e_], moe_wd[0, e_], D_FFE, e_) for e_ in range(8)]
            else:
                experts = [(ffn_wg[0], ffn_wu[0], ffn_wd[0], D_FF, None)]

            def load_tile(tt):
                x1t, pt_, cmbt, rti = tin.next()
                tok = slice(tt * TT, (tt + 1) * TT)
                P.dma("sp", lambda e: e.dma_start(out=x1t, in_=x1T.rearrange("(k p) s -> p k s", p=128)[:, :, tok]), writes=[rti])
                P.dma("sp", lambda e: e.dma_start(out=pt_, in_=pT.rearrange("(k p) s -> p k s", p=128)[:, :, tok]), writes=[rti])
                if moe:
                    P.dma("sp", lambda e: e.dma_start(out=cmbt, in_=combs[:, tt * NCT * 8:(tt + 1) * NCT * 8].rearrange("p (c e) -> p c e", e=8)), writes=[rti])
                return x1t, pt_, cmbt, rti

            def ple(cc, hd):
                x1t, pt_, cmbt, rti = hd
                cs = slice(cc * 128, (cc + 1) * 128)
                for half in range(2):
                    hs = slice(half * 512, (half + 1) * 512)
                    (pa, rpa), (pb2, rpb2) = pgu.next()
                    mm_group(pa, [(x1t[:, k, cs], wpg[:, k, hs]) for k in range(8)], [rti, Rw], [rpa])
                    mm_group(pb2, [(pt_[:, k, cs], wpp[:, k, hs]) for k in range(2)], [rti, Rw], [rpb2])
                    sg_, rsg = sgl.next()
                    P.op("act", lambda e, sg_=sg_, pa=pa: e.activation(out=sg_, in_=pa, func=AF.Sigmoid), [rpa], [rsg])
                    P.op("dve", lambda e, sg_=sg_, pb2=pb2, hs=hs: e.tensor_tensor(out=acc[:, cc, hs], in0=sg_, in1=pb2, op=ALU.mult), [rsg, rpb2], [Racc[cc]])

            def ffn(hd):
                x1t, pt_, cmbt, rti = hd
                for (wg_d, wu_d, wd_d, dff, eidx) in experts:
                    f0 = 0
                    while f0 < dff:
                        fw = min(512, dff - f0)
                        nfc = fw // 128
                        wg_, rwg = wgs.next(); wu_, rwu = wus.next(); wd_, rwd = wds.next()
                        for k in range(8):
                            P.dma("pool", lambda e, wg_=wg_, wg_d=wg_d, k=k, f0=f0, fw=fw: e.dma_start(out=wg_[:, k, 0:fw], in_=wg_d[k * 128:(k + 1) * 128, f0:f0 + fw]), writes=[rwg])
                            P.dma("pool", lambda e, wu_=wu_, wu_d=wu_d, k=k, f0=f0, fw=fw: e.dma_start(out=wu_[:, k, 0:fw], in_=wu_d[k * 128:(k + 1) * 128, f0:f0 + fw]), writes=[rwu])
                        for fc in range(nfc):
                            P.dma("pool", lambda e, wd_=wd_, wd_d=wd_d, fc=fc, f0=f0: e.dma_start(out=wd_[:, fc, :], in_=wd_d[f0 + fc * 128:f0 + (fc + 1) * 128, :]), writes=[rwd])
                        at, rat = actT.next()
                        for ts in range(TT // 512):
                            tsl = slice(ts * 512, (ts + 1) * 512)
                            for fc in range(nfc):
                                (pgt, rpgt), (put, rput) = pgu.next()
                                fs = slice(fc * 128, (fc + 1) * 128)
                                mm_group(pgt, [(wg_[:, k, fs], x1t[:, k, tsl]) for k in range(8)], [rwg, rti], [rpgt])
                                mm_group(put, [(wu_[:, k, fs], x1t[:, k, tsl]) for k in range(8)], [rwu, rti], [rput])
                                sg_, rsg = sgl.next()
                                P.op("act", lambda e, sg_=sg_, pgt=pgt: e.activation(out=sg_, in_=pgt, func=AF.Silu), [rpgt], [rsg])
                                P.op("dve", lambda e, sg_=sg_, put=put, at=at, fc=fc, tsl=tsl: e.tensor_tensor(out=at[:, fc, tsl], in0=sg_, in1=put, op=ALU.mult), [rsg, rput], [rat])
                        for cc in range(NCT):
                            cs = slice(cc * 128, (cc + 1) * 128)
                            for half in range(2):
                                hs = slice(half * 512, (half + 1) * 512)
                                pd, rpd = pdn.next()
                                mm_group(pd, [(at[:, fc, cs], wd_[:, fc, hs]) for fc in range(nfc)], [rat, rwd], [rpd])
                                if eidx is None:
                                    P.op("dve", lambda e, pd=pd, cc=cc, hs=hs: e.tensor_tensor(out=acc[:, cc, hs], in0=acc[:, cc, hs], in1=pd, op=ALU.add), [rpd, Racc[cc]], [Racc[cc]])
                                else:
                                    P.op("dve", lambda e, pd=pd, cc=cc, hs=hs, eidx=eidx: e.scalar_tensor_tensor(out=acc[:, cc, hs], in0=pd, scalar=cmbt[:, cc, eidx:eidx + 1], in1=acc[:, cc, hs], op0=ALU.mult, op1=ALU.add), [rpd, Racc[cc], rti], [Racc[cc]])
                        f0 += fw

            def epi(tt, cc):
                c = tt * NCT + cc
                rows = slice(c * 128, (c + 1) * 128)
                xr_, rxr = xr.next()
                P.dma("sp", lambda e: e.dma_start(out=xr_, in_=x1res[rows, :]), writes=[rxr])
                P.op("dve", lambda e: e.scalar_tensor_tensor(out=acc[:, cc, :], in0=xr_, scalar=ALPHA, in1=acc[:, cc, :], op0=ALU.mult, op1=ALU.add), [rxr, Racc[cc]], [Racc[cc]])
                x2_, rx2 = x2b.next()
                layernorm(acc[:, cc, :], Racc[cc], g_bc, b_bc, Rgb, x2_, rx2, lntmp.next())
                dst = out if last else xres
                P.dma("sp", lambda e: e.dma_start(out=dst[rows, :], in_=x2_), reads=[rx2], writes=[Rout])
                if not last:
                    to_xT(x2_, rx2, xT, rows, RxTn, xbst, ostt)

            ntile = S // TT
            hd = load_tile(0)
            for cc in range(NCT):
                ple(cc, hd)
            for tt in range(ntile):
                hn = load_tile(tt + 1) if tt + 1 < ntile else None
                ffn(hd)
                for cc in range(NCT):
                    epi(tt, cc)
                    if hn is not None:
                        ple(cc, hn)
                hd = hn

        if "p0" in phases:
            phase0()
            P.barrier()
            ar.top = base_top
        for li in (0, 1):
            if f"l{li}" not in phases:
                continue
            if "1" in subph:
                phase1(li)
                P.barrier()
                ar.top = base_top
            if "A" in subph:
                phaseA()
                P.barrier()
                ar.top = base_top
            if "C" in subph:
                phaseC(li)
                P.barrier()
                ar.top = base_top
            if "D" in subph:
                phaseD(li)
                P.barrier()
                ar.top = base_top
            if "34" in subph:
                phaseP(li)
                P.barrier()
                ar.top = base_top
                phase3(li)
                P.barrier()
                ar.top = base_top
                phase4(li)
                P.barrier()
                ar.top = base_top

        P.barrier()
        lasts = list(P.dma_last.values())
        P.emit(final_ops=lasts)
    P.inputs = I
    return nc, P


def host_consts():
    r = np.arange(128)
    U = (r[:, None] <= r[None, :]).astype(np.float32)
    Lm8 = -8.0 * (r[:, None] >= r[None, :]).astype(np.float32)
    t = np.arange(512)
    maskd = np.zeros((128, 4, 512), np.float32)
    for i in range(4):
        maskd[:, i, :] = (r[:, None] + 128 * i < t[None, :])
    return {"c_ident": np.eye(128, dtype=np.float32), "c_U": U, "c_Lm8": Lm8, "c_maskd": maskd}


def host_layout(inputs, b, S):
    f = lambda a: np.ascontiguousarray(np.asarray(a, dtype=np.float32))
    m = {}
    m["x"] = f(inputs["x"][b, :S])
    m["p"] = f(inputs["p"][:, b, :S])
    for k in ["w_in", "w_br_a", "w_br_b", "w_br_c", "w_br_d", "w_out", "sg_ln_g", "sg_ln_b",
              "ssd_dt_bias", "ssd_a_log", "ssd_d", "ssd_norm_g", "ln1_g", "ln1_b",
              "ffn_w_gate", "ffn_w_up", "ffn_w_down", "moe_router", "moe_w_gate", "moe_w_up",
              "moe_w_down", "ple_w_gate", "ple_w_proj", "ln2_g", "ln2_b"]:
        m[k] = f(inputs[k])
    m["sg_wT"] = f(np.transpose(np.asarray(inputs["sg_w"]), (0, 3, 1, 2)))
    m["sg_bT"] = f(np.transpose(np.asarray(inputs["sg_b"]), (0, 2, 1)))
    rb = np.asarray(inputs["ca_rel_bias"], dtype=np.float32)
    j = np.arange(128)[:, None, None]
    o = np.arange(5)[None, :, None]
    i = np.arange(128)[None, None, :]
    tq = 512 + i
    sk = o * 128 + j
    rel = tq - sk
    idx = np.clip(rel, -63, 256) + 63
    diff = tq // 64 - sk // 64
    ok = (diff >= 0) & (diff <= 8)
    g = rb[:, :, idx]
    g = np.where(ok[None, None], g, np.float32(NEG))
    m["ca_biasT"] = f(np.transpose(g, (0, 2, 1, 3, 4)))
    cw = np.asarray(inputs["ssd_conv_w"], dtype=np.float32)
    m["conv_wT"] = f(np.transpose(cw.reshape(2, 4, 6, 128), (0, 3, 2, 1)))
    cb = np.asarray(inputs["ssd_conv_b"], dtype=np.float32)
    m["conv_bT"] = f(np.transpose(cb.reshape(2, 6, 128), (0, 2, 1)))
    m.update(host_consts())
    return m


def kernel(**inputs):
    S = 8192
    nc, _ = build(S)
    in_maps = [host_layout(inputs, b, S) for b in range(8)]
    res = run_bass_kernel_spmd(nc, in_maps, core_ids=list(range(8)))
    return np.stack([np.asarray(r["out"], dtype=np.float32) for r in res.results], axis=0)
```
